# Optimizing a Trainium2 kernel written in Bass

```python
import jax, jax.numpy as jnp
from jax import lax
import numpy as np

D_MODEL = 1024
BATCH = 8
SEQ = 2048
DEPTH = 2

MEM_TOKENS = 256
HEAD_DIM = 64
CONV_CH = 3 * D_MODEL // 8
N_DIL_HEADS = (3 * D_MODEL // 8) // HEAD_DIM
N_MEM_HEADS = 4
DIL_WIDTH = N_DIL_HEADS * HEAD_DIM
MEM_WIDTH = N_MEM_HEADS * HEAD_DIM
MIX_WIDTH = CONV_CH + DIL_WIDTH + MEM_WIDTH
IN_SPLITS = [CONV_CH, 2 * CONV_CH, 3 * CONV_CH,
             3 * CONV_CH + DIL_WIDTH, 3 * CONV_CH + 2 * DIL_WIDTH, 3 * CONV_CH + 3 * DIL_WIDTH]
IN_PROJ_WIDTH = 3 * CONV_CH + 3 * DIL_WIDTH + MEM_WIDTH
CONV_KSIZE = 3
DILATED_PATTERNS = ((128, 1), (512, 4), (2048, 16))
N_EXPERTS = 16
D_FF = 2 * D_MODEL
EC_CAPACITY = 2
RMS_EPS = 1e-6
NEG_INF = -1e30

kernel_name = "hybrid_conv_dilattn_mem_ecmoe_encoder"


def rmsnorm(x, g):
    xf = x.astype(jnp.float32)
    y = xf * lax.rsqrt(jnp.mean(xf * xf, axis=-1, keepdims=True) + RMS_EPS)
    return (y * g.astype(jnp.float32)).astype(x.dtype)


def alibi_slopes(n):
    return jnp.asarray([2.0 ** (-8.0 * (h + 1) / n) for h in range(n)], dtype=jnp.float32)


def split_heads(t, n):
    b, s, _ = t.shape
    return t.reshape(b, s, n, HEAD_DIM).transpose(0, 2, 1, 3)


def merge_heads(t):
    b, h, s, d = t.shape
    return t.transpose(0, 2, 1, 3).reshape(b, s, h * d)


def short_conv_mixer(xc, b_gate, c_gate, conv_w):
    s = xc.shape[1]
    u = c_gate * xc
    up = jnp.pad(u, ((0, 0), (1, 1), (0, 0)))
    conv = conv_w[0] * up[:, :s] + conv_w[1] * up[:, 1:s + 1] + conv_w[2] * up[:, 2:]
    return b_gate * conv


def dilated_branch(q, k, v, slopes, window, dil):
    bsz, h, s, hd = q.shape
    half = window // (2 * dil)
    L = s // dil
    nb = -(-L // half)
    Lp = nb * half

    def to_res(t):
        return t.reshape(bsz, h, L, dil, hd).transpose(0, 1, 3, 2, 4)

    qr, kr, vr = to_res(q), to_res(k), to_res(v)
    qr = jnp.pad(qr, ((0, 0), (0, 0), (0, 0), (0, Lp - L), (0, 0)))
    pad_k = ((0, 0), (0, 0), (0, 0), (half, Lp - L + half), (0, 0))
    kr, vr = jnp.pad(kr, pad_k), jnp.pad(vr, pad_k)
    qb = qr.reshape(bsz, h, dil, nb, half, hd)

    def band(t):
        tb = t.reshape(bsz, h, dil, nb + 2, half, hd)
        return jnp.concatenate([tb[:, :, :, :-2], tb[:, :, :, 1:-1], tb[:, :, :, 2:]], axis=4)

    kb, vb = band(kr), band(vr)
    scores = jnp.einsum('bhrnqd,bhrnkd->bhrnqk', qb, kb) * (HEAD_DIM ** -0.5)
    qi = jnp.arange(nb)[:, None, None] * half + jnp.arange(half)[None, :, None]
    kj = jnp.arange(nb)[:, None, None] * half - half + jnp.arange(3 * half)[None, None, :]
    rel = kj - qi
    valid = (jnp.abs(rel) <= half) & (kj >= 0) & (kj < L)
    dist = (jnp.abs(rel) * dil).astype(jnp.float32)
    bias = -slopes[:, None, None, None] * dist[None]
    scores = jnp.where(valid[None, None, None], scores + bias[None, :, None], NEG_INF)
    m = jnp.max(scores, axis=-1, keepdims=True)
    p = jnp.exp(scores - m)
    l = jnp.sum(p, axis=-1, keepdims=True)
    o = jnp.einsum('bhrnqk,bhrnkd->bhrnqd', p, vb) / l
    lse = (m + jnp.log(l))[..., 0]
    o = o.reshape(bsz, h, dil, Lp, hd)[:, :, :, :L].transpose(0, 1, 3, 2, 4).reshape(bsz, h, s, hd)
    lse = lse.reshape(bsz, h, dil, Lp)[..., :L].transpose(0, 1, 3, 2).reshape(bsz, h, s)
    return o, lse


def dilated_attention(q, k, v, slopes):
    outs, lses = [], []
    for window, dil in DILATED_PATTERNS:
        o, lse = dilated_branch(q, k, v, slopes, window, dil)
        outs.append(o)
        lses.append(lse)
    w = jax.nn.softmax(jnp.stack(lses, axis=0), axis=0)
    return jnp.sum(w[..., None] * jnp.stack(outs, axis=0), axis=0)


def memory_attention(qm, km, vm):
    scores = jnp.einsum('bhqd,bhkd->bhqk', qm.astype(jnp.float32), km.astype(jnp.float32)) * (HEAD_DIM ** -0.5)
    p = jax.nn.softmax(scores, axis=-1)
    return jnp.einsum('bhqk,bhkd->bhqd', p, vm.astype(jnp.float32))


def parallel_mixer(xn, mem_n, w_in, conv_w, w_mem_kv, w_out, slopes):
    h = xn @ w_in
    xc, bg, cg, q, k, v, qm = jnp.split(h, IN_SPLITS, axis=-1)
    conv_out = short_conv_mixer(xc, bg, cg, conv_w)
    f32 = jnp.float32
    dil = dilated_attention(split_heads(q, N_DIL_HEADS).astype(f32),
                            split_heads(k, N_DIL_HEADS).astype(f32),
                            split_heads(v, N_DIL_HEADS).astype(f32), slopes)
    dil_out = merge_heads(dil).astype(xn.dtype)
    km, vm = jnp.split(mem_n @ w_mem_kv, 2, axis=-1)
    mo = memory_attention(split_heads(qm, N_MEM_HEADS), split_heads(km, N_MEM_HEADS),
                          split_heads(vm, N_MEM_HEADS))
    mem_out = merge_heads(mo).astype(xn.dtype)
    return jnp.concatenate([conv_out, dil_out, mem_out], axis=-1) @ w_out


def expert_choice_ffn(xn, w_router, w_gate, w_up, w_down):
    bsz, s, d = xn.shape
    cap = EC_CAPACITY * s // N_EXPERTS
    aff = jax.nn.softmax((xn @ w_router).astype(jnp.float32), axis=-1)
    g, idx = lax.top_k(aff.transpose(0, 2, 1), cap)
    xg = jax.vmap(lambda xb, ib: xb[ib])(xn, idx)
    hid = jax.nn.silu(jnp.einsum('becd,edf->becf', xg, w_gate)) * jnp.einsum('becd,edf->becf', xg, w_up)
    y = jnp.einsum('becf,efd->becd', hid, w_down) * g[..., None].astype(xn.dtype)
    return jax.vmap(lambda yb, ib: jax.ops.segment_sum(yb.reshape(-1, d), ib.reshape(-1), num_segments=s))(y, idx)


def setup_inputs(seed: int = 0) -> dict:
    key = jax.random.key(seed)
    ks = jax.random.split(key, 14)
    f32 = jnp.float32
    D, L, E, F = D_MODEL, DEPTH, N_EXPERTS, D_FF
    nrm = lambda k, shape, scale: (jax.random.normal(k, shape, f32) * scale).astype(f32)
    return {
        "x": nrm(ks[0], (BATCH, SEQ, D), 1.0),
        "mem": nrm(ks[1], (BATCH, MEM_TOKENS, D), 1.0),
        "mem_norm": 1.0 + nrm(ks[2], (D,), 0.02),
        "norm_mix": 1.0 + nrm(ks[3], (L, D), 0.02),
        "w_in": nrm(ks[4], (L, D, IN_PROJ_WIDTH), D ** -0.5),
        "conv_w": nrm(ks[5], (L, CONV_KSIZE, CONV_CH), 0.5),
        "w_mem_kv": nrm(ks[6], (L, D, 2 * MEM_WIDTH), D ** -0.5),
        "w_out": nrm(ks[7], (L, MIX_WIDTH, D), MIX_WIDTH ** -0.5),
        "norm_ffn": 1.0 + nrm(ks[8], (L, D), 0.02),
        "w_router": nrm(ks[9], (L, D, E), D ** -0.5),
        "w_gate": nrm(ks[10], (L, E, D, F), D ** -0.5),
        "w_up": nrm(ks[11], (L, E, D, F), D ** -0.5),
        "w_down": nrm(ks[12], (L, E, F, D), F ** -0.5),
        "norm_final": 1.0 + nrm(ks[13], (D,), 0.02),
    }


def reference(x, mem, mem_norm, norm_mix, w_in, conv_w, w_mem_kv, w_out, norm_ffn,
              w_router, w_gate, w_up, w_down, norm_final):
    slopes = alibi_slopes(N_DIL_HEADS)
    mem_n = rmsnorm(mem, mem_norm)
    for l in range(DEPTH):
        x = x + parallel_mixer(rmsnorm(x, norm_mix[l]), mem_n, w_in[l], conv_w[l],
                               w_mem_kv[l], w_out[l], slopes)
        x = x + expert_choice_ffn(rmsnorm(x, norm_ffn[l]), w_router[l], w_gate[l], w_up[l], w_down[l])
    return rmsnorm(x, norm_final)
```

```python
import contextlib
import numpy as np
import ml_dtypes
import concourse.bass as bass
import concourse.mybir as mybir
from concourse.bass_utils import run_bass_kernel_spmd

F32 = mybir.dt.float32
BF16 = mybir.dt.bfloat16
AF = mybir.ActivationFunctionType
ALU = mybir.AluOpType
AX = mybir.AxisListType

D = 1024
S = 2048
NT = 16
KC = 8
L = 2
E = 16
FF = 2048
CAP = 256
MEM = 256
EPS = 1e-6
ET_W = 2944
ET_OFF = 1408
N_CORES = 8


class _Rec:
    def __init__(self):
        self.call = None

    def __getattr__(self, name):
        def f(*a, **k):
            self.call = (name, a, k)
            return self
        return f


class Prog:
    def __init__(self):
        self.ops = []
        self.last_writer = {}
        self.readers = {}
        self.chan_count = {}
        self.last_eng = {}
        self.last_chan = {}

    def add(self, eng, fn, reads=(), writes=(), dma_chan=None, extra_deps=()):
        idx = len(self.ops)
        deps = set(extra_deps)
        for k in reads:
            w = self.last_writer.get(k)
            if w is not None:
                deps.add(w)
        for k in writes:
            w = self.last_writer.get(k)
            if w is not None:
                deps.add(w)
            for r in self.readers.get(k, {}).values():
                deps.add(r)
        deps.discard(idx)
        rkey = eng if dma_chan is None else ("dma", idx)
        for k in reads:
            self.readers.setdefault(k, {})[rkey] = idx
        for k in writes:
            self.last_writer[k] = idx
            self.readers[k] = {}
        if fn is not None:
            rec = _Rec()
            fn(rec)
            call = rec.call
            assert call is not None
            fn = (lambda e, call=call: getattr(e, call[0])(*call[1], **call[2]))
        op = dict(eng=eng, fn=fn, deps=deps, chan=dma_chan, signal=False)
        if dma_chan is not None:
            self.chan_count[dma_chan] = self.chan_count.get(dma_chan, 0) + 16
            op["dmaval"] = self.chan_count[dma_chan]
            self.last_chan[dma_chan] = idx
        elif fn is not None:
            self.last_eng[eng] = idx
        self.ops.append(op)
        return idx

    def barrier(self):
        deps = set(self.last_eng.values()) | set(self.last_chan.values())
        for e in ["pe", "act", "dve", "pool", "sp"]:
            self.add(e, None, extra_deps=deps)

    def emit(self, nc, final_waits=()):
        ops = self.ops
        for op in ops:
            for d in op["deps"]:
                p = ops[d]
                if p["chan"] is None:
                    if p["eng"] == "pe" and op["eng"] == "pe" and op["chan"] is None and op["fn"] is not None:
                        continue
                    p["signal"] = True
        for (_, fo) in final_waits:
            if ops[fo]["chan"] is None:
                ops[fo]["signal"] = True
        engs = ["pe", "act", "dve", "pool", "sp"]
        seq = {e: 0 for e in engs}
        for op in ops:
            if op["chan"] is None and op["signal"]:
                seq[op["eng"]] += 1
                op["seqval"] = seq[op["eng"]]
        chans = sorted(self.chan_count.keys(), key=str)
        with contextlib.ExitStack() as st:
            esem = {e: st.enter_context(nc.semaphore("s_" + e)) for e in engs}
            csem = {c: st.enter_context(nc.semaphore("c_%d" % i)) for i, c in enumerate(chans)}
            block = st.enter_context(nc.Block())

            def run_engine(ename):
                def body(eng):
                    waited = {}
                    for op in ops:
                        if op["eng"] != ename:
                            continue
                        for d in sorted(op["deps"]):
                            p = ops[d]
                            if p["chan"] is not None:
                                key = ("c", p["chan"]); val = p["dmaval"]; sem = csem[p["chan"]]
                            else:
                                if p["eng"] == "pe" and ename == "pe" and op["chan"] is None and op["fn"] is not None:
                                    continue
                                key = ("e", p["eng"]); val = p["seqval"]; sem = esem[p["eng"]]
                            if waited.get(key, 0) >= val:
                                continue
                            eng.wait_ge(sem, val)
                            waited[key] = val
                        if op["fn"] is None:
                            continue
                        ins = op["fn"](eng)
                        if op["chan"] is not None:
                            ins.then_inc(csem[op["chan"]], 16)
                        elif op["signal"]:
                            ins.then_inc(esem[ename], 1)
                    for (e2, fo) in final_waits:
                        if e2 != ename:
                            continue
                        p = ops[fo]
                        if p["chan"] is not None:
                            eng.wait_ge(csem[p["chan"]], p["dmaval"])
                        else:
                            eng.wait_ge(esem[p["eng"]], p["seqval"])
                return body

            block.tensor(run_engine("pe"))
            block.scalar(run_engine("act"))
            block.vector(run_engine("dve"))
            block.gpsimd(run_engine("pool"))
            block.sync(run_engine("sp"))


class Arena:
    def __init__(self, t, nbytes):
        self.t = t
        self.n = nbytes
        self.off = 0
        self.mark = 0

    def take(self, nbytes, dt=F32, pattern=None, **kw):
        off = self.off
        self.off += (nbytes + 63) // 64 * 64
        assert self.off <= self.n, ("arena overflow", self.off, self.n)
        ap = self.t[:, off // 4:(off + nbytes) // 4]
        if dt != F32:
            ap = ap.bitcast(dt)
        if pattern:
            ap = ap.rearrange(pattern, **kw)
        return ap


def build_program(n_layers=L, do_moe=True, debug=False):
    nc = bass.Bass("TRN2", target_bir_lowering=False)

    def din(name, shape, dt=F32):
        return nc.dram_tensor(name, list(shape), dt, kind="ExternalInput").ap()

    x_d = din("x", [S, D])
    mem_d = din("mem", [MEM, D])
    mem_norm_d = din("mem_norm", [1, D])
    norm_mix_d = din("norm_mix", [L, D])
    norm_ffn_d = din("norm_ffn", [L, D])
    norm_final_d = din("norm_final", [1, D])
    w_in_d = din("w_in", [L, D, 2560])
    convw_d = din("convw_t", [L, 128, 9])
    w_mkv_d = din("w_mem_kv", [L, D, 512])
    w_out_d = din("w_out", [L, D, D])
    if do_moe:
        w_router_d = din("w_router", [L, D, E])
        w_gate_d = din("w_gate", [L, E, D, FF])
        w_up_d = din("w_up", [L, E, D, FF])
        w_down_d = din("w_down", [L, E, FF, D])
    identb_d = din("ident_bf", [128, 128], BF16)
    identf_d = din("ident_f", [128, 128])
    iota1_d = din("iota1", [128, CAP])
    etab_d = din("etab", [6, 128, ET_W], BF16)
    out_d = nc.dram_tensor("out", [S, D], F32, kind="ExternalOutput").ap()
    dbg = {}
    if debug:
        def dout(name, shape, dt=F32):
            dbg[name] = nc.dram_tensor(name, list(shape), dt, kind="ExternalOutput").ap()
        dout("d_xnT", [128, KC * S], BF16)
        dout("d_x_conv", [S, D])
        dout("d_convT", [128, 3 * S], BF16)
        dout("d_qT0", [128, S], BF16)
        dout("d_kT0", [128, S], BF16)
        dout("d_v", [128, NT * 6 * 65], BF16)
        dout("d_oT0", [64, 2 * S], BF16)
        dout("d_x_dil", [S, D])
        dout("d_osb", [65, 512])
        dout("d_psb", [128, 512], BF16)
        dout("d_ET", [128, 2 * ET_W], BF16)
        dout("d_esb", [128, 512], BF16)
        dout("d_S", [128, 512])
        dout("d_psb0", [128, 512], BF16)

    etab_nz = _etab().astype(np.float32) != 0
    PERS_BYTES = 114 * 1024 + 512
    PH_BYTES = 92 * 1024 - 512

    with contextlib.ExitStack() as st:
        pers_t = st.enter_context(nc.sbuf_tensor("pers", [128, PERS_BYTES // 4], F32))
        ph_t = st.enter_context(nc.sbuf_tensor("phase", [128, PH_BYTES // 4], F32))
        PS = [st.enter_context(nc.psum_tensor("ps%d" % i, [128, 512], F32)) for i in range(8)]
        PSB = [p[:].bitcast(BF16) for p in PS]

        pa = Arena(pers_t, PERS_BYTES)
        X = pa.take(NT * D * 4, F32, "p (i d) -> p i d", i=NT)
        A = pa.take(NT * D * 2, BF16)
        xnT = A.rearrange("p (k t) -> p k t", k=KC)
        xnTok = A.rearrange("p (i d) -> p i d", i=NT)
        ident_b = pa.take(128 * 2, BF16)
        ident_f = pa.take(128 * 4, F32)
        iota1 = pa.take(CAP * 4, F32)
        ones_f = pa.take(64 * 4, F32)
        memT = pa.take(KC * MEM * 2, BF16, "p (k t) -> p k t", k=KC)
        G = pa.take(D * 4, F32)
        junk = pa.take(D * 2, BF16)
        XN = [pa.take(D * 2, BF16), pa.take(D * 2, BF16)]
        ss = pa.take(NT * 4, F32)
        std = pa.take(NT * 4, F32)
        rstd = pa.take(NT * 4, F32)
        cw = pa.take(9 * 4, F32)
        aff_all = pa.take(NT * E * 4, F32, "p (i e) -> p i e", i=NT)
        sel_all = pa.take(NT * E * 4, F32, "p (i e) -> p i e", i=NT)
        sm = pa.take(16 * 4, F32)

        P = Prog()
        cnt = [0]

        def alt():
            cnt[0] += 1
            return "act" if cnt[0] % 2 == 0 else "dve"

        def copy_op(engname, out, in_):
            if engname == "act":
                return lambda e: e.copy(out, in_)
            return lambda e: e.tensor_copy(out, in_)

        def dump(name, src, reads, idx=None):
            if not debug or l != 0:
                return
            dst = dbg[name] if idx is None else dbg[name][idx]
            final_ops.append(P.add("sp", (lambda e: e.dma_start(out=dst, in_=src)), reads=reads, dma_chan="dbg", extra_deps=final_ops[-1:]))

        def dump_x(name):
            if not debug or l != 0:
                return
            for i in range(NT):
                final_ops.append(P.add("sp", (lambda e, i=i: e.dma_start(out=dbg[name][128 * i:128 * (i + 1), :], in_=X[:, i, :])),
                                       reads=[("X", i, 0), ("X", i, 1)], dma_chan="dbg", extra_deps=final_ops[-1:]))

        final_ops = []
        l = 0
        for i in range(NT):
            P.add("sp", (lambda e, i=i: e.dma_start(out=X[:, i, :], in_=x_d[128 * i:128 * (i + 1), :])),
                  writes=[("X", i, 0), ("X", i, 1)], dma_chan=("x", i))
        P.add("act", lambda e: e.dma_start(out=ident_b, in_=identb_d), writes=["ident_b"], dma_chan="c0")
        P.add("act", lambda e: e.dma_start(out=ident_f, in_=identf_d), writes=["ident_f"], dma_chan="c1")
        P.add("act", lambda e: e.dma_start(out=iota1, in_=iota1_d), writes=["iota1"], dma_chan="c2")
        P.add("dve", lambda e: e.memset(ones_f, 1.0), writes=["ones_f"])

        def load_gain(src_row):
            P.add("act", lambda e: e.dma_start(out=G, in_=src_row.partition_broadcast(128)), writes=["G"], dma_chan="g")

        def rms_stats(src_fn, keys_fn, n):
            for i in range(n):
                P.add("act", (lambda e, i=i: e.activation(junk, src_fn(i), AF.Square, accum_out=ss[:, i:i + 1])),
                      reads=keys_fn(i), writes=["junk", ("ss", i)])
            P.add("act", lambda e: e.activation(std[:, 0:n], ss[:, 0:n], AF.Sqrt, bias=eps_ap, scale=1.0 / D),
                  reads=[("ss", i) for i in range(n)] + ["eps"], writes=["std"])
            P.add("dve", lambda e: e.reciprocal(rstd[:, 0:n], std[:, 0:n]), reads=["std"], writes=["rstd"])

        eps_ap = pa.take(4, F32)
        P.add("dve", lambda e: e.memset(eps_ap, EPS), writes=["eps"])

        ph = Arena(ph_t, PH_BYTES)
        memx = ph.take(2 * D * 4, F32, "p (i d) -> p i d", i=2)
        for i in range(2):
            P.add("sp", (lambda e, i=i: e.dma_start(out=memx[:, i, :], in_=mem_d[128 * i:128 * (i + 1), :])),
                  writes=[("memx", i)], dma_chan=("mx", i))
        load_gain(mem_norm_d[0])
        rms_stats(lambda i: memx[:, i, :], lambda i: [("memx", i)], 2)
        for i in range(2):
            xb = XN[i % 2]
            P.add("dve", (lambda e, i=i, xb=xb: e.scalar_tensor_tensor(xb, memx[:, i, :], rstd[:, i:i + 1], G, ALU.mult, ALU.mult)),
                  reads=[("memx", i), "rstd", "G"], writes=[("XN", i % 2)])
            for c in range(KC):
                P.add("pe", (lambda e, c=c, xb=xb, i=i: e.transpose(PSB[i][:, 128 * c:128 * (c + 1)], xb[:, 128 * c:128 * (c + 1)], ident_b)),
                      reads=[("XN", i % 2), "ident_b"], writes=[("ps", i)])
            P.add("act", (lambda e, i=i: e.copy(memT[:, :, 128 * i:128 * (i + 1)], PSB[i].rearrange("p (k t) -> p k t", k=KC))),
                  reads=[("ps", i)], writes=[("memT", i)])
        P.barrier()

        for l in range(n_layers):
            ph = Arena(ph_t, PH_BYTES)
            WOC = ph.take(3 * D * 2, BF16, "p (c d) -> p c d", c=3)
            WI = [ph.take(KC * 384 * 2, BF16, "p (k c) -> p k c", k=KC) for _ in range(2)]
            mix_base = ph.off
            w_in_v = w_in_d[l].rearrange("(k p) c -> p k c", p=128)
            w_out_v = w_out_d[l].rearrange("(k p) d -> p k d", p=128)

            load_gain(norm_mix_d[l])
            P.add("pool", lambda e: e.dma_start(out=WOC, in_=w_out_v[:, 0:3, :]), writes=["WOC"], dma_chan="woc")
            P.add("act", lambda e: e.dma_start(out=cw, in_=convw_d[l]), writes=["cw"], dma_chan="cw")

            def load_wi(buf, ranges):
                off = 0
                for (c0, n) in ranges:
                    P.add("pool", (lambda e, c0=c0, n=n, off=off: e.dma_start(out=WI[buf][:, :, off:off + n], in_=w_in_v[:, :, c0:c0 + n])),
                          writes=[("WI", buf, u) for u in range(off // 128, (off + n) // 128)], dma_chan=("wi", buf, off // 128))
                    off += n

            rms_stats(lambda i: X[:, i, :], lambda i: [("X", i, 0), ("X", i, 1)], NT)
            for i in range(NT):
                xb = XN[i % 2]
                pb = i % 2
                P.add("dve", (lambda e, i=i, xb=xb: e.scalar_tensor_tensor(xb, X[:, i, :], rstd[:, i:i + 1], G, ALU.mult, ALU.mult)),
                      reads=[("X", i, 0), ("X", i, 1), "rstd", "G"], writes=[("XN", i % 2)])
                for c in range(KC):
                    P.add("pe", (lambda e, c=c, xb=xb, pb=pb: e.transpose(PSB[pb][:, 128 * c:128 * (c + 1)], xb[:, 128 * c:128 * (c + 1)], ident_b)),
                          reads=[("XN", i % 2), "ident_b"], writes=[("ps", pb)])
                P.add("act", (lambda e, i=i, pb=pb: e.copy(xnT[:, :, 128 * i:128 * (i + 1)], PSB[pb].rearrange("p (k t) -> p k t", k=KC))),
                      reads=[("ps", pb)], writes=[("xnT", i // 4)])

            dump("d_xnT", A, [("xnT", tb) for tb in range(4)])

            def proj_feat(wi_ap, wi_keys, dst_fn, dst_key_fn, bank0, nbank=2, evac_scale=None):
                for tb in range(4):
                    bk = bank0 + tb % nbank
                    for k in range(KC):
                        P.add("pe", (lambda e, k=k, tb=tb, bk=bk: e.matmul(PS[bk][:, :], wi_ap[:, k, :], xnT[:, k, 512 * tb:512 * (tb + 1)],
                                                                         start=(k == 0), stop=(k == KC - 1))),
                              reads=wi_keys + [("xnT", tb)], writes=[("ps", bk)])
                    en = alt()
                    P.add(en, copy_op(en, dst_fn(tb), PS[bk][:, :]), reads=[("ps", bk)], writes=[dst_key_fn(tb)])

            ph.off = mix_base
            u_pad = ph.take((S + 2) * 4 + 8, F32)
            tmpc = ph.take(S * 4, F32)
            bg_sb = ph.take(S * 4, F32)
            cg_sb = [ph.take(512 * 4, F32) for _ in range(2)]
            convT = ph.take(3 * S * 2, BF16, "p (c t) -> p c t", c=3)
            P.add("dve", lambda e: e.memset(u_pad[:, 0:1], 0.0), writes=["u_l"])
            P.add("dve", lambda e: e.memset(u_pad[:, S + 1:S + 2], 0.0), writes=["u_r"])
            for c in range(3):
                buf = c % 2
                load_wi(buf, [(128 * c, 128), (384 + 128 * c, 128), (768 + 128 * c, 128)])
                for tb in range(4):
                    for j in range(3):
                        bk = 2 + j
                        for k in range(KC):
                            P.add("pe", (lambda e, k=k, tb=tb, bk=bk, j=j, buf=buf: e.matmul(
                                PS[bk][:, :], WI[buf][:, k, 128 * j:128 * (j + 1)], xnT[:, k, 512 * tb:512 * (tb + 1)],
                                start=(k == 0), stop=(k == KC - 1))),
                                reads=[("WI", buf, j), ("xnT", tb)], writes=[("ps", bk)])
                    cb = cg_sb[tb % 2]
                    P.add("act", (lambda e, cb=cb: e.copy(cb, PS[4][:, :])), reads=[("ps", 4)], writes=[("cg", tb % 2)])
                    P.add("dve", (lambda e, cb=cb, tb=tb: e.tensor_tensor(u_pad[:, 1 + 512 * tb:1 + 512 * (tb + 1)], cb, PS[2][:, :], ALU.mult)),
                          reads=[("cg", tb % 2), ("ps", 2)], writes=[("u", tb)])
                    P.add("act", (lambda e, tb=tb: e.copy(bg_sb[:, 512 * tb:512 * (tb + 1)], PS[3][:, :])),
                          reads=[("ps", 3)], writes=[("bg", tb)])
                ukeys = [("u", tb) for tb in range(4)] + ["u_l", "u_r"]
                P.add("dve", (lambda e, c=c: e.tensor_scalar(tmpc, u_pad[:, 1:S + 1], cw[:, 3 * c + 1:3 * c + 2], None, ALU.mult)),
                      reads=ukeys + ["cw"], writes=["tmpc"])
                P.add("dve", (lambda e, c=c: e.scalar_tensor_tensor(tmpc, u_pad[:, 0:S], cw[:, 3 * c:3 * c + 1], tmpc, ALU.mult, ALU.add)),
                      reads=ukeys + ["cw", "tmpc"], writes=["tmpc"])
                P.add("dve", (lambda e, c=c: e.scalar_tensor_tensor(tmpc, u_pad[:, 2:S + 2], cw[:, 3 * c + 2:3 * c + 3], tmpc, ALU.mult, ALU.add)),
                      reads=ukeys + ["cw", "tmpc"], writes=["tmpc"])
                P.add("dve", (lambda e, c=c: e.tensor_tensor(convT[:, c, :], tmpc, bg_sb, ALU.mult)),
                      reads=["tmpc"] + [("bg", tb) for tb in range(4)], writes=[("convT", c)])
            for i in range(NT):
                for hf in range(2):
                    bk = 5 + (2 * i + hf) % 3
                    for c in range(3):
                        P.add("pe", (lambda e, i=i, hf=hf, c=c, bk=bk: e.matmul(
                            PS[bk][:, :], convT[:, c, 128 * i:128 * (i + 1)], WOC[:, c, 512 * hf:512 * (hf + 1)],
                            start=(c == 0), stop=(c == 2))),
                            reads=[("convT", c), "WOC"], writes=[("ps", bk)])
                    P.add("dve", (lambda e, i=i, hf=hf, bk=bk: e.tensor_tensor(
                        X[:, i, 512 * hf:512 * (hf + 1)], X[:, i, 512 * hf:512 * (hf + 1)], PS[bk][:, :], ALU.add)),
                        reads=[("ps", bk), ("X", i, hf)], writes=[("X", i, hf)])
            dump("d_convT", convT.rearrange("p c t -> p (c t)"), [("convT", c) for c in range(3)])
            dump_x("d_x_conv")
            P.barrier()

            ph.off = mix_base
            v_flat = ph.take(NT * 6 * 65 * 2 + 136, BF16)
            v_aug = v_flat[:, 0:NT * 6 * 65].rearrange("p (i h c) -> p i h c", i=NT, h=6)
            qz = [ph.take(S * 2, BF16) for _ in range(2)]
            kT = [ph.take(S * 2, BF16) for _ in range(2)]
            P.add("dve", lambda e: e.memset(qz[0][64:128, :], 0.0), writes=["qz0pad"])
            P.add("dve", lambda e: e.memset(qz[1][0:64, :], 0.0), writes=["qz1pad"])
            P.add("dve", lambda e: e.memset(v_flat[:, NT * 6 * 65:NT * 6 * 65 + 68], 0.0), writes=["v_pad"])
            ET = ph.take(2 * ET_W * 2, BF16, "p (h v) -> p h v", h=2)
            oT = ph.take(2 * S * 2, BF16, "p (h t) -> p h t", h=2)
            WOH = ph.take(2 * D * 2, BF16, "p (h d) -> p h d", h=2)
            e_sb = [ph.take(512 * 2, BF16) for _ in range(3)]
            p_sb = [ph.take(512 * 2, BF16) for _ in range(3)]
            o_sb = [ph.take(512 * 4, F32) for _ in range(2)]
            r_row = ph.take(512 * 4, F32)

            def attn_finish(par, h_local, qb, att_key):
                bo = 3 + par
                lnl = o_sb[0]
                bcs = o_sb[1]
                P.add("act", (lambda e: e.activation(lnl[64:65, :], PS[bo][64:65, :], AF.Ln)), reads=[("ps", bo)], writes=["lnl"])
                P.add("act", (lambda e: e.activation(r_row[64:65, :], lnl[64:65, :], AF.Exp, scale=-1.0)), reads=["lnl"], writes=["r_row"])
                P.add("pe", lambda e: e.matmul(PS[5][0:64, :], ones_f[64:65, 0:64], r_row[64:65, :], start=True, stop=True),
                      reads=["r_row", "ones_f"], writes=[("ps", 5)])
                P.add("dve", (lambda e: e.tensor_copy(bcs[0:64, :], PS[5][0:64, :])), reads=[("ps", 5)], writes=["bcs"])
                P.add("dve", (lambda e: e.tensor_tensor(oT[0:64, h_local, 512 * qb:512 * (qb + 1)], PS[bo][0:64, :], bcs[0:64, :], ALU.mult)),
                      reads=[("ps", bo), "bcs"], writes=[(att_key, h_local, qb)])

            def head_outproj(att_key, row0):
                P.add("pool", (lambda e, row0=row0: e.dma_start(out=WOH[0:64, :, :], in_=w_out_d[l][row0:row0 + 128, :].rearrange("(h p) d -> p h d", p=64))),
                      writes=["WOH"], dma_chan="woh")
                for i in range(NT):
                    for hf in range(2):
                        bk = 6 + (2 * i + hf) % 2
                        for hh in range(2):
                            P.add("pe", (lambda e, i=i, hf=hf, hh=hh, bk=bk: e.matmul(
                                PS[bk][:, :], oT[0:64, hh, 128 * i:128 * (i + 1)], WOH[0:64, hh, 512 * hf:512 * (hf + 1)],
                                start=(hh == 0), stop=(hh == 1))),
                                reads=[(att_key, hh, i // 4), "WOH"], writes=[("ps", bk)])
                        P.add("dve", (lambda e, i=i, hf=hf, bk=bk: e.tensor_tensor(
                            X[:, i, 512 * hf:512 * (hf + 1)], X[:, i, 512 * hf:512 * (hf + 1)], PS[bk][:, :], ALU.add)),
                            reads=[("ps", bk), ("X", i, hf)], writes=[("X", i, hf)])

            load_wi(0, [(1920, 384)])
            P.add("dve", lambda e: e.memset(v_aug[:, :, :, 64:65], 1.0), writes=["v_ones"])
            for i in range(NT):
                bk = i % 2
                for k in range(KC):
                    P.add("pe", (lambda e, i=i, k=k, bk=bk: e.matmul(PS[bk][:, 0:384], xnT[:, k, 128 * i:128 * (i + 1)], WI[0][:, k, 0:384],
                                                                     start=(k == 0), stop=(k == KC - 1))),
                          reads=[("WI", 0, 0), ("WI", 0, 1), ("WI", 0, 2), ("xnT", i // 4)], writes=[("ps", bk)])
                en = alt()
                P.add(en, copy_op(en, v_aug[:, i, :, 0:64], PS[bk][:, 0:384].rearrange("p (h c) -> p h c", h=6)),
                      reads=[("ps", bk)], writes=[("v", i)])
            vkeys = ["v_ones"]
            dump("d_v", v_aug.rearrange("p i h c -> p (i h c)"), [("v", i) for i in range(NT)] + vkeys)

            for hp in range(3):
                buf = 1 - (hp % 2) if hp > 0 else 1
                buf = (hp + 1) % 2
                load_wi(buf, [(1152 + 128 * hp, 128), (1536 + 128 * hp, 128)])
                P.add("sp", (lambda e, hp=hp: e.dma_start(out=ET, in_=etab_d[2 * hp:2 * hp + 2].rearrange("h p v -> p h v"))),
                      writes=["ET"], dma_chan="et")
                kb_ = kT[hp % 2]
                for tb in range(4):
                    bk = 6 + tb % 2
                    for k in range(KC):
                        P.add("pe", (lambda e, k=k, tb=tb, bk=bk: e.matmul(PS[bk][:, :], WI[buf][:, k, 0:128], xnT[:, k, 512 * tb:512 * (tb + 1)],
                                                                         start=(k == 0), stop=(k == KC - 1))),
                              reads=[("WI", buf, 0), ("xnT", tb)], writes=[("ps", bk)])
                    P.add("act", (lambda e, tb=tb, bk=bk: e.copy(qz[0][0:64, 512 * tb:512 * (tb + 1)], PS[bk][0:64, :])),
                          reads=[("ps", bk), "qz0pad"], writes=[("qz", 0, tb)])
                    P.add("dve", (lambda e, tb=tb, bk=bk: e.tensor_copy(qz[1][64:128, 512 * tb:512 * (tb + 1)], PS[bk][64:128, :])),
                          reads=[("ps", bk), "qz1pad"], writes=[("qz", 1, tb)])
                proj_feat(WI[buf][:, :, 128:256], [("WI", buf, 1)], (lambda tb, kb_=kb_: kb_[:, 512 * tb:512 * (tb + 1)]),
                          (lambda tb, hp=hp: ("kT", hp % 2, tb)), 6)
                gi = 0
                for hh in range(2):
                    h = 2 * hp + hh
                    for qb in range(4):
                        kts = []
                        for kt in range(max(0, 4 * qb - 8), min(15, 4 * qb + 11) + 1):
                            v0 = ET_OFF - (128 * kt - 512 * qb)
                            if etab_nz[h][:, v0:v0 + 512].any():
                                kts.append(kt)
                        par = gi % 2
                        gi += 1
                        bo = 3 + par

                        def s_mm(kt, n, hh=hh, qb=qb, kb_=kb_, hp=hp):
                            bs = n % 3
                            P.add("pe", (lambda e: e.matmul(PS[bs][:, :], kb_[:, 128 * kt:128 * (kt + 1)],
                                                            qz[hh][:, 512 * qb:512 * (qb + 1)], start=True, stop=True)),
                                  reads=[("kT", hp % 2, kt // 4), ("qz", hh, qb), "qz%dpad" % hh], writes=[("ps", bs)])

                        s_mm(kts[0], 0)
                        if len(kts) > 1:
                            s_mm(kts[1], 1)
                        for n, kt in enumerate(kts):
                            bs = n % 3
                            v0 = ET_OFF - (128 * kt - 512 * qb)
                            P.add("act", (lambda e, bs=bs: e.activation(e_sb[bs], PS[bs][:, :], AF.Exp, scale=0.125)),
                                  reads=[("ps", bs)], writes=[("e_sb", bs)])
                            P.add("dve", (lambda e, bs=bs, hh=hh, v0=v0: e.tensor_tensor(p_sb[bs], e_sb[bs], ET[:, hh, v0:v0 + 512], ALU.mult)),
                                  reads=[("e_sb", bs), "ET"], writes=[("p_sb", bs)])
                            if n + 2 < len(kts):
                                s_mm(kts[n + 2], n + 2)
                            voff = (kt * 6 + h) * 65
                            P.add("pe", (lambda e, bs=bs, voff=voff, bo=bo, n=n, last=(n == len(kts) - 1): e.matmul(
                                PS[bo][:, :], v_flat[:, voff:voff + 128], p_sb[bs], start=(n == 0), stop=last)),
                                reads=[("p_sb", bs), "v_pad"] + [("v", i) for i in range(kt, min(NT, kt + 2))] + vkeys, writes=[("ps", bo)])
                        attn_finish(par, hh, qb, "oT")
                head_outproj("oT", 384 + 128 * hp)
            dump_x("d_x_dil")
            P.barrier()

            ph.off = mix_base
            WMKV = ph.take(KC * 512 * 2, BF16, "p (k c) -> p k c", k=KC)
            kmT = ph.take(2 * MEM * 2, BF16, "p (h t) -> p h t", h=2)
            vm_flat = ph.take(2 * 4 * 65 * 2 + 136, BF16)
            vm_aug = vm_flat[:, 0:2 * 4 * 65].rearrange("p (i h c) -> p i h c", i=2, h=4)
            qmz = [ph.take(S * 2, BF16) for _ in range(2)]
            oT = ph.take(2 * S * 2, BF16, "p (h t) -> p h t", h=2)
            WOH = ph.take(2 * D * 2, BF16, "p (h d) -> p h d", h=2)
            p_sb = [ph.take(512 * 2, BF16) for _ in range(3)]
            o_sb = [ph.take(512 * 4, F32) for _ in range(2)]
            r_row = ph.take(512 * 4, F32)
            P.add("dve", lambda e: e.memset(qmz[0][64:128, :], 0.0), writes=["qmz0pad"])
            P.add("dve", lambda e: e.memset(qmz[1][0:64, :], 0.0), writes=["qmz1pad"])
            P.add("dve", lambda e: e.memset(vm_flat[:, 2 * 4 * 65:2 * 4 * 65 + 68], 0.0), writes=["vm_pad"])
            P.add("pool", lambda e: e.dma_start(out=WMKV, in_=w_mkv_d[l].rearrange("(k p) c -> p k c", p=128)), writes=["WMKV"], dma_chan="wmkv")
            P.add("dve", lambda e: e.memset(vm_aug[:, :, :, 64:65], 1.0), writes=["vm_ones"])
            for mp in range(2):
                for k in range(KC):
                    P.add("pe", (lambda e, mp=mp, k=k: e.matmul(PS[0][:, 0:MEM], WMKV[:, k, 128 * mp:128 * (mp + 1)], memT[:, k, :],
                                                                start=(k == 0), stop=(k == KC - 1))),
                          reads=["WMKV", ("memT", 0), ("memT", 1)], writes=[("ps", 0)])
                P.add("act", (lambda e, mp=mp: e.copy(kmT[:, mp, :], PS[0][:, 0:MEM])), reads=[("ps", 0)], writes=[("kmT", mp)])
            for i in range(2):
                for k in range(KC):
                    P.add("pe", (lambda e, i=i, k=k: e.matmul(PS[1][:, 0:256], memT[:, k, 128 * i:128 * (i + 1)], WMKV[:, k, 256:512],
                                                              start=(k == 0), stop=(k == KC - 1))),
                          reads=["WMKV", ("memT", 0), ("memT", 1)], writes=[("ps", 1)])
                P.add("dve", (lambda e, i=i: e.tensor_copy(vm_aug[:, i, :, 0:64], PS[1][:, 0:256].rearrange("p (h c) -> p h c", h=4))),
                      reads=[("ps", 1)], writes=[("vm", i)])
            for mp in range(2):
                buf = mp % 2
                load_wi(buf, [(2304 + 128 * mp, 128)])
                for tb in range(4):
                    bk = 6 + tb % 2
                    for k in range(KC):
                        P.add("pe", (lambda e, k=k, tb=tb, bk=bk: e.matmul(PS[bk][:, :], WI[buf][:, k, 0:128], xnT[:, k, 512 * tb:512 * (tb + 1)],
                                                                         start=(k == 0), stop=(k == KC - 1))),
                              reads=[("WI", buf, 0), ("xnT", tb)], writes=[("ps", bk)])
                    P.add("act", (lambda e, tb=tb, bk=bk: e.copy(qmz[0][0:64, 512 * tb:512 * (tb + 1)], PS[bk][0:64, :])),
                          reads=[("ps", bk), "qmz0pad"], writes=[("qmz", 0, tb)])
                    P.add("dve", (lambda e, tb=tb, bk=bk: e.tensor_copy(qmz[1][64:128, 512 * tb:512 * (tb + 1)], PS[bk][64:128, :])),
                          reads=[("ps", bk), "qmz1pad"], writes=[("qmz", 1, tb)])
                gi = 0
                for hh in range(2):
                    hm = 2 * mp + hh
                    for qb in range(4):
                        par = gi % 2
                        gi += 1
                        bo = 3 + par
                        for kt in range(2):
                            bs = kt
                            P.add("pe", (lambda e, bs=bs, kt=kt, mp=mp, qb=qb, hh=hh: e.matmul(
                                PS[bs][:, :], kmT[:, mp, 128 * kt:128 * (kt + 1)], qmz[hh][:, 512 * qb:512 * (qb + 1)],
                                start=True, stop=True)),
                                reads=[("kmT", mp), ("qmz", hh, qb), "qmz%dpad" % hh], writes=[("ps", bs)])
                        for kt in range(2):
                            bs = kt
                            P.add("act", (lambda e, bs=bs: e.activation(p_sb[bs], PS[bs][:, :], AF.Exp, scale=0.125)),
                                  reads=[("ps", bs)], writes=[("p_sb", bs)])
                            voff = (kt * 4 + hm) * 65
                            P.add("pe", (lambda e, bs=bs, voff=voff, bo=bo, kt=kt: e.matmul(
                                PS[bo][:, :], vm_flat[:, voff:voff + 128], p_sb[bs], start=(kt == 0), stop=(kt == 1))),
                                reads=[("p_sb", bs), ("vm", 0), ("vm", 1), "vm_ones", "vm_pad"], writes=[("ps", bo)])
                        attn_finish(par, hh, qb, "oTm")
                head_outproj("oTm", 768 + 128 * mp)
            P.barrier()

            if not do_moe:
                continue
            ph = Arena(ph_t, PH_BYTES)
            WG = [ph.take(KC * 512 * 2, BF16, "p (k f) -> p k f", k=KC) for _ in range(2)]
            WU = [ph.take(KC * 512 * 2, BF16, "p (k f) -> p k f", k=KC) for _ in range(2)]
            WD = [ph.take(4 * D * 2, BF16, "p (c d) -> p c d", c=4) for _ in range(2)]
            WR = ph.take(KC * E * 4, F32, "p (k e) -> p k e", k=KC)
            moe_base = ph.off

            def load_expert_w(e_, fs):
                buf = (e_ * 4 + fs) % 2
                P.add("pool", (lambda e, e_=e_, fs=fs, buf=buf: e.dma_start(
                    out=WG[buf], in_=w_gate_d[l, e_].rearrange("(k p) f -> p k f", p=128)[:, :, 512 * fs:512 * (fs + 1)])),
                    writes=[("WG", buf)], dma_chan=("wg", buf))
                P.add("pool", (lambda e, e_=e_, fs=fs, buf=buf: e.dma_start(
                    out=WU[buf], in_=w_up_d[l, e_].rearrange("(k p) f -> p k f", p=128)[:, :, 512 * fs:512 * (fs + 1)])),
                    writes=[("WU", buf)], dma_chan=("wu", buf))
                P.add("pool", (lambda e, e_=e_, fs=fs, buf=buf: e.dma_start(
                    out=WD[buf], in_=w_down_d[l, e_][512 * fs:512 * (fs + 1), :].rearrange("(c p) d -> p c d", p=128))),
                    writes=[("WD", buf)], dma_chan=("wd", buf))

            load_gain(norm_ffn_d[l])
            P.add("act", lambda e: e.dma_start(out=WR, in_=w_router_d[l].rearrange("(k p) e -> p k e", p=128)), writes=["WR"], dma_chan="wr")
            load_expert_w(0, 0)
            load_expert_w(0, 1)

            xnf = ph.take(D * 4, F32)
            xnTf = ph.take(KC * 128 * 4, F32, "p (k t) -> p k t", k=KC)
            affT = ph.take(S * 4, F32)
            wk = [ph.take(S * 4, F32) for _ in range(2)]
            maskT = ph.take(S * 4, F32)
            m8 = ph.take(8 * 4, F32)
            rms_stats(lambda i: X[:, i, :], lambda i: [("X", i, 0), ("X", i, 1)], NT)
            for i in range(NT):
                P.add("dve", (lambda e, i=i: e.scalar_tensor_tensor(xnf, X[:, i, :], rstd[:, i:i + 1], G, ALU.mult, ALU.mult)),
                      reads=[("X", i, 0), ("X", i, 1), "rstd", "G"], writes=["xnf"])
                P.add("act", (lambda e, i=i: e.copy(xnTok[:, i, :], xnf)), reads=["xnf"], writes=[("xnTok", i)])
                for c in range(KC):
                    bk = c // 4
                    P.add("pe", (lambda e, c=c, bk=bk: e.transpose(PS[bk][:, 128 * (c % 4):128 * (c % 4 + 1)], xnf[:, 128 * c:128 * (c + 1)], ident_f)),
                          reads=["xnf", "ident_f"], writes=[("ps", bk)])
                P.add("act", lambda e: e.copy(xnTf[:, 0:4, :], PS[0][:, :].rearrange("p (k t) -> p k t", k=4)), reads=[("ps", 0)], writes=[("xnTf", 0)])
                P.add("dve", lambda e: e.tensor_copy(xnTf[:, 4:8, :], PS[1][:, :].rearrange("p (k t) -> p k t", k=4)), reads=[("ps", 1)], writes=[("xnTf", 1)])
                for k in range(KC):
                    P.add("pe", (lambda e, k=k: e.matmul(PS[2][:, 0:E], xnTf[:, k, :], WR[:, k, :], start=(k == 0), stop=(k == KC - 1))),
                          reads=[("xnTf", k // 4), "WR"], writes=[("ps", 2)])
                P.add("dve", lambda e: e.reduce_max(sm[:, 0:1], PS[2][:, 0:E], axis=AX.X), reads=[("ps", 2)], writes=["sm0"])
                P.add("dve", lambda e: e.tensor_scalar(sm[:, 1:2], sm[:, 0:1], -1.0, None, ALU.mult), reads=["sm0"], writes=["sm1"])
                P.add("act", (lambda e, i=i: e.activation(aff_all[:, i, :], PS[2][:, 0:E], AF.Exp, bias=sm[:, 1:2], scale=1.0, accum_out=sm[:, 2:3])),
                      reads=[("ps", 2), "sm1"], writes=[("aff", i), "sm2"])
                P.add("dve", lambda e: e.reciprocal(sm[:, 3:4], sm[:, 2:3]), reads=["sm2"], writes=["sm3"])
                P.add("dve", (lambda e, i=i: e.tensor_scalar(aff_all[:, i, :], aff_all[:, i, :], sm[:, 3:4], None, ALU.mult)),
                      reads=[("aff", i), "sm3"], writes=[("aff", i)])
            for i in range(NT):
                bk = 3 + i // 4
                P.add("pe", (lambda e, i=i, bk=bk: e.transpose(PS[bk][0:E, 128 * (i % 4):128 * (i % 4 + 1)], aff_all[:, i, :], ident_f)),
                      reads=[("aff", i), "ident_f"], writes=[("ps", bk)])
            for j in range(4):
                P.add("act", (lambda e, j=j: e.copy(affT[0:E, 512 * j:512 * (j + 1)], PS[3 + j][0:E, :])), reads=[("ps", 3 + j)], writes=[("affT", j)])
                P.add("dve", (lambda e, j=j: e.tensor_copy(wk[0][0:E, 512 * j:512 * (j + 1)], PS[3 + j][0:E, :])), reads=[("ps", 3 + j)], writes=["wk0"])
            nit = CAP // 8
            for it in range(nit):
                src = wk[it % 2]
                dst = wk[(it + 1) % 2]
                P.add("dve", (lambda e, src=src: e.max(m8[0:E, :], src[0:E, :])), reads=["wk%d" % (it % 2)], writes=["m8"])
                if it < nit - 1:
                    P.add("dve", (lambda e, src=src, dst=dst: e.match_replace(dst[0:E, :], m8[0:E, :], src[0:E, :], -1e30)),
                          reads=["m8", "wk%d" % (it % 2)], writes=["wk%d" % ((it + 1) % 2)])
            affT_keys = [("affT", j) for j in range(4)]
            P.add("dve", lambda e: e.tensor_scalar(maskT[0:E, :], affT[0:E, :], m8[0:E, 7:8], None, ALU.is_ge), reads=affT_keys + ["m8"], writes=["maskT"])
            P.add("dve", lambda e: e.tensor_tensor_scan(wk[0][0:E, :], maskT[0:E, :], maskT[0:E, :], 0.0, ALU.add, ALU.max),
                  reads=["maskT", "wk0"], writes=["wk0"])
            P.add("dve", lambda e: e.tensor_tensor(wk[1][0:E, :], wk[0][0:E, :], maskT[0:E, :], ALU.mult), reads=["wk0", "maskT", "wk1"], writes=["wk1"])
            for i in range(NT):
                P.add("pe", (lambda e, i=i: e.transpose(PS[7][:, E * i:E * (i + 1)], wk[1][0:E, 128 * i:128 * (i + 1)], ident_f[0:E, 0:E])),
                      reads=["wk1", "ident_f"], writes=[("ps", 7)])
            P.add("act", lambda e: e.copy(sel_all, PS[7][:, 0:NT * E].rearrange("p (i e) -> p i e", i=NT)), reads=[("ps", 7)], writes=["sel"])
            P.barrier()

            ph.off = moe_base
            Ssel = ph.take(NT * CAP * 2, BF16, "p (i j) -> p i j", i=NT)
            Sg = ph.take(8 * CAP * 2, BF16, "p (i j) -> p i j", i=8)
            SgT = [ph.take(2 * S * 2, BF16, "p (j t) -> p j t", j=2) for _ in range(2)]
            xgT = ph.take(KC * CAP * 2, BF16, "p (k j) -> p k j", k=KC)
            hT = [ph.take(4 * CAP * 2, BF16, "p (c j) -> p c j", c=4) for _ in range(2)]
            sg_sb = [ph.take(CAP * 4, F32) for _ in range(2)]
            y_sb = ph.take(2 * D * 2, BF16, "p (j d) -> p j d", j=2)
            xg_keys = [("xgT", c) for c in range(KC)]

            def scatter_unit(es, u):
                i, hf = u // 2, u % 2
                bk = 6 + u % 2
                for jc in range(2):
                    P.add("pe", (lambda e, jc=jc: e.matmul(
                        PS[bk][:, :], SgT[es % 2][:, jc, 128 * i:128 * (i + 1)], y_sb[:, jc, 512 * hf:512 * (hf + 1)], start=(jc == 0), stop=(jc == 1))),
                        reads=[("SgT", es % 2, jc, i // 8), ("y", jc, hf)], writes=[("ps", bk)])
                P.add("dve", (lambda e: e.tensor_tensor(
                    X[:, i, 512 * hf:512 * (hf + 1)], X[:, i, 512 * hf:512 * (hf + 1)], PS[bk][:, :], ALU.add)),
                    reads=[("ps", bk), ("X", i, hf)], writes=[("X", i, hf)])

            for e_ in range(E):
                for half in range(2):
                    for ii in range(8):
                        i = 8 * half + ii
                        P.add("dve", (lambda e, i=i: e.tensor_scalar(Ssel[:, i, :], iota1, sel_all[:, i, e_:e_ + 1], None, ALU.is_equal)),
                              reads=["iota1", "sel"], writes=[("Ssel", i)])
                        P.add("dve", (lambda e, i=i, ii=ii: e.tensor_scalar(Sg[:, ii, :], iota1, sel_all[:, i, e_:e_ + 1], aff_all[:, i, e_:e_ + 1],
                                                                            ALU.is_equal, ALU.mult)),
                              reads=["iota1", "sel", ("aff", i)], writes=[("Sg", ii)])
                    for jc in range(2):
                        bk = 6 + jc
                        for ii in range(8):
                            P.add("pe", (lambda e, ii=ii, jc=jc, bk=bk: e.transpose(
                                PSB[bk][:, 128 * ii:128 * (ii + 1)], Sg[:, ii, 128 * jc:128 * (jc + 1)], ident_b)),
                                reads=[("Sg", ii), "ident_b"], writes=[("ps", bk)])
                        en = alt()
                        P.add(en, copy_op(en, SgT[e_ % 2][:, jc, 1024 * half:1024 * (half + 1)], PSB[bk]), reads=[("ps", bk)],
                              writes=[("SgT", e_ % 2, jc, half)])
                for c in range(KC):
                    bk = 6 + c % 2
                    for i in range(NT):
                        P.add("pe", (lambda e, c=c, i=i, bk=bk: e.matmul(PS[bk][:, 0:CAP], xnTok[:, i, 128 * c:128 * (c + 1)], Ssel[:, i, :],
                                                                         start=(i == 0), stop=(i == NT - 1))),
                              reads=[("xnTok", i), ("Ssel", i)], writes=[("ps", bk)])
                    en = alt()
                    P.add(en, copy_op(en, xgT[:, c, :], PS[bk][:, 0:CAP]), reads=[("ps", bk)], writes=[("xgT", c)])

                def gate_up(f):
                    fs, fc = f // 4, f % 4
                    buf = (e_ * 4 + fs) % 2
                    bk = 4 + f % 2
                    for k in range(KC):
                        P.add("pe", (lambda e, k=k: e.matmul(PS[bk][:, 0:CAP], WG[buf][:, k, 128 * fc:128 * (fc + 1)], xgT[:, k, :],
                                                             start=(k == 0), stop=(k == KC - 1))),
                              reads=[("WG", buf)] + xg_keys, writes=[("ps", bk)])
                    for k in range(KC):
                        P.add("pe", (lambda e, k=k: e.matmul(PS[bk][:, CAP:2 * CAP], WU[buf][:, k, 128 * fc:128 * (fc + 1)], xgT[:, k, :],
                                                             start=(k == 0), stop=(k == KC - 1))),
                              reads=[("WU", buf)] + xg_keys, writes=[("ps", bk)])

                gate_up(0)
                for f in range(16):
                    fs, fc = f // 4, f % 4
                    buf = (e_ * 4 + fs) % 2
                    bk = 4 + f % 2
                    hb = hT[fs % 2]
                    sgb = sg_sb[f % 2]
                    P.add("act", (lambda e: e.activation(sgb, PS[bk][:, 0:CAP], AF.Silu)), reads=[("ps", bk)], writes=[("sg", f % 2)])
                    P.add("dve", (lambda e: e.tensor_tensor(hb[:, fc, :], sgb, PS[bk][:, CAP:2 * CAP], ALU.mult)),
                          reads=[("sg", f % 2), ("ps", bk)], writes=[("hT", fs % 2, fc)])
                    if f + 1 < 16:
                        gate_up(f + 1)
                    for jc in range(2):
                        for hf in range(2):
                            by = 2 * jc + hf
                            P.add("pe", (lambda e, jc=jc, hf=hf, by=by: e.matmul(
                                PS[by][:, :], hb[:, fc, 128 * jc:128 * (jc + 1)], WD[buf][:, fc, 512 * hf:512 * (hf + 1)],
                                start=(f == 0), stop=(f == 15))),
                                reads=[("hT", fs % 2, fc), ("WD", buf)], writes=[("ps", by)])
                    if e_ > 0:
                        scatter_unit(e_ - 1, 2 * f)
                        scatter_unit(e_ - 1, 2 * f + 1)
                    if fc == 3:
                        nxt = e_ * 4 + fs + 2
                        if nxt < E * 4:
                            load_expert_w(nxt // 4, nxt % 4)
                for jc in range(2):
                    for hf in range(2):
                        by = 2 * jc + hf
                        en = alt()
                        P.add(en, copy_op(en, y_sb[:, jc, 512 * hf:512 * (hf + 1)], PS[by][:, :]), reads=[("ps", by)], writes=[("y", jc, hf)])
            for u in range(32):
                scatter_unit(E - 1, u)
            P.barrier()

        ph = Arena(ph_t, PH_BYTES)
        ob = [ph.take(D * 4, F32) for _ in range(4)]
        if debug:
            for i in range(NT):
                final_ops.append(P.add("sp", (lambda e, i=i: e.dma_start(out=out_d[128 * i:128 * (i + 1), :], in_=X[:, i, :])),
                                       reads=[("X", i, 0), ("X", i, 1)], dma_chan="dbg", extra_deps=final_ops[-1:]))
        else:
            load_gain(norm_final_d[0])
            rms_stats(lambda i: X[:, i, :], lambda i: [("X", i, 0), ("X", i, 1)], NT)
            for i in range(NT):
                P.add("dve", (lambda e, i=i: e.scalar_tensor_tensor(ob[i % 4], X[:, i, :], rstd[:, i:i + 1], G, ALU.mult, ALU.mult)),
                      reads=[("X", i, 0), ("X", i, 1), "rstd", "G"], writes=[("ob", i % 4)])
                final_ops.append(P.add("sp", (lambda e, i=i: e.dma_start(out=out_d[128 * i:128 * (i + 1), :], in_=ob[i % 4])),
                                       reads=[("ob", i % 4)], writes=[("obd", i % 4)], dma_chan=("o", i % 4)))
        P.emit(nc, final_waits=[("sp", o) for o in final_ops])
    return nc


def _etab():
    t = np.zeros((6, 128, ET_W), np.float64)
    p = np.arange(128)[:, None]
    v = np.arange(ET_W)[None, :]
    d = p - v + ET_OFF
    ad = np.abs(d)
    m = (ad <= 64).astype(np.float64) + ((d % 4 == 0) & (ad <= 256)) + ((d % 16 == 0) & (ad <= 1024))
    for h in range(6):
        slope = 2.0 ** (-8.0 * (h + 1) / 6)
        t[h] = m * np.exp(-slope * ad)
    return t.astype(np.float32).astype(ml_dtypes.bfloat16)


def _consts():
    return {
        "ident_bf": np.eye(128, dtype=np.float32).astype(ml_dtypes.bfloat16),
        "ident_f": np.eye(128, dtype=np.float32),
        "iota1": np.tile(np.arange(1, CAP + 1, dtype=np.float32)[None, :], (128, 1)),
        "etab": _etab(),
    }


def make_in_maps(x, mem, mem_norm, norm_mix, w_in, conv_w, w_mem_kv, w_out, norm_ffn,
                 w_router, w_gate, w_up, w_down, norm_final):
    f = lambda a: np.ascontiguousarray(np.asarray(a, dtype=np.float32))
    conv_w = f(conv_w)
    convw_t = np.ascontiguousarray(conv_w.reshape(L, 3, 3, 128).transpose(0, 3, 2, 1).reshape(L, 128, 9))
    shared = {
        "mem_norm": f(mem_norm).reshape(1, D), "norm_mix": f(norm_mix), "norm_ffn": f(norm_ffn),
        "norm_final": f(norm_final).reshape(1, D), "w_in": f(w_in), "convw_t": convw_t,
        "w_mem_kv": f(w_mem_kv), "w_out": f(w_out), "w_router": f(w_router),
        "w_gate": f(w_gate), "w_up": f(w_up), "w_down": f(w_down),
    }
    shared.update(_consts())
    x = f(x)
    mem = f(mem)
    return [dict(shared, x=x[b], mem=mem[b]) for b in range(N_CORES)]


def kernel(x, mem, mem_norm, norm_mix, w_in, conv_w, w_mem_kv, w_out, norm_ffn,
           w_router, w_gate, w_up, w_down, norm_final):
    in_maps = make_in_maps(x, mem, mem_norm, norm_mix, w_in, conv_w, w_mem_kv, w_out, norm_ffn,
                           w_router, w_gate, w_up, w_down, norm_final)
    nc = build_program()
    res = run_bass_kernel_spmd(nc, in_maps, core_ids=list(range(N_CORES)))
    return np.stack([np.asarray(r["out"], dtype=np.float32) for r in res.results], axis=0)
```

```python
import contextlib
import numpy as np
import ml_dtypes
import concourse.bass as bass
import concourse.mybir as mybir
from concourse.bass_utils import run_bass_kernel_spmd

F32 = mybir.dt.float32
BF16 = mybir.dt.bfloat16
AF = mybir.ActivationFunctionType
ALU = mybir.AluOpType
AX = mybir.AxisListType

D = 1024
S = 2048
NT = 16
KC = 8
L = 2
E = 16
FF = 2048
CAP = 256
MEM = 256
EPS = 1e-6
ET_W = 2944
ET_OFF = 1408
N_CORES = 8


class _Rec:
    def __init__(self):
        self.call = None

    def __getattr__(self, name):
        def f(*a, **k):
            self.call = (name, a, k)
            return self
        return f


class Prog:
    def __init__(self):
        self.ops = []
        self.last_writer = {}
        self.readers = {}
        self.chan_count = {}
        self.last_eng = {}
        self.last_chan = {}

    def add(self, eng, fn, reads=(), writes=(), dma_chan=None, extra_deps=()):
        idx = len(self.ops)
        deps = set(extra_deps)
        for k in reads:
            w = self.last_writer.get(k)
            if w is not None:
                deps.add(w)
        for k in writes:
            w = self.last_writer.get(k)
            if w is not None:
                deps.add(w)
            for r in self.readers.get(k, {}).values():
                deps.add(r)
        deps.discard(idx)
        rkey = eng if dma_chan is None else ("dma", idx)
        for k in reads:
            self.readers.setdefault(k, {})[rkey] = idx
        for k in writes:
            self.last_writer[k] = idx
            self.readers[k] = {}
        if fn is not None:
            rec = _Rec()
            fn(rec)
            call = rec.call
            assert call is not None
            fn = (lambda e, call=call: getattr(e, call[0])(*call[1], **call[2]))
        op = dict(eng=eng, fn=fn, deps=deps, chan=dma_chan, signal=False)
        if dma_chan is not None:
            self.chan_count[dma_chan] = self.chan_count.get(dma_chan, 0) + 16
            op["dmaval"] = self.chan_count[dma_chan]
            self.last_chan[dma_chan] = idx
        elif fn is not None:
            self.last_eng[eng] = idx
        self.ops.append(op)
        return idx

    def barrier(self):
        deps = set(self.last_eng.values()) | set(self.last_chan.values())
        for e in ["pe", "act", "dve", "pool", "sp"]:
            self.add(e, None, extra_deps=deps)

    def emit(self, nc, final_waits=()):
        ops = self.ops
        for op in ops:
            for d in op["deps"]:
                p = ops[d]
                if p["chan"] is None:
                    if p["eng"] == "pe" and op["eng"] == "pe" and op["chan"] is None and op["fn"] is not None:
                        continue
                    p["signal"] = True
        for (_, fo) in final_waits:
            if ops[fo]["chan"] is None:
                ops[fo]["signal"] = True
        engs = ["pe", "act", "dve", "pool", "sp"]
        seq = {e: 0 for e in engs}
        for op in ops:
            if op["chan"] is None and op["signal"]:
                seq[op["eng"]] += 1
                op["seqval"] = seq[op["eng"]]
        chans = sorted(self.chan_count.keys(), key=str)
        with contextlib.ExitStack() as st:
            esem = {e: st.enter_context(nc.semaphore("s_" + e)) for e in engs}
            csem = {c: st.enter_context(nc.semaphore("c_%d" % i)) for i, c in enumerate(chans)}
            block = st.enter_context(nc.Block())

            def run_engine(ename):
                def body(eng):
                    waited = {}
                    for op in ops:
                        if op["eng"] != ename:
                            continue
                        for d in sorted(op["deps"]):
                            p = ops[d]
                            if p["chan"] is not None:
                                key = ("c", p["chan"]); val = p["dmaval"]; sem = csem[p["chan"]]
                            else:
                                if p["eng"] == "pe" and ename == "pe" and op["chan"] is None and op["fn"] is not None:
                                    continue
                                key = ("e", p["eng"]); val = p["seqval"]; sem = esem[p["eng"]]
                            if waited.get(key, 0) >= val:
                                continue
                            eng.wait_ge(sem, val)
                            waited[key] = val
                        if op["fn"] is None:
                            continue
                        ins = op["fn"](eng)
                        if op["chan"] is not None:
                            ins.then_inc(csem[op["chan"]], 16)
                        elif op["signal"]:
                            ins.then_inc(esem[ename], 1)
                    for (e2, fo) in final_waits:
                        if e2 != ename:
                            continue
                        p = ops[fo]
                        if p["chan"] is not None:
                            eng.wait_ge(csem[p["chan"]], p["dmaval"])
                        else:
                            eng.wait_ge(esem[p["eng"]], p["seqval"])
                return body

            block.tensor(run_engine("pe"))
            block.scalar(run_engine("act"))
            block.vector(run_engine("dve"))
            block.gpsimd(run_engine("pool"))
            block.sync(run_engine("sp"))


class Arena:
    def __init__(self, t, nbytes):
        self.t = t
        self.n = nbytes
        self.off = 0
        self.mark = 0

    def take(self, nbytes, dt=F32, pattern=None, **kw):
        off = self.off
        self.off += (nbytes + 63) // 64 * 64
        assert self.off <= self.n, ("arena overflow", self.off, self.n)
        ap = self.t[:, off // 4:(off + nbytes) // 4]
        if dt != F32:
            ap = ap.bitcast(dt)
        if pattern:
            ap = ap.rearrange(pattern, **kw)
        return ap


def build_program(n_layers=L, do_moe=True, debug=False, skip_mem=False, skip_dil=False):
    nc = bass.Bass("TRN2", target_bir_lowering=False)

    def din(name, shape, dt=F32):
        return nc.dram_tensor(name, list(shape), dt, kind="ExternalInput").ap()

    x_d = din("x", [S, D])
    mem_d = din("mem", [MEM, D])
    mem_norm_d = din("mem_norm", [1, D])
    norm_mix_d = din("norm_mix", [L, D])
    norm_ffn_d = din("norm_ffn", [L, D])
    norm_final_d = din("norm_final", [1, D])
    w_in_d = din("w_in", [L, D, 2560])
    convw_d = din("convw_t", [L, 128, 9])
    w_mkv_d = din("w_mem_kv", [L, D, 512])
    w_out_d = din("w_out", [L, D, D])
    if do_moe:
        w_router_d = din("w_router", [L, D, E])
        w_gate_d = din("w_gate", [L, E, D, FF])
        w_up_d = din("w_up", [L, E, D, FF])
        w_down_d = din("w_down", [L, E, FF, D])
    identb_d = din("ident_bf", [128, 128], BF16)
    identf_d = din("ident_f", [128, 128])
    iota1_d = din("iota1", [128, CAP])
    etab_d = din("etab", [6, 128, ET_W], BF16)
    out_d = nc.dram_tensor("out", [S, D], F32, kind="ExternalOutput").ap()
    dbg = {}
    if debug:
        def dout(name, shape, dt=F32):
            dbg[name] = nc.dram_tensor(name, list(shape), dt, kind="ExternalOutput").ap()
        dout("d_xnT", [128, KC * S], BF16)
        dout("d_x_conv", [S, D])
        dout("d_convT", [128, 3 * S], BF16)
        dout("d_qT0", [128, S], BF16)
        dout("d_kT0", [128, S], BF16)
        dout("d_v", [128, NT * 6 * 65], BF16)
        dout("d_oT0", [64, 2 * S], BF16)
        dout("d_x_dil", [S, D])
        dout("d_osb", [65, 512])
        dout("d_psb", [128, 512], BF16)
        dout("d_ET", [128, 2 * ET_W], BF16)
        dout("d_esb", [128, 512], BF16)
        dout("d_S", [128, 512])
        dout("d_psb0", [128, 512], BF16)

    etab_nz = _etab().astype(np.float32) != 0
    PERS_BYTES = 114 * 1024 + 768
    PH_BYTES = 92 * 1024 - 768

    with contextlib.ExitStack() as st:
        pers_t = st.enter_context(nc.sbuf_tensor("pers", [128, PERS_BYTES // 4], F32))
        ph_t = st.enter_context(nc.sbuf_tensor("phase", [128, PH_BYTES // 4], F32))
        PS = [st.enter_context(nc.psum_tensor("ps%d" % i, [128, 512], F32)) for i in range(8)]
        PSB = [p[:].bitcast(BF16) for p in PS]

        pa = Arena(pers_t, PERS_BYTES)
        X = pa.take(NT * D * 4, F32, "p (i d) -> p i d", i=NT)
        A = pa.take(NT * D * 2, BF16)
        xnT = A.rearrange("p (k t) -> p k t", k=KC)
        xnTok = A.rearrange("p (i d) -> p i d", i=NT)
        ident_b = pa.take(128 * 2, BF16)
        ident_f = pa.take(128 * 4, F32)
        iota1 = pa.take(CAP * 4, F32)
        ones_f = pa.take(128 * 4, F32)
        memT = pa.take(KC * MEM * 2, BF16, "p (k t) -> p k t", k=KC)
        G = pa.take(D * 4, F32)
        junk = pa.take(D * 2, BF16)
        XN = [pa.take(D * 2, BF16), pa.take(D * 2, BF16)]
        ss = pa.take(NT * 4, F32)
        std = pa.take(NT * 4, F32)
        rstd = pa.take(NT * 4, F32)
        cw = pa.take(9 * 4, F32)
        aff_all = pa.take(NT * E * 4, F32, "p (i e) -> p i e", i=NT)
        sel_all = pa.take(NT * E * 4, F32, "p (i e) -> p i e", i=NT)
        sm = pa.take(16 * 4, F32)

        P = Prog()
        cnt = [0]

        def alt():
            cnt[0] += 1
            return "act" if cnt[0] % 2 == 0 else "dve"

        def copy_op(engname, out, in_):
            if engname == "act":
                return lambda e: e.copy(out, in_)
            return lambda e: e.tensor_copy(out, in_)

        def dump(name, src, reads, idx=None):
            if not debug or l != 0:
                return
            dst = dbg[name] if idx is None else dbg[name][idx]
            final_ops.append(P.add("sp", (lambda e: e.dma_start(out=dst, in_=src)), reads=reads, dma_chan="dbg", extra_deps=final_ops[-1:]))

        def dump_x(name):
            if not debug or l != 0:
                return
            for i in range(NT):
                final_ops.append(P.add("sp", (lambda e, i=i: e.dma_start(out=dbg[name][128 * i:128 * (i + 1), :], in_=X[:, i, :])),
                                       reads=[("X", i, 0), ("X", i, 1)], dma_chan="dbg", extra_deps=final_ops[-1:]))

        final_ops = []
        l = 0
        for i in range(NT):
            P.add("sp", (lambda e, i=i: e.dma_start(out=X[:, i, :], in_=x_d[128 * i:128 * (i + 1), :])),
                  writes=[("X", i, 0), ("X", i, 1)], dma_chan=("x", i))
        P.add("act", lambda e: e.dma_start(out=ident_b, in_=identb_d), writes=["ident_b"], dma_chan="c0")
        P.add("act", lambda e: e.dma_start(out=ident_f, in_=identf_d), writes=["ident_f"], dma_chan="c1")
        P.add("act", lambda e: e.dma_start(out=iota1, in_=iota1_d), writes=["iota1"], dma_chan="c2")
        P.add("dve", lambda e: e.memset(ones_f, 1.0), writes=["ones_f"])

        def load_gain(src_row):
            P.add("act", lambda e: e.dma_start(out=G, in_=src_row.partition_broadcast(128)), writes=["G"], dma_chan="g")

        def rms_stats(src_fn, keys_fn, n):
            for i in range(n):
                P.add("act", (lambda e, i=i: e.activation(junk, src_fn(i), AF.Square, accum_out=ss[:, i:i + 1])),
                      reads=keys_fn(i), writes=["junk", ("ss", i)])
            P.add("act", lambda e: e.activation(std[:, 0:n], ss[:, 0:n], AF.Sqrt, bias=eps_ap, scale=1.0 / D),
                  reads=[("ss", i) for i in range(n)] + ["eps"], writes=["std"])
            P.add("dve", lambda e: e.reciprocal(rstd[:, 0:n], std[:, 0:n]), reads=["std"], writes=["rstd"])

        eps_ap = pa.take(4, F32)
        P.add("dve", lambda e: e.memset(eps_ap, EPS), writes=["eps"])

        ph = Arena(ph_t, PH_BYTES)
        memx = ph.take(2 * D * 4, F32, "p (i d) -> p i d", i=2)
        for i in range(2):
            P.add("sp", (lambda e, i=i: e.dma_start(out=memx[:, i, :], in_=mem_d[128 * i:128 * (i + 1), :])),
                  writes=[("memx", i)], dma_chan=("mx", i))
        load_gain(mem_norm_d[0])
        rms_stats(lambda i: memx[:, i, :], lambda i: [("memx", i)], 2)
        for i in range(2):
            xb = XN[i % 2]
            P.add("dve", (lambda e, i=i, xb=xb: e.scalar_tensor_tensor(xb, memx[:, i, :], rstd[:, i:i + 1], G, ALU.mult, ALU.mult)),
                  reads=[("memx", i), "rstd", "G"], writes=[("XN", i % 2)])
            for c in range(KC):
                P.add("pe", (lambda e, c=c, xb=xb, i=i: e.transpose(PSB[i][:, 128 * c:128 * (c + 1)], xb[:, 128 * c:128 * (c + 1)], ident_b)),
                      reads=[("XN", i % 2), "ident_b"], writes=[("ps", i)])
            P.add("act", (lambda e, i=i: e.copy(memT[:, :, 128 * i:128 * (i + 1)], PSB[i].rearrange("p (k t) -> p k t", k=KC))),
                  reads=[("ps", i)], writes=[("memT", i)])
        P.barrier()

        for l in range(n_layers):
            ph = Arena(ph_t, PH_BYTES)
            WI = [ph.take(KC * 384 * 2, BF16, "p (k c) -> p k c", k=KC) for _ in range(2)]
            mix_base = ph.off
            w_in_v = w_in_d[l].rearrange("(k p) c -> p k c", p=128)
            w_out_v = w_out_d[l].rearrange("(k p) d -> p k d", p=128)

            load_gain(norm_mix_d[l])
            P.add("act", lambda e: e.dma_start(out=cw, in_=convw_d[l]), writes=["cw"], dma_chan="cw")

            def load_wi(buf, ranges):
                off = 0
                for (c0, n) in ranges:
                    P.add("pool", (lambda e, c0=c0, n=n, off=off: e.dma_start(out=WI[buf][:, :, off:off + n], in_=w_in_v[:, :, c0:c0 + n])),
                          writes=[("WI", buf, u) for u in range(off // 128, (off + n) // 128)], dma_chan=("wi", buf, off // 128))
                    off += n

            rms_stats(lambda i: X[:, i, :], lambda i: [("X", i, 0), ("X", i, 1)], NT)
            for i in range(NT):
                xb = XN[i % 2]
                pb = i % 2
                P.add("dve", (lambda e, i=i, xb=xb: e.scalar_tensor_tensor(xb, X[:, i, :], rstd[:, i:i + 1], G, ALU.mult, ALU.mult)),
                      reads=[("X", i, 0), ("X", i, 1), "rstd", "G"], writes=[("XN", i % 2)])
                for c in range(KC):
                    P.add("pe", (lambda e, c=c, xb=xb, pb=pb: e.transpose(PSB[pb][:, 128 * c:128 * (c + 1)], xb[:, 128 * c:128 * (c + 1)], ident_b)),
                          reads=[("XN", i % 2), "ident_b"], writes=[("ps", pb)])
                P.add("act", (lambda e, i=i, pb=pb: e.copy(xnT[:, :, 128 * i:128 * (i + 1)], PSB[pb].rearrange("p (k t) -> p k t", k=KC))),
                      reads=[("ps", pb)], writes=[("xnT", i // 4)])

            dump("d_xnT", A, [("xnT", tb) for tb in range(4)])

            def proj_feat(wi_ap, wi_keys, dst_fn, dst_key_fn, bank0, nbank=2, evac_scale=None):
                for tb in range(4):
                    bk = bank0 + tb % nbank
                    for k in range(KC):
                        P.add("pe", (lambda e, k=k, tb=tb, bk=bk: e.matmul(PS[bk][:, :], wi_ap[:, k, :], xnT[:, k, 512 * tb:512 * (tb + 1)],
                                                                         start=(k == 0), stop=(k == KC - 1))),
                              reads=wi_keys + [("xnT", tb)], writes=[("ps", bk)])
                    en = alt()
                    P.add(en, copy_op(en, dst_fn(tb), PS[bk][:, :]), reads=[("ps", bk)], writes=[dst_key_fn(tb)])

            ph.off = mix_base
            WOC = ph.take(3 * D * 2, BF16, "p (c d) -> p c d", c=3)
            P.add("pool", lambda e: e.dma_start(out=WOC, in_=w_out_v[:, 0:3, :]), writes=["WOC"], dma_chan="woc")
            u_pad = ph.take((S + 2) * 4 + 8, F32)
            tmpc = ph.take(S * 4, F32)
            bg_sb = ph.take(S * 4, F32)
            cg_sb = [ph.take(512 * 4, F32) for _ in range(2)]
            convT = ph.take(3 * S * 2, BF16, "p (c t) -> p c t", c=3)
            P.add("dve", lambda e: e.memset(u_pad[:, 0:1], 0.0), writes=["u_l"])
            P.add("dve", lambda e: e.memset(u_pad[:, S + 1:S + 2], 0.0), writes=["u_r"])
            for c in range(3):
                buf = c % 2
                load_wi(buf, [(128 * c, 128), (384 + 128 * c, 128), (768 + 128 * c, 128)])
                for tb in range(4):
                    for j in range(3):
                        bk = 2 + j
                        for k in range(KC):
                            P.add("pe", (lambda e, k=k, tb=tb, bk=bk, j=j, buf=buf: e.matmul(
                                PS[bk][:, :], WI[buf][:, k, 128 * j:128 * (j + 1)], xnT[:, k, 512 * tb:512 * (tb + 1)],
                                start=(k == 0), stop=(k == KC - 1))),
                                reads=[("WI", buf, j), ("xnT", tb)], writes=[("ps", bk)])
                    cb = cg_sb[tb % 2]
                    P.add("act", (lambda e, cb=cb: e.copy(cb, PS[4][:, :])), reads=[("ps", 4)], writes=[("cg", tb % 2)])
                    P.add("dve", (lambda e, cb=cb, tb=tb: e.tensor_tensor(u_pad[:, 1 + 512 * tb:1 + 512 * (tb + 1)], cb, PS[2][:, :], ALU.mult)),
                          reads=[("cg", tb % 2), ("ps", 2)], writes=[("u", tb)])
                    P.add("act", (lambda e, tb=tb: e.copy(bg_sb[:, 512 * tb:512 * (tb + 1)], PS[3][:, :])),
                          reads=[("ps", 3)], writes=[("bg", tb)])
                ukeys = [("u", tb) for tb in range(4)] + ["u_l", "u_r"]
                P.add("dve", (lambda e, c=c: e.tensor_scalar(tmpc, u_pad[:, 1:S + 1], cw[:, 3 * c + 1:3 * c + 2], None, ALU.mult)),
                      reads=ukeys + ["cw"], writes=["tmpc"])
                P.add("dve", (lambda e, c=c: e.scalar_tensor_tensor(tmpc, u_pad[:, 0:S], cw[:, 3 * c:3 * c + 1], tmpc, ALU.mult, ALU.add)),
                      reads=ukeys + ["cw", "tmpc"], writes=["tmpc"])
                P.add("dve", (lambda e, c=c: e.scalar_tensor_tensor(tmpc, u_pad[:, 2:S + 2], cw[:, 3 * c + 2:3 * c + 3], tmpc, ALU.mult, ALU.add)),
                      reads=ukeys + ["cw", "tmpc"], writes=["tmpc"])
                P.add("dve", (lambda e, c=c: e.tensor_tensor(convT[:, c, :], tmpc, bg_sb, ALU.mult)),
                      reads=["tmpc"] + [("bg", tb) for tb in range(4)], writes=[("convT", c)])
            for i in range(NT):
                for hf in range(2):
                    bk = 5 + (2 * i + hf) % 3
                    for c in range(3):
                        P.add("pe", (lambda e, i=i, hf=hf, c=c, bk=bk: e.matmul(
                            PS[bk][:, :], convT[:, c, 128 * i:128 * (i + 1)], WOC[:, c, 512 * hf:512 * (hf + 1)],
                            start=(c == 0), stop=(c == 2))),
                            reads=[("convT", c), "WOC"], writes=[("ps", bk)])
                    P.add("dve", (lambda e, i=i, hf=hf, bk=bk: e.tensor_tensor(
                        X[:, i, 512 * hf:512 * (hf + 1)], X[:, i, 512 * hf:512 * (hf + 1)], PS[bk][:, :], ALU.add)),
                        reads=[("ps", bk), ("X", i, hf)], writes=[("X", i, hf)])
            dump("d_convT", convT.rearrange("p c t -> p (c t)"), [("convT", c) for c in range(3)])
            dump_x("d_x_conv")
            P.barrier()

            pending = []

            import os as _os
            DEFER = _os.environ.get("K_DEFER", "1") == "1"

            def flush_pending():
                while pending:
                    pending.pop(0)()

            def attn_finish(par, hh, qb, oT_dst, att_key, r_row, o_sb):
                bo = 3 + par
                lrow = 64 if hh == 0 else 0
                olo = 0 if hh == 0 else 64
                lnl = o_sb[0]
                bcs = o_sb[1]
                P.add("act", (lambda e: e.activation(lnl[lrow:lrow + 1, :], PS[bo][lrow:lrow + 1, :], AF.Ln)), reads=[("ps", bo)], writes=["lnl"])
                P.add("act", (lambda e: e.activation(r_row[lrow:lrow + 1, :], lnl[lrow:lrow + 1, :], AF.Exp, scale=-1.0)), reads=["lnl"], writes=["r_row"])
                mo = 64 if hh == 0 else 128
                P.add("pe", lambda e: e.matmul(PS[5][0:mo, :], ones_f[lrow:lrow + 1, 0:mo], r_row[lrow:lrow + 1, :], start=True, stop=True),
                      reads=["r_row", "ones_f"], writes=[("ps", 5)])
                P.add("dve", (lambda e: e.tensor_copy(bcs[olo:olo + 64, :], PS[5][olo:olo + 64, :])), reads=[("ps", 5)], writes=["bcs"])
                P.add("dve", (lambda e: e.tensor_tensor(oT_dst[olo:olo + 64, 512 * qb:512 * (qb + 1)], PS[bo][olo:olo + 64, :], bcs[olo:olo + 64, :], ALU.mult)),
                      reads=[("ps", bo), "bcs"], writes=[att_key + (hh, qb)])

            def attn_outproj(oT_all, npair, att_key, WO_part):
                for i in range(NT):
                    for hf in range(2):
                        bk = 6 + (2 * i + hf) % 2
                        for hp_ in range(npair):
                            P.add("pe", (lambda e, hp_=hp_: e.matmul(
                                PS[bk][:, :], oT_all[:, hp_, 128 * i:128 * (i + 1)], WO_part[:, hp_, 512 * hf:512 * (hf + 1)],
                                start=(hp_ == 0), stop=(hp_ == npair - 1))),
                                reads=[(att_key, hp_, 0, i // 4), (att_key, hp_, 1, i // 4), "WO_part"], writes=[("ps", bk)])
                        P.add("dve", (lambda e: e.tensor_tensor(
                            X[:, i, 512 * hf:512 * (hf + 1)], X[:, i, 512 * hf:512 * (hf + 1)], PS[bk][:, :], ALU.add)),
                            reads=[("ps", bk), ("X", i, hf)], writes=[("X", i, hf)])

            def proj_q_pair(wi_ap, wi_key, qz_, zkey):
                for tb in range(4):
                    bk = 6 + tb % 2
                    for k in range(KC):
                        P.add("pe", (lambda e, k=k: e.matmul(PS[bk][:, :], wi_ap[:, k, :], xnT[:, k, 512 * tb:512 * (tb + 1)],
                                                             start=(k == 0), stop=(k == KC - 1))),
                              reads=[wi_key, ("xnT", tb)], writes=[("ps", bk)])
                    P.add("act", (lambda e: e.copy(qz_[0][0:64, 512 * tb:512 * (tb + 1)], PS[bk][0:64, :])),
                          reads=[("ps", bk), zkey + "0pad"], writes=[(zkey, 0, tb)])
                    P.add("dve", (lambda e: e.tensor_copy(qz_[1][64:128, 512 * tb:512 * (tb + 1)], PS[bk][64:128, :])),
                          reads=[("ps", bk), zkey + "1pad"], writes=[(zkey, 1, tb)])

            ph.off = mix_base
            VB = 192
            v_flat = ph.take(NT * 3 * VB * 2, BF16)
            v4 = v_flat.rearrange("p (i h c) -> p i h c", i=NT, h=3)
            qz = [ph.take(S * 2, BF16) for _ in range(2)]
            kT = [ph.take(S * 2, BF16) for _ in range(2)]
            ET = ph.take(2 * ET_W * 2, BF16, "p (h v) -> p h v", h=2)
            oT_all = ph.take(3 * S * 2, BF16, "p (h t) -> p h t", h=3)
            WOD = ph.take(3 * D * 2, BF16, "p (c d) -> p c d", c=3)
            e_sb = [ph.take(512 * 2, BF16) for _ in range(3)]
            p_sb = [ph.take(512 * 2, BF16) for _ in range(3)]
            o_sb = [ph.take(512 * 4, F32) for _ in range(2)]
            r_row = ph.take(512 * 4, F32)
            P.add("dve", lambda e: e.memset(qz[0][64:128, :], 0.0), writes=["qz0pad"])
            P.add("dve", lambda e: e.memset(qz[1][0:64, :], 0.0), writes=["qz1pad"])
            P.add("dve", lambda e: e.memset(v4[:, :, :, 64:128], 0.0), writes=["v_ones"])
            P.add("dve", lambda e: e.memset(v4[:, :, :, 64:65], 1.0), reads=["v_ones"], writes=["v_ones"])
            P.add("dve", lambda e: e.memset(v4[:, :, :, 96:97], 1.0), reads=["v_ones"], writes=["v_ones"])
            P.add("pool", lambda e: e.dma_start(out=WOD, in_=w_out_d[l][384:768, :].rearrange("(c p) d -> p c d", p=128)),
                  writes=["WO_part"], dma_chan="wod")
            load_wi(0, [(1920, 384)])
            for i in range(NT):
                bk = i % 2
                for k in range(KC):
                    P.add("pe", (lambda e, k=k: e.matmul(PS[bk][:, 0:384], xnT[:, k, 128 * i:128 * (i + 1)], WI[0][:, k, 0:384],
                                                         start=(k == 0), stop=(k == KC - 1))),
                          reads=[("WI", 0, 0), ("WI", 0, 1), ("WI", 0, 2), ("xnT", i // 4)], writes=[("ps", bk)])
                psv = PS[bk][:, 0:384].rearrange("p (h two c) -> p h two c", h=3, two=2)
                en = "act" if i % 2 == 0 else "dve"
                P.add(en, copy_op(en, v4[:, i, :, 0:64], psv[:, :, 0, :]), reads=[("ps", bk)], writes=[("vA", i)])
                P.add(en, copy_op(en, v4[:, i, :, 128:192], psv[:, :, 1, :]), reads=[("ps", bk)], writes=[("vB", i)])

            for hp in range(3):
                buf = (hp + 1) % 2
                load_wi(buf, [(1152 + 128 * hp, 128), (1536 + 128 * hp, 128)])
                P.add("sp", (lambda e: e.dma_start(out=ET, in_=etab_d[2 * hp:2 * hp + 2].rearrange("h p v -> p h v"))),
                      writes=["ET"], dma_chan="et")
                kb_ = kT[hp % 2]
                proj_q_pair(WI[buf][:, :, 0:128], ("WI", buf, 0), qz, "qz")
                proj_feat(WI[buf][:, :, 128:256], [("WI", buf, 1)], (lambda tb, kb_=kb_: kb_[:, 512 * tb:512 * (tb + 1)]),
                          (lambda tb, hp=hp: ("kT", hp % 2, tb)), 6)
                gi = 0
                for hh in range(2):
                    h = 2 * hp + hh
                    for qb in range(4):
                        kts = []
                        for kt in range(max(0, 4 * qb - 8), min(15, 4 * qb + 11) + 1):
                            v0 = ET_OFF - (128 * kt - 512 * qb)
                            if etab_nz[h][:, v0:v0 + 512].any():
                                kts.append(kt)
                        par = gi % 2
                        gi += 1
                        bo = 3 + par

                        def s_mm(kt, n):
                            bs = n % 3
                            P.add("pe", (lambda e: e.matmul(PS[bs][:, :], kb_[:, 128 * kt:128 * (kt + 1)],
                                                            qz[hh][:, 512 * qb:512 * (qb + 1)], start=True, stop=True)),
                                  reads=[("kT", hp % 2, kt // 4), ("qz", hh, qb), "qz%dpad" % hh], writes=[("ps", bs)])

                        s_mm(kts[0], 0)
                        if len(kts) > 1:
                            s_mm(kts[1], 1)
                        for n, kt in enumerate(kts):
                            bs = n % 3
                            v0 = ET_OFF - (128 * kt - 512 * qb)
                            P.add("act", (lambda e: e.activation(e_sb[bs], PS[bs][:, :], AF.Exp, scale=0.125)),
                                  reads=[("ps", bs)], writes=[("e_sb", bs)])
                            P.add("dve", (lambda e: e.tensor_tensor(p_sb[bs], e_sb[bs], ET[:, hh, v0:v0 + 512], ALU.mult)),
                                  reads=[("e_sb", bs), "ET"], writes=[("p_sb", bs)])
                            if n + 2 < len(kts):
                                s_mm(kts[n + 2], n + 2)
                            voff = (kt * 3 + hp) * VB + 64 * hh
                            P.add("pe", (lambda e: e.matmul(PS[bo][:, :], v_flat[:, voff:voff + 128], p_sb[bs],
                                                            start=(n == 0), stop=(n == len(kts) - 1))),
                                  reads=[("p_sb", bs), ("vA", kt), ("vB", kt), "v_ones"], writes=[("ps", bo)])
                            if n == min(1, len(kts) - 1):
                                flush_pending()
                        pending.append(lambda par=par, hh=hh, qb=qb, hp=hp: attn_finish(
                            par, hh, qb, oT_all[:, hp, :], ("oT", hp), r_row, o_sb))
                        if not DEFER:
                            flush_pending()
            flush_pending()
            attn_outproj(oT_all, 3, "oT", WOD)
            P.barrier()

            ph.off = mix_base
            if skip_mem:
                continue
            WMKV = ph.take(KC * 512 * 2, BF16, "p (k c) -> p k c", k=KC)
            kmT = ph.take(2 * MEM * 2, BF16, "p (h t) -> p h t", h=2)
            vm_flat = ph.take(2 * 2 * VB * 2, BF16)
            vm4 = vm_flat.rearrange("p (i h c) -> p i h c", i=2, h=2)
            qmz = [ph.take(S * 2, BF16) for _ in range(2)]
            oTm_all = ph.take(2 * S * 2, BF16, "p (h t) -> p h t", h=2)
            WOM = ph.take(2 * D * 2, BF16, "p (c d) -> p c d", c=2)
            p_sb = [ph.take(512 * 2, BF16) for _ in range(4)]
            o_sb = [ph.take(512 * 4, F32) for _ in range(2)]
            r_row = ph.take(512 * 4, F32)
            P.add("dve", lambda e: e.memset(qmz[0][64:128, :], 0.0), writes=["qmz0pad"])
            P.add("dve", lambda e: e.memset(qmz[1][0:64, :], 0.0), writes=["qmz1pad"])
            P.add("dve", lambda e: e.memset(vm4[:, :, :, 64:128], 0.0), writes=["vm_ones"])
            P.add("dve", lambda e: e.memset(vm4[:, :, :, 64:65], 1.0), reads=["vm_ones"], writes=["vm_ones"])
            P.add("dve", lambda e: e.memset(vm4[:, :, :, 96:97], 1.0), reads=["vm_ones"], writes=["vm_ones"])
            P.add("pool", lambda e: e.dma_start(out=WMKV, in_=w_mkv_d[l].rearrange("(k p) c -> p k c", p=128)), writes=["WMKV"], dma_chan="wmkv")
            P.add("pool", lambda e: e.dma_start(out=WOM, in_=w_out_d[l][768:1024, :].rearrange("(c p) d -> p c d", p=128)),
                  writes=["WO_part"], dma_chan="wod")
            for mp in range(2):
                for k in range(KC):
                    P.add("pe", (lambda e, k=k: e.matmul(PS[0][:, 0:MEM], WMKV[:, k, 128 * mp:128 * (mp + 1)], memT[:, k, :],
                                                         start=(k == 0), stop=(k == KC - 1))),
                          reads=["WMKV", ("memT", 0), ("memT", 1)], writes=[("ps", 0)])
                P.add("act", (lambda e: e.copy(kmT[:, mp, :], PS[0][:, 0:MEM])), reads=[("ps", 0)], writes=[("kmT", mp)])
            for i in range(2):
                for k in range(KC):
                    P.add("pe", (lambda e, k=k: e.matmul(PS[1][:, 0:256], memT[:, k, 128 * i:128 * (i + 1)], WMKV[:, k, 256:512],
                                                         start=(k == 0), stop=(k == KC - 1))),
                          reads=["WMKV", ("memT", 0), ("memT", 1)], writes=[("ps", 1)])
                psv = PS[1][:, 0:256].rearrange("p (h two c) -> p h two c", h=2, two=2)
                P.add("act", (lambda e: e.copy(vm4[:, i, :, 0:64], psv[:, :, 0, :])), reads=[("ps", 1)], writes=[("vmA", i)])
                P.add("act", (lambda e: e.copy(vm4[:, i, :, 128:192], psv[:, :, 1, :])), reads=[("ps", 1)], writes=[("vmB", i)])
            vmkeys = [("vmA", 0), ("vmA", 1), ("vmB", 0), ("vmB", 1), "vm_ones"]
            gi = 0
            for mp in range(2):
                buf = mp % 2
                load_wi(buf, [(2304 + 128 * mp, 128)])
                proj_q_pair(WI[buf][:, :, 0:128], ("WI", buf, 0), qmz, "qmz")
                for hh in range(2):
                    for qb in range(4):
                        par = gi % 2
                        sb0 = 2 * (gi % 2)
                        gi += 1
                        bo = 3 + par
                        for kt in range(2):
                            bs = sb0 + kt
                            P.add("pe", (lambda e: e.matmul(PS[bs if bs < 3 else 7][:, :], kmT[:, mp, 128 * kt:128 * (kt + 1)],
                                                            qmz[hh][:, 512 * qb:512 * (qb + 1)], start=True, stop=True)),
                                  reads=[("kmT", mp), ("qmz", hh, qb), "qmz%dpad" % hh], writes=[("ps", bs if bs < 3 else 7)])
                        for kt in range(2):
                            bs = sb0 + kt
                            pbk = bs if bs < 3 else 7
                            P.add("act", (lambda e: e.activation(p_sb[bs], PS[pbk][:, :], AF.Exp, scale=0.125)),
                                  reads=[("ps", pbk)], writes=[("p_sb", bs)])
                            if kt == 0:
                                flush_pending()
                            voff = (kt * 2 + mp) * VB + 64 * hh
                            P.add("pe", (lambda e: e.matmul(PS[bo][:, :], vm_flat[:, voff:voff + 128], p_sb[bs], start=(kt == 0), stop=(kt == 1))),
                                  reads=[("p_sb", bs)] + vmkeys, writes=[("ps", bo)])
                        pending.append(lambda par=par, hh=hh, qb=qb, mp=mp: attn_finish(
                            par, hh, qb, oTm_all[:, mp, :], ("oTm", mp), r_row, o_sb))
            flush_pending()
            attn_outproj(oTm_all, 2, "oTm", WOM)
            P.barrier()

            if not do_moe:
                continue
            ph = Arena(ph_t, PH_BYTES)
            WG = [ph.take(KC * 512 * 2, BF16, "p (k f) -> p k f", k=KC) for _ in range(2)]
            WU = [ph.take(KC * 512 * 2, BF16, "p (k f) -> p k f", k=KC) for _ in range(2)]
            WD = [ph.take(4 * D * 2, BF16, "p (c d) -> p c d", c=4) for _ in range(2)]
            WR = ph.take(KC * E * 4, F32, "p (k e) -> p k e", k=KC)
            moe_base = ph.off

            def load_expert_w(e_, fs):
                buf = (e_ * 4 + fs) % 2
                P.add("pool", (lambda e, e_=e_, fs=fs, buf=buf: e.dma_start(
                    out=WG[buf], in_=w_gate_d[l, e_].rearrange("(k p) f -> p k f", p=128)[:, :, 512 * fs:512 * (fs + 1)])),
                    writes=[("WG", buf)], dma_chan=("wg", buf))
                P.add("pool", (lambda e, e_=e_, fs=fs, buf=buf: e.dma_start(
                    out=WU[buf], in_=w_up_d[l, e_].rearrange("(k p) f -> p k f", p=128)[:, :, 512 * fs:512 * (fs + 1)])),
                    writes=[("WU", buf)], dma_chan=("wu", buf))
                P.add("pool", (lambda e, e_=e_, fs=fs, buf=buf: e.dma_start(
                    out=WD[buf], in_=w_down_d[l, e_][512 * fs:512 * (fs + 1), :].rearrange("(c p) d -> p c d", p=128))),
                    writes=[("WD", buf)], dma_chan=("wd", buf))

            load_gain(norm_ffn_d[l])
            P.add("act", lambda e: e.dma_start(out=WR, in_=w_router_d[l].rearrange("(k p) e -> p k e", p=128)), writes=["WR"], dma_chan="wr")
            load_expert_w(0, 0)
            load_expert_w(0, 1)

            xnf = ph.take(D * 4, F32)
            xnTf = ph.take(KC * 128 * 4, F32, "p (k t) -> p k t", k=KC)
            affT = ph.take(S * 4, F32)
            wk = [ph.take(S * 4, F32) for _ in range(2)]
            maskT = ph.take(S * 4, F32)
            m8 = ph.take(8 * 4, F32)
            rms_stats(lambda i: X[:, i, :], lambda i: [("X", i, 0), ("X", i, 1)], NT)
            for i in range(NT):
                P.add("dve", (lambda e, i=i: e.scalar_tensor_tensor(xnf, X[:, i, :], rstd[:, i:i + 1], G, ALU.mult, ALU.mult)),
                      reads=[("X", i, 0), ("X", i, 1), "rstd", "G"], writes=["xnf"])
                P.add("act", (lambda e, i=i: e.copy(xnTok[:, i, :], xnf)), reads=["xnf"], writes=[("xnTok", i)])
                for c in range(KC):
                    bk = c // 4
                    P.add("pe", (lambda e, c=c, bk=bk: e.transpose(PS[bk][:, 128 * (c % 4):128 * (c % 4 + 1)], xnf[:, 128 * c:128 * (c + 1)], ident_f)),
                          reads=["xnf", "ident_f"], writes=[("ps", bk)])
                P.add("act", lambda e: e.copy(xnTf[:, 0:4, :], PS[0][:, :].rearrange("p (k t) -> p k t", k=4)), reads=[("ps", 0)], writes=[("xnTf", 0)])
                P.add("dve", lambda e: e.tensor_copy(xnTf[:, 4:8, :], PS[1][:, :].rearrange("p (k t) -> p k t", k=4)), reads=[("ps", 1)], writes=[("xnTf", 1)])
                for k in range(KC):
                    P.add("pe", (lambda e, k=k: e.matmul(PS[2][:, 0:E], xnTf[:, k, :], WR[:, k, :], start=(k == 0), stop=(k == KC - 1))),
                          reads=[("xnTf", k // 4), "WR"], writes=[("ps", 2)])
                P.add("dve", lambda e: e.reduce_max(sm[:, 0:1], PS[2][:, 0:E], axis=AX.X), reads=[("ps", 2)], writes=["sm0"])
                P.add("dve", lambda e: e.tensor_scalar(sm[:, 1:2], sm[:, 0:1], -1.0, None, ALU.mult), reads=["sm0"], writes=["sm1"])
                P.add("act", (lambda e, i=i: e.activation(aff_all[:, i, :], PS[2][:, 0:E], AF.Exp, bias=sm[:, 1:2], scale=1.0, accum_out=sm[:, 2:3])),
                      reads=[("ps", 2), "sm1"], writes=[("aff", i), "sm2"])
                P.add("dve", lambda e: e.reciprocal(sm[:, 3:4], sm[:, 2:3]), reads=["sm2"], writes=["sm3"])
                P.add("dve", (lambda e, i=i: e.tensor_scalar(aff_all[:, i, :], aff_all[:, i, :], sm[:, 3:4], None, ALU.mult)),
                      reads=[("aff", i), "sm3"], writes=[("aff", i)])
            for i in range(NT):
                bk = 3 + i // 4
                P.add("pe", (lambda e, i=i, bk=bk: e.transpose(PS[bk][0:E, 128 * (i % 4):128 * (i % 4 + 1)], aff_all[:, i, :], ident_f)),
                      reads=[("aff", i), "ident_f"], writes=[("ps", bk)])
            for j in range(4):
                P.add("act", (lambda e, j=j: e.copy(affT[0:E, 512 * j:512 * (j + 1)], PS[3 + j][0:E, :])), reads=[("ps", 3 + j)], writes=[("affT", j)])
                P.add("dve", (lambda e, j=j: e.tensor_copy(wk[0][0:E, 512 * j:512 * (j + 1)], affT[0:E, 512 * j:512 * (j + 1)])), reads=[("affT", j)], writes=["wk0"])
            nit = CAP // 8
            for it in range(nit):
                src = wk[it % 2]
                dst = wk[(it + 1) % 2]
                P.add("dve", (lambda e, src=src: e.max(m8[0:E, :], src[0:E, :])), reads=["wk%d" % (it % 2)], writes=["m8"])
                if it < nit - 1:
                    P.add("dve", (lambda e, src=src, dst=dst: e.match_replace(dst[0:E, :], m8[0:E, :], src[0:E, :], -1e30)),
                          reads=["m8", "wk%d" % (it % 2)], writes=["wk%d" % ((it + 1) % 2)])
            affT_keys = [("affT", j) for j in range(4)]
            P.add("dve", lambda e: e.tensor_scalar(maskT[0:E, :], affT[0:E, :], m8[0:E, 7:8], None, ALU.is_ge), reads=affT_keys + ["m8"], writes=["maskT"])
            P.add("dve", lambda e: e.tensor_tensor_scan(wk[0][0:E, :], maskT[0:E, :], maskT[0:E, :], 0.0, ALU.add, ALU.max),
                  reads=["maskT", "wk0"], writes=["wk0"])
            P.add("dve", lambda e: e.tensor_tensor(wk[1][0:E, :], wk[0][0:E, :], maskT[0:E, :], ALU.mult), reads=["wk0", "maskT", "wk1"], writes=["wk1"])
            for i in range(NT):
                P.add("pe", (lambda e, i=i: e.transpose(PS[7][:, E * i:E * (i + 1)], wk[1][0:E, 128 * i:128 * (i + 1)], ident_f[0:E, 0:E])),
                      reads=["wk1", "ident_f"], writes=[("ps", 7)])
            P.add("act", lambda e: e.copy(sel_all, PS[7][:, 0:NT * E].rearrange("p (i e) -> p i e", i=NT)), reads=[("ps", 7)], writes=["sel"])
            P.barrier()

            ph.off = moe_base
            Ssel = ph.take(NT * CAP * 2, BF16, "p (i j) -> p i j", i=NT)
            Sg = ph.take(8 * CAP * 2, BF16, "p (i j) -> p i j", i=8)
            SgT = [ph.take(2 * S * 2, BF16, "p (j t) -> p j t", j=2) for _ in range(2)]
            xgT = ph.take(KC * CAP * 2, BF16, "p (k j) -> p k j", k=KC)
            hT = [ph.take(4 * CAP * 2, BF16, "p (c j) -> p c j", c=4) for _ in range(2)]
            sg_sb = [ph.take(CAP * 4, F32) for _ in range(2)]
            y_sb = ph.take(2 * D * 2, BF16, "p (j d) -> p j d", j=2)
            xg_keys = [("xgT", c) for c in range(KC)]

            def scatter_unit(es, u):
                i, hf = u // 2, u % 2
                bk = 6 + u % 2
                for jc in range(2):
                    P.add("pe", (lambda e, jc=jc: e.matmul(
                        PS[bk][:, :], SgT[es % 2][:, jc, 128 * i:128 * (i + 1)], y_sb[:, jc, 512 * hf:512 * (hf + 1)], start=(jc == 0), stop=(jc == 1))),
                        reads=[("SgT", es % 2, jc, i // 8), ("y", jc, hf)], writes=[("ps", bk)])
                P.add("dve", (lambda e: e.tensor_tensor(
                    X[:, i, 512 * hf:512 * (hf + 1)], X[:, i, 512 * hf:512 * (hf + 1)], PS[bk][:, :], ALU.add)),
                    reads=[("ps", bk), ("X", i, hf)], writes=[("X", i, hf)])

            for e_ in range(E):
                for half in range(2):
                    for ii in range(8):
                        i = 8 * half + ii
                        P.add("dve", (lambda e, i=i: e.tensor_scalar(Ssel[:, i, :], iota1, sel_all[:, i, e_:e_ + 1], None, ALU.is_equal)),
                              reads=["iota1", "sel"], writes=[("Ssel", i)])
                        P.add("dve", (lambda e, i=i, ii=ii: e.tensor_scalar(Sg[:, ii, :], iota1, sel_all[:, i, e_:e_ + 1], aff_all[:, i, e_:e_ + 1],
                                                                            ALU.is_equal, ALU.mult)),
                              reads=["iota1", "sel", ("aff", i)], writes=[("Sg", ii)])
                    for jc in range(2):
                        bk = 6 + jc
                        for ii in range(8):
                            P.add("pe", (lambda e, ii=ii, jc=jc, bk=bk: e.transpose(
                                PSB[bk][:, 128 * ii:128 * (ii + 1)], Sg[:, ii, 128 * jc:128 * (jc + 1)], ident_b)),
                                reads=[("Sg", ii), "ident_b"], writes=[("ps", bk)])
                        en = alt()
                        P.add(en, copy_op(en, SgT[e_ % 2][:, jc, 1024 * half:1024 * (half + 1)], PSB[bk]), reads=[("ps", bk)],
                              writes=[("SgT", e_ % 2, jc, half)])
                for c in range(KC):
                    bk = 6 + c % 2
                    for i in range(NT):
                        P.add("pe", (lambda e, c=c, i=i, bk=bk: e.matmul(PS[bk][:, 0:CAP], xnTok[:, i, 128 * c:128 * (c + 1)], Ssel[:, i, :],
                                                                         start=(i == 0), stop=(i == NT - 1))),
                              reads=[("xnTok", i), ("Ssel", i)], writes=[("ps", bk)])
                    en = alt()
                    P.add(en, copy_op(en, xgT[:, c, :], PS[bk][:, 0:CAP]), reads=[("ps", bk)], writes=[("xgT", c)])

                def gate_up(f):
                    fs, fc = f // 4, f % 4
                    buf = (e_ * 4 + fs) % 2
                    bk = 4 + f % 2
                    for k in range(KC):
                        P.add("pe", (lambda e, k=k: e.matmul(PS[bk][:, 0:CAP], WG[buf][:, k, 128 * fc:128 * (fc + 1)], xgT[:, k, :],
                                                             start=(k == 0), stop=(k == KC - 1))),
                              reads=[("WG", buf)] + xg_keys, writes=[("ps", bk)])
                    for k in range(KC):
                        P.add("pe", (lambda e, k=k: e.matmul(PS[bk][:, CAP:2 * CAP], WU[buf][:, k, 128 * fc:128 * (fc + 1)], xgT[:, k, :],
                                                             start=(k == 0), stop=(k == KC - 1))),
                              reads=[("WU", buf)] + xg_keys, writes=[("ps", bk)])

                gate_up(0)
                for f in range(16):
                    fs, fc = f // 4, f % 4
                    buf = (e_ * 4 + fs) % 2
                    bk = 4 + f % 2
                    hb = hT[fs % 2]
                    sgb = sg_sb[f % 2]
                    P.add("act", (lambda e: e.activation(sgb, PS[bk][:, 0:CAP], AF.Silu)), reads=[("ps", bk)], writes=[("sg", f % 2)])
                    P.add("dve", (lambda e: e.tensor_tensor(hb[:, fc, :], sgb, PS[bk][:, CAP:2 * CAP], ALU.mult)),
                          reads=[("sg", f % 2), ("ps", bk)], writes=[("hT", fs % 2, fc)])
                    if f + 1 < 16:
                        gate_up(f + 1)
                    for jc in range(2):
                        for hf in range(2):
                            by = 2 * jc + hf
                            P.add("pe", (lambda e, jc=jc, hf=hf, by=by: e.matmul(
                                PS[by][:, :], hb[:, fc, 128 * jc:128 * (jc + 1)], WD[buf][:, fc, 512 * hf:512 * (hf + 1)],
                                start=(f == 0), stop=(f == 15))),
                                reads=[("hT", fs % 2, fc), ("WD", buf)], writes=[("ps", by)])
                    if e_ > 0:
                        scatter_unit(e_ - 1, 2 * f)
                        scatter_unit(e_ - 1, 2 * f + 1)
                    if fc == 3:
                        nxt = e_ * 4 + fs + 2
                        if nxt < E * 4:
                            load_expert_w(nxt // 4, nxt % 4)
                for jc in range(2):
                    for hf in range(2):
                        by = 2 * jc + hf
                        en = alt()
                        P.add(en, copy_op(en, y_sb[:, jc, 512 * hf:512 * (hf + 1)], PS[by][:, :]), reads=[("ps", by)], writes=[("y", jc, hf)])
            for u in range(32):
                scatter_unit(E - 1, u)
            P.barrier()

        ph = Arena(ph_t, PH_BYTES)
        ob = [ph.take(D * 4, F32) for _ in range(4)]
        if debug:
            for i in range(NT):
                final_ops.append(P.add("sp", (lambda e, i=i: e.dma_start(out=out_d[128 * i:128 * (i + 1), :], in_=X[:, i, :])),
                                       reads=[("X", i, 0), ("X", i, 1)], dma_chan="dbg", extra_deps=final_ops[-1:]))
        else:
            load_gain(norm_final_d[0])
            rms_stats(lambda i: X[:, i, :], lambda i: [("X", i, 0), ("X", i, 1)], NT)
            for i in range(NT):
                P.add("dve", (lambda e, i=i: e.scalar_tensor_tensor(ob[i % 4], X[:, i, :], rstd[:, i:i + 1], G, ALU.mult, ALU.mult)),
                      reads=[("X", i, 0), ("X", i, 1), "rstd", "G"], writes=[("ob", i % 4)])
                final_ops.append(P.add("sp", (lambda e, i=i: e.dma_start(out=out_d[128 * i:128 * (i + 1), :], in_=ob[i % 4])),
                                       reads=[("ob", i % 4)], writes=[("obd", i % 4)], dma_chan=("o", i % 4)))
        P.emit(nc, final_waits=[("sp", o) for o in final_ops])
    return nc


def _etab():
    t = np.zeros((6, 128, ET_W), np.float64)
    p = np.arange(128)[:, None]
    v = np.arange(ET_W)[None, :]
    d = p - v + ET_OFF
    ad = np.abs(d)
    m = (ad <= 64).astype(np.float64) + ((d % 4 == 0) & (ad <= 256)) + ((d % 16 == 0) & (ad <= 1024))
    for h in range(6):
        slope = 2.0 ** (-8.0 * (h + 1) / 6)
        t[h] = m * np.exp(-slope * ad)
    return t.astype(np.float32).astype(ml_dtypes.bfloat16)


def _consts():
    return {
        "ident_bf": np.eye(128, dtype=np.float32).astype(ml_dtypes.bfloat16),
        "ident_f": np.eye(128, dtype=np.float32),
        "iota1": np.tile(np.arange(1, CAP + 1, dtype=np.float32)[None, :], (128, 1)),
        "etab": _etab(),
    }


def make_in_maps(x, mem, mem_norm, norm_mix, w_in, conv_w, w_mem_kv, w_out, norm_ffn,
                 w_router, w_gate, w_up, w_down, norm_final):
    f = lambda a: np.ascontiguousarray(np.asarray(a, dtype=np.float32))
    conv_w = f(conv_w)
    convw_t = np.ascontiguousarray(conv_w.reshape(L, 3, 3, 128).transpose(0, 3, 2, 1).reshape(L, 128, 9))
    shared = {
        "mem_norm": f(mem_norm).reshape(1, D), "norm_mix": f(norm_mix), "norm_ffn": f(norm_ffn),
        "norm_final": f(norm_final).reshape(1, D), "w_in": f(w_in), "convw_t": convw_t,
        "w_mem_kv": f(w_mem_kv), "w_out": f(w_out), "w_router": f(w_router),
        "w_gate": f(w_gate), "w_up": f(w_up), "w_down": f(w_down),
    }
    shared.update(_consts())
    x = f(x)
    mem = f(mem)
    return [dict(shared, x=x[b], mem=mem[b]) for b in range(N_CORES)]


def kernel(x, mem, mem_norm, norm_mix, w_in, conv_w, w_mem_kv, w_out, norm_ffn,
           w_router, w_gate, w_up, w_down, norm_final):
    in_maps = make_in_maps(x, mem, mem_norm, norm_mix, w_in, conv_w, w_mem_kv, w_out, norm_ffn,
                           w_router, w_gate, w_up, w_down, norm_final)
    nc = build_program()
    res = run_bass_kernel_spmd(nc, in_maps, core_ids=list(range(N_CORES)))
    return np.stack([np.asarray(r["out"], dtype=np.float32) for r in res.results], axis=0)
```

```python
import contextlib
import numpy as np
import ml_dtypes
import concourse.bass as bass
import concourse.mybir as mybir
from concourse.bass_utils import run_bass_kernel_spmd

F32 = mybir.dt.float32
BF16 = mybir.dt.bfloat16
AF = mybir.ActivationFunctionType
ALU = mybir.AluOpType
AX = mybir.AxisListType

D = 1024
S = 2048
NT = 16
KC = 8
L = 2
E = 16
FF = 2048
CAP = 256
MEM = 256
EPS = 1e-6
ET_W = 2944
ET_OFF = 1408
N_CORES = 8


class _Rec:
    def __init__(self):
        self.call = None

    def __getattr__(self, name):
        def f(*a, **k):
            self.call = (name, a, k)
            return self
        return f


class Prog:
    def __init__(self):
        self.ops = []
        self.last_writer = {}
        self.readers = {}
        self.chan_count = {}
        self.last_eng = {}
        self.last_chan = {}

    def add(self, eng, fn, reads=(), writes=(), dma_chan=None, extra_deps=()):
        idx = len(self.ops)
        deps = set(extra_deps)
        for k in reads:
            w = self.last_writer.get(k)
            if w is not None:
                deps.add(w)
        for k in writes:
            w = self.last_writer.get(k)
            if w is not None:
                deps.add(w)
            for r in self.readers.get(k, {}).values():
                deps.add(r)
        deps.discard(idx)
        rkey = eng if dma_chan is None else ("dma", idx)
        for k in reads:
            self.readers.setdefault(k, {})[rkey] = idx
        for k in writes:
            self.last_writer[k] = idx
            self.readers[k] = {}
        if fn is not None:
            rec = _Rec()
            fn(rec)
            call = rec.call
            assert call is not None
            fn = (lambda e, call=call: getattr(e, call[0])(*call[1], **call[2]))
        op = dict(eng=eng, fn=fn, deps=deps, chan=dma_chan, signal=False)
        if dma_chan is not None:
            self.chan_count[dma_chan] = self.chan_count.get(dma_chan, 0) + 16
            op["dmaval"] = self.chan_count[dma_chan]
            self.last_chan[dma_chan] = idx
        elif fn is not None:
            self.last_eng[eng] = idx
        self.ops.append(op)
        return idx

    def barrier(self):
        deps = set(self.last_eng.values()) | set(self.last_chan.values())
        for e in ["pe", "act", "dve", "pool", "sp"]:
            self.add(e, None, extra_deps=deps)

    def emit(self, nc, final_waits=()):
        ops = self.ops
        for op in ops:
            for d in op["deps"]:
                p = ops[d]
                if p["chan"] is None:
                    if p["eng"] == "pe" and op["eng"] == "pe" and op["chan"] is None and op["fn"] is not None:
                        continue
                    p["signal"] = True
        for (_, fo) in final_waits:
            if ops[fo]["chan"] is None:
                ops[fo]["signal"] = True
        engs = ["pe", "act", "dve", "pool", "sp"]
        seq = {e: 0 for e in engs}
        for op in ops:
            if op["chan"] is None and op["signal"]:
                seq[op["eng"]] += 1
                op["seqval"] = seq[op["eng"]]
        chans = sorted(self.chan_count.keys(), key=str)
        with contextlib.ExitStack() as st:
            esem = {e: st.enter_context(nc.semaphore("s_" + e)) for e in engs}
            csem = {c: st.enter_context(nc.semaphore("c_%d" % i)) for i, c in enumerate(chans)}
            block = st.enter_context(nc.Block())

            def run_engine(ename):
                def body(eng):
                    waited = {}
                    for op in ops:
                        if op["eng"] != ename:
                            continue
                        for d in sorted(op["deps"]):
                            p = ops[d]
                            if p["chan"] is not None:
                                key = ("c", p["chan"]); val = p["dmaval"]; sem = csem[p["chan"]]
                            else:
                                if p["eng"] == "pe" and ename == "pe" and op["chan"] is None and op["fn"] is not None:
                                    continue
                                key = ("e", p["eng"]); val = p["seqval"]; sem = esem[p["eng"]]
                            if waited.get(key, 0) >= val:
                                continue
                            eng.wait_ge(sem, val)
                            waited[key] = val
                        if op["fn"] is None:
                            continue
                        ins = op["fn"](eng)
                        if op["chan"] is not None:
                            ins.then_inc(csem[op["chan"]], 16)
                        elif op["signal"]:
                            ins.then_inc(esem[ename], 1)
                    for (e2, fo) in final_waits:
                        if e2 != ename:
                            continue
                        p = ops[fo]
                        if p["chan"] is not None:
                            eng.wait_ge(csem[p["chan"]], p["dmaval"])
                        else:
                            eng.wait_ge(esem[p["eng"]], p["seqval"])
                return body

            block.tensor(run_engine("pe"))
            block.scalar(run_engine("act"))
            block.vector(run_engine("dve"))
            block.gpsimd(run_engine("pool"))
            block.sync(run_engine("sp"))


class Arena:
    def __init__(self, t, nbytes):
        self.t = t
        self.n = nbytes
        self.off = 0
        self.mark = 0

    def take(self, nbytes, dt=F32, pattern=None, **kw):
        off = self.off
        self.off += (nbytes + 63) // 64 * 64
        assert self.off <= self.n, ("arena overflow", self.off, self.n)
        ap = self.t[:, off // 4:(off + nbytes) // 4]
        if dt != F32:
            ap = ap.bitcast(dt)
        if pattern:
            ap = ap.rearrange(pattern, **kw)
        return ap


def build_program(n_layers=L, do_moe=True, debug=False, skip_mem=False, skip_dil=False):
    nc = bass.Bass("TRN2", target_bir_lowering=False)

    def din(name, shape, dt=F32):
        return nc.dram_tensor(name, list(shape), dt, kind="ExternalInput").ap()

    x_d = din("x", [S, D])
    mem_d = din("mem", [MEM, D])
    mem_norm_d = din("mem_norm", [1, D])
    norm_mix_d = din("norm_mix", [L, D])
    norm_ffn_d = din("norm_ffn", [L, D])
    norm_final_d = din("norm_final", [1, D])
    w_in_d = din("w_in", [L, D, 2560])
    convw_d = din("convw_t", [L, 128, 9])
    w_mkv_d = din("w_mem_kv", [L, D, 512])
    w_out_d = din("w_out", [L, D, D])
    if do_moe:
        w_router_d = din("w_router", [L, D, E])
        w_gate_d = din("w_gate", [L, E, D, FF])
        w_up_d = din("w_up", [L, E, D, FF])
        w_down_d = din("w_down", [L, E, FF, D])
    identb_d = din("ident_bf", [128, 128], BF16)
    identf_d = din("ident_f", [128, 128])
    iota1_d = din("iota1", [128, CAP])
    etab_d = din("etab", [6, 128, ET_W], BF16)
    out_d = nc.dram_tensor("out", [S, D], F32, kind="ExternalOutput").ap()
    dbg = {}
    if debug:
        def dout(name, shape, dt=F32):
            dbg[name] = nc.dram_tensor(name, list(shape), dt, kind="ExternalOutput").ap()
        dout("d_xnT", [128, KC * S], BF16)
        dout("d_x_conv", [S, D])
        dout("d_convT", [128, 3 * S], BF16)
        dout("d_qT0", [128, S], BF16)
        dout("d_kT0", [128, S], BF16)
        dout("d_v", [128, NT * 6 * 65], BF16)
        dout("d_oT0", [64, 2 * S], BF16)
        dout("d_x_dil", [S, D])
        dout("d_osb", [65, 512])
        dout("d_psb", [128, 512], BF16)
        dout("d_ET", [128, 2 * ET_W], BF16)
        dout("d_esb", [128, 512], BF16)
        dout("d_S", [128, 512])
        dout("d_psb0", [128, 512], BF16)

    etab_nz = _etab().astype(np.float32) != 0
    PERS_BYTES = 114 * 1024 + 768
    PH_BYTES = 92 * 1024 - 768

    with contextlib.ExitStack() as st:
        pers_t = st.enter_context(nc.sbuf_tensor("pers", [128, PERS_BYTES // 4], F32))
        ph_t = st.enter_context(nc.sbuf_tensor("phase", [128, PH_BYTES // 4], F32))
        PS = [st.enter_context(nc.psum_tensor("ps%d" % i, [128, 512], F32)) for i in range(8)]
        PSB = [p[:].bitcast(BF16) for p in PS]

        pa = Arena(pers_t, PERS_BYTES)
        X = pa.take(NT * D * 4, F32, "p (i d) -> p i d", i=NT)
        A = pa.take(NT * D * 2, BF16)
        xnT = A.rearrange("p (k t) -> p k t", k=KC)
        xnTok = A.rearrange("p (i d) -> p i d", i=NT)
        ident_b = pa.take(128 * 2, BF16)
        ident_f = pa.take(128 * 4, F32)
        iota1 = pa.take(CAP * 4, F32)
        ones_f = pa.take(128 * 4, F32)
        memT = pa.take(KC * MEM * 2, BF16, "p (k t) -> p k t", k=KC)
        G = pa.take(D * 4, F32)
        junk = pa.take(D * 2, BF16)
        XN = [pa.take(D * 2, BF16), pa.take(D * 2, BF16)]
        ss = pa.take(NT * 4, F32)
        std = pa.take(NT * 4, F32)
        rstd = pa.take(NT * 4, F32)
        cw = pa.take(9 * 4, F32)
        aff_all = pa.take(NT * E * 4, F32, "p (i e) -> p i e", i=NT)
        sel_all = pa.take(NT * E * 4, F32, "p (i e) -> p i e", i=NT)
        sm = pa.take(16 * 4, F32)

        P = Prog()
        cnt = [0]

        def alt():
            cnt[0] += 1
            return "act" if cnt[0] % 2 == 0 else "dve"

        def copy_op(engname, out, in_):
            if engname == "act":
                return lambda e: e.copy(out, in_)
            return lambda e: e.tensor_copy(out, in_)

        def dump(name, src, reads, idx=None):
            if not debug or l != 0:
                return
            dst = dbg[name] if idx is None else dbg[name][idx]
            final_ops.append(P.add("sp", (lambda e: e.dma_start(out=dst, in_=src)), reads=reads, dma_chan="dbg", extra_deps=final_ops[-1:]))

        def dump_x(name):
            if not debug or l != 0:
                return
            for i in range(NT):
                final_ops.append(P.add("sp", (lambda e, i=i: e.dma_start(out=dbg[name][128 * i:128 * (i + 1), :], in_=X[:, i, :])),
                                       reads=[("X", i, 0), ("X", i, 1)], dma_chan="dbg", extra_deps=final_ops[-1:]))

        final_ops = []
        l = 0
        for i in range(NT):
            P.add("sp", (lambda e, i=i: e.dma_start(out=X[:, i, :], in_=x_d[128 * i:128 * (i + 1), :])),
                  writes=[("X", i, 0), ("X", i, 1)], dma_chan=("x", i))
        P.add("act", lambda e: e.dma_start(out=ident_b, in_=identb_d), writes=["ident_b"], dma_chan="c0")
        P.add("act", lambda e: e.dma_start(out=ident_f, in_=identf_d), writes=["ident_f"], dma_chan="c1")
        P.add("act", lambda e: e.dma_start(out=iota1, in_=iota1_d), writes=["iota1"], dma_chan="c2")
        P.add("dve", lambda e: e.memset(ones_f, 1.0), writes=["ones_f"])

        def load_gain(src_row):
            P.add("act", lambda e: e.dma_start(out=G, in_=src_row.partition_broadcast(128)), writes=["G"], dma_chan="g")

        def rms_stats(src_fn, keys_fn, n):
            for i in range(n):
                P.add("act", (lambda e, i=i: e.activation(junk, src_fn(i), AF.Square, accum_out=ss[:, i:i + 1])),
                      reads=keys_fn(i), writes=["junk", ("ss", i)])
            P.add("act", lambda e: e.activation(std[:, 0:n], ss[:, 0:n], AF.Sqrt, bias=eps_ap, scale=1.0 / D),
                  reads=[("ss", i) for i in range(n)] + ["eps"], writes=["std"])
            P.add("dve", lambda e: e.reciprocal(rstd[:, 0:n], std[:, 0:n]), reads=["std"], writes=["rstd"])

        eps_ap = pa.take(4, F32)
        P.add("dve", lambda e: e.memset(eps_ap, EPS), writes=["eps"])

        ph = Arena(ph_t, PH_BYTES)
        memx = ph.take(2 * D * 4, F32, "p (i d) -> p i d", i=2)
        for i in range(2):
            P.add("sp", (lambda e, i=i: e.dma_start(out=memx[:, i, :], in_=mem_d[128 * i:128 * (i + 1), :])),
                  writes=[("memx", i)], dma_chan=("mx", i))
        load_gain(mem_norm_d[0])
        rms_stats(lambda i: memx[:, i, :], lambda i: [("memx", i)], 2)
        for i in range(2):
            xb = XN[i % 2]
            P.add("dve", (lambda e, i=i, xb=xb: e.scalar_tensor_tensor(xb, memx[:, i, :], rstd[:, i:i + 1], G, ALU.mult, ALU.mult)),
                  reads=[("memx", i), "rstd", "G"], writes=[("XN", i % 2)])
            for c in range(KC):
                P.add("pe", (lambda e, c=c, xb=xb, i=i: e.transpose(PSB[i][:, 128 * c:128 * (c + 1)], xb[:, 128 * c:128 * (c + 1)], ident_b)),
                      reads=[("XN", i % 2), "ident_b"], writes=[("ps", i)])
            P.add("act", (lambda e, i=i: e.copy(memT[:, :, 128 * i:128 * (i + 1)], PSB[i].rearrange("p (k t) -> p k t", k=KC))),
                  reads=[("ps", i)], writes=[("memT", i)])
        P.barrier()

        for l in range(n_layers):
            ph = Arena(ph_t, PH_BYTES)
            WI = [ph.take(KC * 384 * 2, BF16, "p (k c) -> p k c", k=KC) for _ in range(2)]
            mix_base = ph.off
            w_in_v = w_in_d[l].rearrange("(k p) c -> p k c", p=128)
            w_out_v = w_out_d[l].rearrange("(k p) d -> p k d", p=128)

            load_gain(norm_mix_d[l])
            P.add("act", lambda e: e.dma_start(out=cw, in_=convw_d[l]), writes=["cw"], dma_chan="cw")

            def load_wi(buf, ranges):
                off = 0
                for (c0, n) in ranges:
                    P.add("pool", (lambda e, c0=c0, n=n, off=off: e.dma_start(out=WI[buf][:, :, off:off + n], in_=w_in_v[:, :, c0:c0 + n])),
                          writes=[("WI", buf, u) for u in range(off // 128, (off + n) // 128)], dma_chan=("wi", buf, off // 128))
                    off += n

            rms_stats(lambda i: X[:, i, :], lambda i: [("X", i, 0), ("X", i, 1)], NT)
            for i in range(NT):
                xb = XN[i % 2]
                pb = i % 2
                P.add("dve", (lambda e, i=i, xb=xb: e.scalar_tensor_tensor(xb, X[:, i, :], rstd[:, i:i + 1], G, ALU.mult, ALU.mult)),
                      reads=[("X", i, 0), ("X", i, 1), "rstd", "G"], writes=[("XN", i % 2)])
                for c in range(KC):
                    P.add("pe", (lambda e, c=c, xb=xb, pb=pb: e.transpose(PSB[pb][:, 128 * c:128 * (c + 1)], xb[:, 128 * c:128 * (c + 1)], ident_b)),
                          reads=[("XN", i % 2), "ident_b"], writes=[("ps", pb)])
                P.add("act", (lambda e, i=i, pb=pb: e.copy(xnT[:, :, 128 * i:128 * (i + 1)], PSB[pb].rearrange("p (k t) -> p k t", k=KC))),
                      reads=[("ps", pb)], writes=[("xnT", i // 4)])

            dump("d_xnT", A, [("xnT", tb) for tb in range(4)])

            def proj_feat(wi_ap, wi_keys, dst_fn, dst_key_fn, bank0, nbank=2, evac_scale=None):
                for tb in range(4):
                    bk = bank0 + tb % nbank
                    for k in range(KC):
                        P.add("pe", (lambda e, k=k, tb=tb, bk=bk: e.matmul(PS[bk][:, :], wi_ap[:, k, :], xnT[:, k, 512 * tb:512 * (tb + 1)],
                                                                         start=(k == 0), stop=(k == KC - 1))),
                              reads=wi_keys + [("xnT", tb)], writes=[("ps", bk)])
                    en = alt()
                    P.add(en, copy_op(en, dst_fn(tb), PS[bk][:, :]), reads=[("ps", bk)], writes=[dst_key_fn(tb)])

            ph.off = mix_base
            WOC = ph.take(3 * D * 2, BF16, "p (c d) -> p c d", c=3)
            P.add("pool", lambda e: e.dma_start(out=WOC, in_=w_out_v[:, 0:3, :]), writes=["WOC"], dma_chan="woc")
            u_pad = ph.take((S + 2) * 4 + 8, F32)
            tmpc = ph.take(S * 4, F32)
            bg_sb = ph.take(S * 4, F32)
            cg_sb = [ph.take(512 * 4, F32) for _ in range(2)]
            convT = ph.take(3 * S * 2, BF16, "p (c t) -> p c t", c=3)
            P.add("dve", lambda e: e.memset(u_pad[:, 0:1], 0.0), writes=["u_l"])
            P.add("dve", lambda e: e.memset(u_pad[:, S + 1:S + 2], 0.0), writes=["u_r"])
            for c in range(3):
                buf = c % 2
                load_wi(buf, [(128 * c, 128), (384 + 128 * c, 128), (768 + 128 * c, 128)])
                for tb in range(4):
                    for j in range(3):
                        bk = 2 + j
                        for k in range(KC):
                            P.add("pe", (lambda e, k=k, tb=tb, bk=bk, j=j, buf=buf: e.matmul(
                                PS[bk][:, :], WI[buf][:, k, 128 * j:128 * (j + 1)], xnT[:, k, 512 * tb:512 * (tb + 1)],
                                start=(k == 0), stop=(k == KC - 1))),
                                reads=[("WI", buf, j), ("xnT", tb)], writes=[("ps", bk)])
                    cb = cg_sb[tb % 2]
                    P.add("act", (lambda e, cb=cb: e.copy(cb, PS[4][:, :])), reads=[("ps", 4)], writes=[("cg", tb % 2)])
                    P.add("dve", (lambda e, cb=cb, tb=tb: e.tensor_tensor(u_pad[:, 1 + 512 * tb:1 + 512 * (tb + 1)], cb, PS[2][:, :], ALU.mult)),
                          reads=[("cg", tb % 2), ("ps", 2)], writes=[("u", tb)])
                    P.add("act", (lambda e, tb=tb: e.copy(bg_sb[:, 512 * tb:512 * (tb + 1)], PS[3][:, :])),
                          reads=[("ps", 3)], writes=[("bg", tb)])
                ukeys = [("u", tb) for tb in range(4)] + ["u_l", "u_r"]
                P.add("dve", (lambda e, c=c: e.tensor_scalar(tmpc, u_pad[:, 1:S + 1], cw[:, 3 * c + 1:3 * c + 2], None, ALU.mult)),
                      reads=ukeys + ["cw"], writes=["tmpc"])
                P.add("dve", (lambda e, c=c: e.scalar_tensor_tensor(tmpc, u_pad[:, 0:S], cw[:, 3 * c:3 * c + 1], tmpc, ALU.mult, ALU.add)),
                      reads=ukeys + ["cw", "tmpc"], writes=["tmpc"])
                P.add("dve", (lambda e, c=c: e.scalar_tensor_tensor(tmpc, u_pad[:, 2:S + 2], cw[:, 3 * c + 2:3 * c + 3], tmpc, ALU.mult, ALU.add)),
                      reads=ukeys + ["cw", "tmpc"], writes=["tmpc"])
                P.add("dve", (lambda e, c=c: e.tensor_tensor(convT[:, c, :], tmpc, bg_sb, ALU.mult)),
                      reads=["tmpc"] + [("bg", tb) for tb in range(4)], writes=[("convT", c)])
            for i in range(NT):
                for hf in range(2):
                    bk = 5 + (2 * i + hf) % 3
                    for c in range(3):
                        P.add("pe", (lambda e, i=i, hf=hf, c=c, bk=bk: e.matmul(
                            PS[bk][:, :], convT[:, c, 128 * i:128 * (i + 1)], WOC[:, c, 512 * hf:512 * (hf + 1)],
                            start=(c == 0), stop=(c == 2))),
                            reads=[("convT", c), "WOC"], writes=[("ps", bk)])
                    P.add("dve", (lambda e, i=i, hf=hf, bk=bk: e.tensor_tensor(
                        X[:, i, 512 * hf:512 * (hf + 1)], X[:, i, 512 * hf:512 * (hf + 1)], PS[bk][:, :], ALU.add)),
                        reads=[("ps", bk), ("X", i, hf)], writes=[("X", i, hf)])
            dump("d_convT", convT.rearrange("p c t -> p (c t)"), [("convT", c) for c in range(3)])
            dump_x("d_x_conv")
            P.barrier()

            pending = []

            import os as _os
            DEFER = _os.environ.get("K_DEFER", "1") == "1"

            def flush_pending():
                while pending:
                    pending.pop(0)()

            def attn_finish(par, hh, qb, oT_dst, att_key, r_row, o_sb):
                bo = 3 + par
                lrow = 64 if hh == 0 else 0
                olo = 0 if hh == 0 else 64
                lnl = o_sb[0]
                bcs = o_sb[1]
                P.add("act", (lambda e: e.activation(lnl[lrow:lrow + 1, :], PS[bo][lrow:lrow + 1, :], AF.Ln)), reads=[("ps", bo)], writes=["lnl"])
                P.add("act", (lambda e: e.activation(r_row[lrow:lrow + 1, :], lnl[lrow:lrow + 1, :], AF.Exp, scale=-1.0)), reads=["lnl"], writes=["r_row"])
                mo = 64 if hh == 0 else 128
                P.add("pe", lambda e: e.matmul(PS[5][0:mo, :], ones_f[lrow:lrow + 1, 0:mo], r_row[lrow:lrow + 1, :], start=True, stop=True),
                      reads=["r_row", "ones_f"], writes=[("ps", 5)])
                P.add("dve", (lambda e: e.tensor_copy(bcs[olo:olo + 64, :], PS[5][olo:olo + 64, :])), reads=[("ps", 5)], writes=["bcs"])
                P.add("dve", (lambda e: e.tensor_tensor(oT_dst[olo:olo + 64, 512 * qb:512 * (qb + 1)], PS[bo][olo:olo + 64, :], bcs[olo:olo + 64, :], ALU.mult)),
                      reads=[("ps", bo), "bcs"], writes=[att_key + (hh, qb)])

            def attn_outproj(oT_all, npair, att_key, WO_part):
                for i in range(NT):
                    for hf in range(2):
                        bk = 6 + (2 * i + hf) % 2
                        for hp_ in range(npair):
                            P.add("pe", (lambda e, hp_=hp_: e.matmul(
                                PS[bk][:, :], oT_all[:, hp_, 128 * i:128 * (i + 1)], WO_part[:, hp_, 512 * hf:512 * (hf + 1)],
                                start=(hp_ == 0), stop=(hp_ == npair - 1))),
                                reads=[(att_key, hp_, 0, i // 4), (att_key, hp_, 1, i // 4), "WO_part"], writes=[("ps", bk)])
                        P.add("dve", (lambda e: e.tensor_tensor(
                            X[:, i, 512 * hf:512 * (hf + 1)], X[:, i, 512 * hf:512 * (hf + 1)], PS[bk][:, :], ALU.add)),
                            reads=[("ps", bk), ("X", i, hf)], writes=[("X", i, hf)])

            def proj_q_pair(wi_ap, wi_key, qz_, zkey):
                for tb in range(4):
                    bk = 6 + tb % 2
                    for k in range(KC):
                        P.add("pe", (lambda e, k=k: e.matmul(PS[bk][:, :], wi_ap[:, k, :], xnT[:, k, 512 * tb:512 * (tb + 1)],
                                                             start=(k == 0), stop=(k == KC - 1))),
                              reads=[wi_key, ("xnT", tb)], writes=[("ps", bk)])
                    P.add("act", (lambda e: e.copy(qz_[0][0:64, 512 * tb:512 * (tb + 1)], PS[bk][0:64, :])),
                          reads=[("ps", bk), zkey + "0pad"], writes=[(zkey, 0, tb)])
                    P.add("dve", (lambda e: e.tensor_copy(qz_[1][64:128, 512 * tb:512 * (tb + 1)], PS[bk][64:128, :])),
                          reads=[("ps", bk), zkey + "1pad"], writes=[(zkey, 1, tb)])

            ph.off = mix_base
            VB = 192
            v_flat = ph.take(NT * 3 * VB * 2, BF16)
            v4 = v_flat.rearrange("p (i h c) -> p i h c", i=NT, h=3)
            qz = [ph.take(S * 2, BF16) for _ in range(2)]
            kT = [ph.take(S * 2, BF16) for _ in range(2)]
            ET = ph.take(2 * ET_W * 2, BF16, "p (h v) -> p h v", h=2)
            oT_all = ph.take(3 * S * 2, BF16, "p (h t) -> p h t", h=3)
            WOD = ph.take(3 * D * 2, BF16, "p (c d) -> p c d", c=3)
            e_sb = [ph.take(512 * 2, BF16) for _ in range(3)]
            p_sb = [ph.take(512 * 2, BF16) for _ in range(3)]
            o_sb = [ph.take(512 * 4, F32) for _ in range(2)]
            r_row = ph.take(512 * 4, F32)
            P.add("dve", lambda e: e.memset(qz[0][64:128, :], 0.0), writes=["qz0pad"])
            P.add("dve", lambda e: e.memset(qz[1][0:64, :], 0.0), writes=["qz1pad"])
            P.add("dve", lambda e: e.memset(v4[:, :, :, 64:128], 0.0), writes=["v_ones"])
            P.add("dve", lambda e: e.memset(v4[:, :, :, 64:65], 1.0), reads=["v_ones"], writes=["v_ones"])
            P.add("dve", lambda e: e.memset(v4[:, :, :, 96:97], 1.0), reads=["v_ones"], writes=["v_ones"])
            P.add("pool", lambda e: e.dma_start(out=WOD, in_=w_out_d[l][384:768, :].rearrange("(c p) d -> p c d", p=128)),
                  writes=["WO_part"], dma_chan="wod")
            load_wi(0, [(1920, 384)])
            for i in range(NT):
                bk = i % 2
                for k in range(KC):
                    P.add("pe", (lambda e, k=k: e.matmul(PS[bk][:, 0:384], xnT[:, k, 128 * i:128 * (i + 1)], WI[0][:, k, 0:384],
                                                         start=(k == 0), stop=(k == KC - 1))),
                          reads=[("WI", 0, 0), ("WI", 0, 1), ("WI", 0, 2), ("xnT", i // 4)], writes=[("ps", bk)])
                psv = PS[bk][:, 0:384].rearrange("p (h two c) -> p h two c", h=3, two=2)
                en = "act" if i % 2 == 0 else "dve"
                P.add(en, copy_op(en, v4[:, i, :, 0:64], psv[:, :, 0, :]), reads=[("ps", bk)], writes=[("vA", i)])
                P.add(en, copy_op(en, v4[:, i, :, 128:192], psv[:, :, 1, :]), reads=[("ps", bk)], writes=[("vB", i)])

            for hp in range(3):
                buf = (hp + 1) % 2
                load_wi(buf, [(1152 + 128 * hp, 128), (1536 + 128 * hp, 128)])
                P.add("sp", (lambda e: e.dma_start(out=ET, in_=etab_d[2 * hp:2 * hp + 2].rearrange("h p v -> p h v"))),
                      writes=["ET"], dma_chan="et")
                kb_ = kT[hp % 2]
                proj_q_pair(WI[buf][:, :, 0:128], ("WI", buf, 0), qz, "qz")
                proj_feat(WI[buf][:, :, 128:256], [("WI", buf, 1)], (lambda tb, kb_=kb_: kb_[:, 512 * tb:512 * (tb + 1)]),
                          (lambda tb, hp=hp: ("kT", hp % 2, tb)), 6)
                gi = 0
                for hh in range(2):
                    h = 2 * hp + hh
                    for qb in range(4):
                        kts = []
                        for kt in range(max(0, 4 * qb - 8), min(15, 4 * qb + 11) + 1):
                            v0 = ET_OFF - (128 * kt - 512 * qb)
                            if etab_nz[h][:, v0:v0 + 512].any():
                                kts.append(kt)
                        par = gi % 2
                        gi += 1
                        bo = 3 + par

                        def s_mm(kt, n):
                            bs = n % 3
                            P.add("pe", (lambda e: e.matmul(PS[bs][:, :], kb_[:, 128 * kt:128 * (kt + 1)],
                                                            qz[hh][:, 512 * qb:512 * (qb + 1)], start=True, stop=True)),
                                  reads=[("kT", hp % 2, kt // 4), ("qz", hh, qb), "qz%dpad" % hh], writes=[("ps", bs)])

                        s_mm(kts[0], 0)
                        if len(kts) > 1:
                            s_mm(kts[1], 1)
                        for n, kt in enumerate(kts):
                            bs = n % 3
                            v0 = ET_OFF - (128 * kt - 512 * qb)
                            P.add("act", (lambda e: e.activation(e_sb[bs], PS[bs][:, :], AF.Exp, scale=0.125)),
                                  reads=[("ps", bs)], writes=[("e_sb", bs)])
                            P.add("dve", (lambda e: e.tensor_tensor(p_sb[bs], e_sb[bs], ET[:, hh, v0:v0 + 512], ALU.mult)),
                                  reads=[("e_sb", bs), "ET"], writes=[("p_sb", bs)])
                            if n + 2 < len(kts):
                                s_mm(kts[n + 2], n + 2)
                            voff = (kt * 3 + hp) * VB + 64 * hh
                            P.add("pe", (lambda e: e.matmul(PS[bo][:, :], v_flat[:, voff:voff + 128], p_sb[bs],
                                                            start=(n == 0), stop=(n == len(kts) - 1))),
                                  reads=[("p_sb", bs), ("vA", kt), ("vB", kt), "v_ones"], writes=[("ps", bo)])
                            if n == min(1, len(kts) - 1):
                                flush_pending()
                        pending.append(lambda par=par, hh=hh, qb=qb, hp=hp: attn_finish(
                            par, hh, qb, oT_all[:, hp, :], ("oT", hp), r_row, o_sb))
                        if not DEFER:
                            flush_pending()
            flush_pending()
            attn_outproj(oT_all, 3, "oT", WOD)
            P.barrier()

            ph.off = mix_base
            if skip_mem:
                continue
            WMKV = ph.take(KC * 512 * 2, BF16, "p (k c) -> p k c", k=KC)
            kmT = ph.take(2 * MEM * 2, BF16, "p (h t) -> p h t", h=2)
            vm_flat = ph.take(2 * 2 * VB * 2, BF16)
            vm4 = vm_flat.rearrange("p (i h c) -> p i h c", i=2, h=2)
            qmz = [ph.take(S * 2, BF16) for _ in range(2)]
            oTm_all = ph.take(2 * S * 2, BF16, "p (h t) -> p h t", h=2)
            WOM = ph.take(2 * D * 2, BF16, "p (c d) -> p c d", c=2)
            p_sb = [ph.take(512 * 2, BF16) for _ in range(4)]
            o_sb = [ph.take(512 * 4, F32) for _ in range(2)]
            r_row = ph.take(512 * 4, F32)
            P.add("dve", lambda e: e.memset(qmz[0][64:128, :], 0.0), writes=["qmz0pad"])
            P.add("dve", lambda e: e.memset(qmz[1][0:64, :], 0.0), writes=["qmz1pad"])
            P.add("dve", lambda e: e.memset(vm4[:, :, :, 64:128], 0.0), writes=["vm_ones"])
            P.add("dve", lambda e: e.memset(vm4[:, :, :, 64:65], 1.0), reads=["vm_ones"], writes=["vm_ones"])
            P.add("dve", lambda e: e.memset(vm4[:, :, :, 96:97], 1.0), reads=["vm_ones"], writes=["vm_ones"])
            P.add("pool", lambda e: e.dma_start(out=WMKV, in_=w_mkv_d[l].rearrange("(k p) c -> p k c", p=128)), writes=["WMKV"], dma_chan="wmkv")
            P.add("pool", lambda e: e.dma_start(out=WOM, in_=w_out_d[l][768:1024, :].rearrange("(c p) d -> p c d", p=128)),
                  writes=["WO_part"], dma_chan="wod")
            for mp in range(2):
                for k in range(KC):
                    P.add("pe", (lambda e, k=k: e.matmul(PS[0][:, 0:MEM], WMKV[:, k, 128 * mp:128 * (mp + 1)], memT[:, k, :],
                                                         start=(k == 0), stop=(k == KC - 1))),
                          reads=["WMKV", ("memT", 0), ("memT", 1)], writes=[("ps", 0)])
                P.add("act", (lambda e: e.copy(kmT[:, mp, :], PS[0][:, 0:MEM])), reads=[("ps", 0)], writes=[("kmT", mp)])
            for i in range(2):
                for k in range(KC):
                    P.add("pe", (lambda e, k=k: e.matmul(PS[1][:, 0:256], memT[:, k, 128 * i:128 * (i + 1)], WMKV[:, k, 256:512],
                                                         start=(k == 0), stop=(k == KC - 1))),
                          reads=["WMKV", ("memT", 0), ("memT", 1)], writes=[("ps", 1)])
                psv = PS[1][:, 0:256].rearrange("p (h two c) -> p h two c", h=2, two=2)
                P.add("act", (lambda e: e.copy(vm4[:, i, :, 0:64], psv[:, :, 0, :])), reads=[("ps", 1)], writes=[("vmA", i)])
                P.add("act", (lambda e: e.copy(vm4[:, i, :, 128:192], psv[:, :, 1, :])), reads=[("ps", 1)], writes=[("vmB", i)])
            vmkeys = [("vmA", 0), ("vmA", 1), ("vmB", 0), ("vmB", 1), "vm_ones"]
            gi = 0
            for mp in range(2):
                buf = mp % 2
                load_wi(buf, [(2304 + 128 * mp, 128)])
                proj_q_pair(WI[buf][:, :, 0:128], ("WI", buf, 0), qmz, "qmz")
                for hh in range(2):
                    for qb in range(4):
                        par = gi % 2
                        sb0 = 2 * (gi % 2)
                        gi += 1
                        bo = 3 + par
                        for kt in range(2):
                            bs = sb0 + kt
                            P.add("pe", (lambda e: e.matmul(PS[bs if bs < 3 else 7][:, :], kmT[:, mp, 128 * kt:128 * (kt + 1)],
                                                            qmz[hh][:, 512 * qb:512 * (qb + 1)], start=True, stop=True)),
                                  reads=[("kmT", mp), ("qmz", hh, qb), "qmz%dpad" % hh], writes=[("ps", bs if bs < 3 else 7)])
                        for kt in range(2):
                            bs = sb0 + kt
                            pbk = bs if bs < 3 else 7
                            P.add("act", (lambda e: e.activation(p_sb[bs], PS[pbk][:, :], AF.Exp, scale=0.125)),
                                  reads=[("ps", pbk)], writes=[("p_sb", bs)])
                            if kt == 0:
                                flush_pending()
                            voff = (kt * 2 + mp) * VB + 64 * hh
                            P.add("pe", (lambda e: e.matmul(PS[bo][:, :], vm_flat[:, voff:voff + 128], p_sb[bs], start=(kt == 0), stop=(kt == 1))),
                                  reads=[("p_sb", bs)] + vmkeys, writes=[("ps", bo)])
                        pending.append(lambda par=par, hh=hh, qb=qb, mp=mp: attn_finish(
                            par, hh, qb, oTm_all[:, mp, :], ("oTm", mp), r_row, o_sb))
            flush_pending()
            attn_outproj(oTm_all, 2, "oTm", WOM)
            P.barrier()

            if not do_moe:
                continue
            ph = Arena(ph_t, PH_BYTES)
            WG = [ph.take(KC * 512 * 2, BF16, "p (k f) -> p k f", k=KC) for _ in range(2)]
            WU = [ph.take(KC * 512 * 2, BF16, "p (k f) -> p k f", k=KC) for _ in range(2)]
            WD = [ph.take(4 * D * 2, BF16, "p (c d) -> p c d", c=4) for _ in range(2)]
            WR = ph.take(KC * E * 4, F32, "p (k e) -> p k e", k=KC)
            moe_base = ph.off

            def load_expert_w(e_, fs):
                buf = (e_ * 4 + fs) % 2
                P.add("pool", (lambda e, e_=e_, fs=fs, buf=buf: e.dma_start(
                    out=WG[buf], in_=w_gate_d[l, e_].rearrange("(k p) f -> p k f", p=128)[:, :, 512 * fs:512 * (fs + 1)])),
                    writes=[("WG", buf)], dma_chan=("wg", buf))
                P.add("pool", (lambda e, e_=e_, fs=fs, buf=buf: e.dma_start(
                    out=WU[buf], in_=w_up_d[l, e_].rearrange("(k p) f -> p k f", p=128)[:, :, 512 * fs:512 * (fs + 1)])),
                    writes=[("WU", buf)], dma_chan=("wu", buf))
                P.add("pool", (lambda e, e_=e_, fs=fs, buf=buf: e.dma_start(
                    out=WD[buf], in_=w_down_d[l, e_][512 * fs:512 * (fs + 1), :].rearrange("(c p) d -> p c d", p=128))),
                    writes=[("WD", buf)], dma_chan=("wd", buf))

            load_gain(norm_ffn_d[l])
            P.add("act", lambda e: e.dma_start(out=WR, in_=w_router_d[l].rearrange("(k p) e -> p k e", p=128)), writes=["WR"], dma_chan="wr")
            load_expert_w(0, 0)
            load_expert_w(0, 1)

            affT = ph.take(S * 4, F32)
            cjunk = ph.take(S * 2, BF16)
            maskT = ph.take(S * 4, F32)
            r_base = ph.off
            xnf = [ph.take(D * 4, F32) for _ in range(2)]
            xnTf = [ph.take(KC * 128 * 4, F32, "p (k t) -> p k t", k=KC) for _ in range(2)]
            ph.off = r_base
            cums = ph.take(S * 4, F32)
            selp = ph.take(S * 4, F32)
            lg_sb = [ph.take(128 * 4, F32) for _ in range(2)]
            tq = ph.take(8 * 4, F32)
            rms_stats(lambda i: X[:, i, :], lambda i: [("X", i, 0), ("X", i, 1)], NT)

            def softmax_tile(i):
                lb = 3 + i % 2
                P.add("dve", lambda e: e.reduce_max(sm[:, 0:1], PS[lb][:, 0:E], axis=AX.X), reads=[("ps", lb)], writes=["sm0"])
                P.add("dve", lambda e: e.tensor_scalar(sm[:, 1:2], sm[:, 0:1], -1.0, None, ALU.mult), reads=["sm0"], writes=["sm1"])
                P.add("act", (lambda e: e.activation(aff_all[:, i, :], PS[lb][:, 0:E], AF.Exp, bias=sm[:, 1:2], scale=1.0, accum_out=sm[:, 2:3])),
                      reads=[("ps", lb), "sm1"], writes=[("aff", i), "sm2"])
                P.add("dve", lambda e: e.reciprocal(sm[:, 3:4], sm[:, 2:3]), reads=["sm2"], writes=["sm3"])
                P.add("dve", (lambda e: e.tensor_scalar(aff_all[:, i, :], aff_all[:, i, :], sm[:, 3:4], None, ALU.mult)),
                      reads=[("aff", i), "sm3"], writes=[("aff", i)])

            for i in range(NT):
                xf = xnf[i % 2]
                xt = xnTf[i % 2]
                lb = 3 + i % 2
                P.add("dve", (lambda e: e.scalar_tensor_tensor(xf, X[:, i, :], rstd[:, i:i + 1], G, ALU.mult, ALU.mult)),
                      reads=[("X", i, 0), ("X", i, 1), "rstd", "G"], writes=[("xnf", i % 2)])
                P.add("act", (lambda e: e.copy(xnTok[:, i, :], xf)), reads=[("xnf", i % 2)], writes=[("xnTok", i)])
                for c in range(KC):
                    bk = c // 4
                    P.add("pe", (lambda e, c=c, bk=bk: e.transpose(PS[bk][:, 128 * (c % 4):128 * (c % 4 + 1)], xf[:, 128 * c:128 * (c + 1)], ident_f)),
                          reads=[("xnf", i % 2), "ident_f"], writes=[("ps", bk)])
                P.add("act", lambda e: e.copy(xt[:, 0:4, :], PS[0][:, :].rearrange("p (k t) -> p k t", k=4)), reads=[("ps", 0)], writes=[("xnTf", i % 2, 0)])
                P.add("dve", lambda e: e.tensor_copy(xt[:, 4:8, :], PS[1][:, :].rearrange("p (k t) -> p k t", k=4)), reads=[("ps", 1)], writes=[("xnTf", i % 2, 1)])
                for k in range(KC):
                    P.add("pe", (lambda e, k=k: e.matmul(PS[2][0:E, 0:128], WR[:, k, :], xt[:, k, :], start=(k == 0), stop=(k == KC - 1))),
                          reads=[("xnTf", i % 2, k // 4), "WR"], writes=[("ps", 2)])
                P.add("act", (lambda e: e.copy(lg_sb[i % 2][0:E, :], PS[2][0:E, 0:128])), reads=[("ps", 2)], writes=[("lg", i % 2)])
                P.add("pe", (lambda e: e.transpose(PS[lb][:, 0:E], lg_sb[i % 2][0:E, :], ident_f[0:E, 0:E])),
                      reads=[("lg", i % 2), "ident_f"], writes=[("ps", lb)])
                if i > 0:
                    softmax_tile(i - 1)
            softmax_tile(NT - 1)
            for i in range(NT):
                bk = 5 + (i // 4) % 2
                P.add("pe", (lambda e, i=i, bk=bk: e.transpose(PS[bk][0:E, 128 * (i % 4):128 * (i % 4 + 1)], aff_all[:, i, :], ident_f)),
                      reads=[("aff", i), "ident_f"], writes=[("ps", bk)])
                if i % 4 == 3:
                    j = i // 4
                    P.add("act", (lambda e, j=j, bk=bk: e.copy(affT[0:E, 512 * j:512 * (j + 1)], PS[bk][0:E, :])), reads=[("ps", bk)], writes=[("affT", j)])
            P.barrier()
            affT_keys = [("affT", j) for j in range(4)]
            P.add("dve", lambda e: e.memset(tq[0:E, 0:1], 0.0), writes=["tq_t"])
            for kbit in range(1, 31):
                step = 2.0 ** (-kbit)
                P.add("dve", lambda e: e.tensor_scalar(tq[0:E, 1:2], tq[0:E, 0:1], step, None, ALU.add), reads=["tq_t"], writes=["tq_c"])
                P.add("dve", lambda e: e.tensor_scalar(cjunk[0:E, :], affT[0:E, :], tq[0:E, 1:2], None, ALU.is_ge, ALU.add, accum_out=tq[0:E, 2:3]),
                      reads=affT_keys + ["tq_c"], writes=["cjunk", "tq_n"])
                P.add("dve", lambda e: e.tensor_scalar(tq[0:E, 3:4], tq[0:E, 2:3], CAP - 0.5, step, ALU.is_ge, ALU.mult), reads=["tq_n"], writes=["tq_g"])
                P.add("dve", lambda e: e.tensor_tensor(tq[0:E, 0:1], tq[0:E, 0:1], tq[0:E, 3:4], ALU.add), reads=["tq_t", "tq_g"], writes=["tq_t"])
            P.add("dve", lambda e: e.tensor_scalar(maskT[0:E, :], affT[0:E, :], tq[0:E, 0:1], None, ALU.is_ge), reads=affT_keys + ["tq_t"], writes=["maskT"])
            P.add("dve", lambda e: e.tensor_tensor_scan(cums[0:E, :], maskT[0:E, :], maskT[0:E, :], 0.0, ALU.add, ALU.max),
                  reads=["maskT"], writes=["cums"])
            P.add("dve", lambda e: e.tensor_tensor(selp[0:E, :], cums[0:E, :], maskT[0:E, :], ALU.mult), reads=["cums", "maskT"], writes=["selp"])
            for i in range(NT):
                P.add("pe", (lambda e, i=i: e.transpose(PS[7][:, E * i:E * (i + 1)], selp[0:E, 128 * i:128 * (i + 1)], ident_f[0:E, 0:E])),
                      reads=["selp", "ident_f"], writes=[("ps", 7)])
            P.add("act", lambda e: e.copy(sel_all, PS[7][:, 0:NT * E].rearrange("p (i e) -> p i e", i=NT)), reads=[("ps", 7)], writes=["sel"])
            P.barrier()

            ph.off = moe_base
            Ssel = ph.take(NT * CAP * 2, BF16, "p (i j) -> p i j", i=NT)
            Sg = ph.take(8 * CAP * 2, BF16, "p (i j) -> p i j", i=8)
            SgT = [ph.take(2 * S * 2, BF16, "p (j t) -> p j t", j=2) for _ in range(2)]
            xgT = ph.take(KC * CAP * 2, BF16, "p (k j) -> p k j", k=KC)
            hT = [ph.take(4 * CAP * 2, BF16, "p (c j) -> p c j", c=4) for _ in range(2)]
            sg_sb = [ph.take(CAP * 4, F32) for _ in range(2)]
            y_sb = ph.take(2 * D * 2, BF16, "p (j d) -> p j d", j=2)
            xg_keys = [("xgT", c) for c in range(KC)]

            def scatter_unit(es, u):
                i, hf = u // 2, u % 2
                bk = 6 + u % 2
                for jc in range(2):
                    P.add("pe", (lambda e, jc=jc: e.matmul(
                        PS[bk][:, :], SgT[es % 2][:, jc, 128 * i:128 * (i + 1)], y_sb[:, jc, 512 * hf:512 * (hf + 1)], start=(jc == 0), stop=(jc == 1))),
                        reads=[("SgT", es % 2, jc, i // 8), ("y", jc, hf)], writes=[("ps", bk)])
                P.add("dve", (lambda e: e.tensor_tensor(
                    X[:, i, 512 * hf:512 * (hf + 1)], X[:, i, 512 * hf:512 * (hf + 1)], PS[bk][:, :], ALU.add)),
                    reads=[("ps", bk), ("X", i, hf)], writes=[("X", i, hf)])

            for e_ in range(E):
                for half in range(2):
                    for ii in range(8):
                        i = 8 * half + ii
                        P.add("dve", (lambda e, i=i: e.tensor_scalar(Ssel[:, i, :], iota1, sel_all[:, i, e_:e_ + 1], None, ALU.is_equal)),
                              reads=["iota1", "sel"], writes=[("Ssel", i)])
                        P.add("dve", (lambda e, i=i, ii=ii: e.tensor_scalar(Sg[:, ii, :], iota1, sel_all[:, i, e_:e_ + 1], aff_all[:, i, e_:e_ + 1],
                                                                            ALU.is_equal, ALU.mult)),
                              reads=["iota1", "sel", ("aff", i)], writes=[("Sg", ii)])
                    for jc in range(2):
                        bk = 6 + jc
                        for ii in range(8):
                            P.add("pe", (lambda e, ii=ii, jc=jc, bk=bk: e.transpose(
                                PSB[bk][:, 128 * ii:128 * (ii + 1)], Sg[:, ii, 128 * jc:128 * (jc + 1)], ident_b)),
                                reads=[("Sg", ii), "ident_b"], writes=[("ps", bk)])
                        en = alt()
                        P.add(en, copy_op(en, SgT[e_ % 2][:, jc, 1024 * half:1024 * (half + 1)], PSB[bk]), reads=[("ps", bk)],
                              writes=[("SgT", e_ % 2, jc, half)])
                for c in range(KC):
                    bk = 6 + c % 2
                    for i in range(NT):
                        P.add("pe", (lambda e, c=c, i=i, bk=bk: e.matmul(PS[bk][:, 0:CAP], xnTok[:, i, 128 * c:128 * (c + 1)], Ssel[:, i, :],
                                                                         start=(i == 0), stop=(i == NT - 1))),
                              reads=[("xnTok", i), ("Ssel", i)], writes=[("ps", bk)])
                    en = alt()
                    P.add(en, copy_op(en, xgT[:, c, :], PS[bk][:, 0:CAP]), reads=[("ps", bk)], writes=[("xgT", c)])

                def gate_up(f):
                    fs, fc = f // 4, f % 4
                    buf = (e_ * 4 + fs) % 2
                    bk = 4 + f % 2
                    for k in range(KC):
                        P.add("pe", (lambda e, k=k: e.matmul(PS[bk][:, 0:CAP], WG[buf][:, k, 128 * fc:128 * (fc + 1)], xgT[:, k, :],
                                                             start=(k == 0), stop=(k == KC - 1))),
                              reads=[("WG", buf)] + xg_keys, writes=[("ps", bk)])
                    for k in range(KC):
                        P.add("pe", (lambda e, k=k: e.matmul(PS[bk][:, CAP:2 * CAP], WU[buf][:, k, 128 * fc:128 * (fc + 1)], xgT[:, k, :],
                                                             start=(k == 0), stop=(k == KC - 1))),
                              reads=[("WU", buf)] + xg_keys, writes=[("ps", bk)])

                gate_up(0)
                for f in range(16):
                    fs, fc = f // 4, f % 4
                    buf = (e_ * 4 + fs) % 2
                    bk = 4 + f % 2
                    hb = hT[fs % 2]
                    sgb = sg_sb[f % 2]
                    P.add("act", (lambda e: e.activation(sgb, PS[bk][:, 0:CAP], AF.Silu)), reads=[("ps", bk)], writes=[("sg", f % 2)])
                    P.add("dve", (lambda e: e.tensor_tensor(hb[:, fc, :], sgb, PS[bk][:, CAP:2 * CAP], ALU.mult)),
                          reads=[("sg", f % 2), ("ps", bk)], writes=[("hT", fs % 2, fc)])
                    if f + 1 < 16:
                        gate_up(f + 1)
                    for jc in range(2):
                        for hf in range(2):
                            by = 2 * jc + hf
                            P.add("pe", (lambda e, jc=jc, hf=hf, by=by: e.matmul(
                                PS[by][:, :], hb[:, fc, 128 * jc:128 * (jc + 1)], WD[buf][:, fc, 512 * hf:512 * (hf + 1)],
                                start=(f == 0), stop=(f == 15))),
                                reads=[("hT", fs % 2, fc), ("WD", buf)], writes=[("ps", by)])
                    if e_ > 0:
                        scatter_unit(e_ - 1, 2 * f)
                        scatter_unit(e_ - 1, 2 * f + 1)
                    if fc == 3:
                        nxt = e_ * 4 + fs + 2
                        if nxt < E * 4:
                            load_expert_w(nxt // 4, nxt % 4)
                for jc in range(2):
                    for hf in range(2):
                        by = 2 * jc + hf
                        en = alt()
                        P.add(en, copy_op(en, y_sb[:, jc, 512 * hf:512 * (hf + 1)], PS[by][:, :]), reads=[("ps", by)], writes=[("y", jc, hf)])
            for u in range(32):
                scatter_unit(E - 1, u)
            P.barrier()

        ph = Arena(ph_t, PH_BYTES)
        ob = [ph.take(D * 4, F32) for _ in range(4)]
        if debug:
            for i in range(NT):
                final_ops.append(P.add("sp", (lambda e, i=i: e.dma_start(out=out_d[128 * i:128 * (i + 1), :], in_=X[:, i, :])),
                                       reads=[("X", i, 0), ("X", i, 1)], dma_chan="dbg", extra_deps=final_ops[-1:]))
        else:
            load_gain(norm_final_d[0])
            rms_stats(lambda i: X[:, i, :], lambda i: [("X", i, 0), ("X", i, 1)], NT)
            for i in range(NT):
                P.add("dve", (lambda e, i=i: e.scalar_tensor_tensor(ob[i % 4], X[:, i, :], rstd[:, i:i + 1], G, ALU.mult, ALU.mult)),
                      reads=[("X", i, 0), ("X", i, 1), "rstd", "G"], writes=[("ob", i % 4)])
                final_ops.append(P.add("sp", (lambda e, i=i: e.dma_start(out=out_d[128 * i:128 * (i + 1), :], in_=ob[i % 4])),
                                       reads=[("ob", i % 4)], writes=[("obd", i % 4)], dma_chan=("o", i % 4)))
        P.emit(nc, final_waits=[("sp", o) for o in final_ops])
    return nc


def _etab():
    t = np.zeros((6, 128, ET_W), np.float64)
    p = np.arange(128)[:, None]
    v = np.arange(ET_W)[None, :]
    d = p - v + ET_OFF
    ad = np.abs(d)
    m = (ad <= 64).astype(np.float64) + ((d % 4 == 0) & (ad <= 256)) + ((d % 16 == 0) & (ad <= 1024))
    for h in range(6):
        slope = 2.0 ** (-8.0 * (h + 1) / 6)
        t[h] = m * np.exp(-slope * ad)
    return t.astype(np.float32).astype(ml_dtypes.bfloat16)


def _consts():
    return {
        "ident_bf": np.eye(128, dtype=np.float32).astype(ml_dtypes.bfloat16),
        "ident_f": np.eye(128, dtype=np.float32),
        "iota1": np.tile(np.arange(1, CAP + 1, dtype=np.float32)[None, :], (128, 1)),
        "etab": _etab(),
    }


def make_in_maps(x, mem, mem_norm, norm_mix, w_in, conv_w, w_mem_kv, w_out, norm_ffn,
                 w_router, w_gate, w_up, w_down, norm_final):
    f = lambda a: np.ascontiguousarray(np.asarray(a, dtype=np.float32))
    conv_w = f(conv_w)
    convw_t = np.ascontiguousarray(conv_w.reshape(L, 3, 3, 128).transpose(0, 3, 2, 1).reshape(L, 128, 9))
    shared = {
        "mem_norm": f(mem_norm).reshape(1, D), "norm_mix": f(norm_mix), "norm_ffn": f(norm_ffn),
        "norm_final": f(norm_final).reshape(1, D), "w_in": f(w_in), "convw_t": convw_t,
        "w_mem_kv": f(w_mem_kv), "w_out": f(w_out), "w_router": f(w_router),
        "w_gate": f(w_gate), "w_up": f(w_up), "w_down": f(w_down),
    }
    shared.update(_consts())
    x = f(x)
    mem = f(mem)
    return [dict(shared, x=x[b], mem=mem[b]) for b in range(N_CORES)]


def kernel(x, mem, mem_norm, norm_mix, w_in, conv_w, w_mem_kv, w_out, norm_ffn,
           w_router, w_gate, w_up, w_down, norm_final):
    in_maps = make_in_maps(x, mem, mem_norm, norm_mix, w_in, conv_w, w_mem_kv, w_out, norm_ffn,
                           w_router, w_gate, w_up, w_down, norm_final)
    nc = build_program()
    res = run_bass_kernel_spmd(nc, in_maps, core_ids=list(range(N_CORES)))
    return np.stack([np.asarray(r["out"], dtype=np.float32) for r in res.results], axis=0)
```

```python
import contextlib
import numpy as np
import ml_dtypes
import concourse.bass as bass
import concourse.mybir as mybir
from concourse.bass_utils import run_bass_kernel_spmd

F32 = mybir.dt.float32
BF16 = mybir.dt.bfloat16
AF = mybir.ActivationFunctionType
ALU = mybir.AluOpType
AX = mybir.AxisListType

D = 1024
S = 2048
NT = 16
KC = 8
L = 2
E = 16
FF = 2048
CAP = 256
MEM = 256
EPS = 1e-6
ET_W = 2944
ET_OFF = 1408
N_CORES = 8


class _Rec:
    def __init__(self):
        self.call = None

    def __getattr__(self, name):
        def f(*a, **k):
            self.call = (name, a, k)
            return self
        return f


class Prog:
    def __init__(self):
        self.ops = []
        self.last_writer = {}
        self.readers = {}
        self.chan_count = {}
        self.last_eng = {}
        self.last_chan = {}

    def add(self, eng, fn, reads=(), writes=(), dma_chan=None, extra_deps=()):
        idx = len(self.ops)
        deps = set(extra_deps)
        for k in reads:
            w = self.last_writer.get(k)
            if w is not None:
                deps.add(w)
        for k in writes:
            w = self.last_writer.get(k)
            if w is not None:
                deps.add(w)
            for r in self.readers.get(k, {}).values():
                deps.add(r)
        deps.discard(idx)
        rkey = eng if dma_chan is None else ("dma", idx)
        for k in reads:
            self.readers.setdefault(k, {})[rkey] = idx
        for k in writes:
            self.last_writer[k] = idx
            self.readers[k] = {}
        if fn is not None:
            rec = _Rec()
            fn(rec)
            call = rec.call
            assert call is not None
            fn = (lambda e, call=call: getattr(e, call[0])(*call[1], **call[2]))
        op = dict(eng=eng, fn=fn, deps=deps, chan=dma_chan, signal=False)
        if dma_chan is not None:
            self.chan_count[dma_chan] = self.chan_count.get(dma_chan, 0) + 16
            op["dmaval"] = self.chan_count[dma_chan]
            self.last_chan[dma_chan] = idx
        elif fn is not None:
            self.last_eng[eng] = idx
        self.ops.append(op)
        return idx

    def barrier(self):
        deps = set(self.last_eng.values()) | set(self.last_chan.values())
        for e in ["pe", "act", "dve", "pool", "sp"]:
            self.add(e, None, extra_deps=deps)

    def emit(self, nc, final_waits=()):
        ops = self.ops
        for op in ops:
            for d in op["deps"]:
                p = ops[d]
                if p["chan"] is None:
                    if p["eng"] == "pe" and op["eng"] == "pe" and op["chan"] is None and op["fn"] is not None:
                        continue
                    p["signal"] = True
        for (_, fo) in final_waits:
            if ops[fo]["chan"] is None:
                ops[fo]["signal"] = True
        engs = ["pe", "act", "dve", "pool", "sp"]
        seq = {e: 0 for e in engs}
        for op in ops:
            if op["chan"] is None and op["signal"]:
                seq[op["eng"]] += 1
                op["seqval"] = seq[op["eng"]]
        chans = sorted(self.chan_count.keys(), key=str)
        with contextlib.ExitStack() as st:
            esem = {e: st.enter_context(nc.semaphore("s_" + e)) for e in engs}
            csem = {c: st.enter_context(nc.semaphore("c_%d" % i)) for i, c in enumerate(chans)}
            block = st.enter_context(nc.Block())

            def run_engine(ename):
                def body(eng):
                    waited = {}
                    for op in ops:
                        if op["eng"] != ename:
                            continue
                        for d in sorted(op["deps"]):
                            p = ops[d]
                            if p["chan"] is not None:
                                key = ("c", p["chan"]); val = p["dmaval"]; sem = csem[p["chan"]]
                            else:
                                if p["eng"] == "pe" and ename == "pe" and op["chan"] is None and op["fn"] is not None:
                                    continue
                                key = ("e", p["eng"]); val = p["seqval"]; sem = esem[p["eng"]]
                            if waited.get(key, 0) >= val:
                                continue
                            eng.wait_ge(sem, val)
                            waited[key] = val
                        if op["fn"] is None:
                            continue
                        ins = op["fn"](eng)
                        if op["chan"] is not None:
                            ins.then_inc(csem[op["chan"]], 16)
                        elif op["signal"]:
                            ins.then_inc(esem[ename], 1)
                    for (e2, fo) in final_waits:
                        if e2 != ename:
                            continue
                        p = ops[fo]
                        if p["chan"] is not None:
                            eng.wait_ge(csem[p["chan"]], p["dmaval"])
                        else:
                            eng.wait_ge(esem[p["eng"]], p["seqval"])
                return body

            block.tensor(run_engine("pe"))
            block.scalar(run_engine("act"))
            block.vector(run_engine("dve"))
            block.gpsimd(run_engine("pool"))
            block.sync(run_engine("sp"))


class Arena:
    def __init__(self, t, nbytes):
        self.t = t
        self.n = nbytes
        self.off = 0
        self.mark = 0

    def take(self, nbytes, dt=F32, pattern=None, **kw):
        off = self.off
        self.off += (nbytes + 63) // 64 * 64
        assert self.off <= self.n, ("arena overflow", self.off, self.n)
        ap = self.t[:, off // 4:(off + nbytes) // 4]
        if dt != F32:
            ap = ap.bitcast(dt)
        if pattern:
            ap = ap.rearrange(pattern, **kw)
        return ap


def build_program(n_layers=L, do_moe=True, debug=False, skip_mem=False, skip_dil=False):
    nc = bass.Bass("TRN2", target_bir_lowering=False)

    def din(name, shape, dt=F32):
        return nc.dram_tensor(name, list(shape), dt, kind="ExternalInput").ap()

    x_d = din("x", [S, D])
    mem_d = din("mem", [MEM, D])
    mem_norm_d = din("mem_norm", [1, D])
    norm_mix_d = din("norm_mix", [L, D])
    norm_ffn_d = din("norm_ffn", [L, D])
    norm_final_d = din("norm_final", [1, D])
    w_in_d = din("w_in", [L, D, 2560])
    convw_d = din("convw_t", [L, 128, 9])
    w_mkv_d = din("w_mem_kv", [L, D, 512])
    w_out_d = din("w_out", [L, D, D])
    if do_moe:
        w_router_d = din("w_router", [L, D, E])
        w_gate_d = din("w_gate", [L, E, D, FF])
        w_up_d = din("w_up", [L, E, D, FF])
        w_down_d = din("w_down", [L, E, FF, D])
    identb_d = din("ident_bf", [128, 128], BF16)
    identf_d = din("ident_f", [128, 128])
    iota1_d = din("iota1", [128, CAP])
    etab_d = din("etab", [6, 128, ET_W], BF16)
    out_d = nc.dram_tensor("out", [S, D], F32, kind="ExternalOutput").ap()
    dbg = {}
    if debug:
        def dout(name, shape, dt=F32):
            dbg[name] = nc.dram_tensor(name, list(shape), dt, kind="ExternalOutput").ap()
        dout("d_xnT", [128, KC * S], BF16)
        dout("d_x_conv", [S, D])
        dout("d_convT", [128, 3 * S], BF16)
        dout("d_qT0", [128, S], BF16)
        dout("d_kT0", [128, S], BF16)
        dout("d_v", [128, NT * 6 * 65], BF16)
        dout("d_oT0", [64, 2 * S], BF16)
        dout("d_x_dil", [S, D])
        dout("d_osb", [65, 512])
        dout("d_psb", [128, 512], BF16)
        dout("d_ET", [128, 2 * ET_W], BF16)
        dout("d_esb", [128, 512], BF16)
        dout("d_S", [128, 512])
        dout("d_psb0", [128, 512], BF16)

    etab_nz = _etab().astype(np.float32) != 0
    PERS_BYTES = 114 * 1024 + 768
    PH_BYTES = 92 * 1024 - 768

    with contextlib.ExitStack() as st:
        pers_t = st.enter_context(nc.sbuf_tensor("pers", [128, PERS_BYTES // 4], F32))
        ph_t = st.enter_context(nc.sbuf_tensor("phase", [128, PH_BYTES // 4], F32))
        PS = [st.enter_context(nc.psum_tensor("ps%d" % i, [128, 512], F32)) for i in range(8)]
        PSB = [p[:].bitcast(BF16) for p in PS]

        pa = Arena(pers_t, PERS_BYTES)
        X = pa.take(NT * D * 4, F32, "p (i d) -> p i d", i=NT)
        A = pa.take(NT * D * 2, BF16)
        xnT = A.rearrange("p (k t) -> p k t", k=KC)
        xnTok = A.rearrange("p (i d) -> p i d", i=NT)
        ident_b = pa.take(128 * 2, BF16)
        ident_f = pa.take(128 * 4, F32)
        iota1 = pa.take(CAP * 4, F32)
        ones_f = pa.take(128 * 4, F32)
        memT = pa.take(KC * MEM * 2, BF16, "p (k t) -> p k t", k=KC)
        G = pa.take(D * 4, F32)
        junk = pa.take(D * 2, BF16)
        XN = [pa.take(D * 2, BF16), pa.take(D * 2, BF16)]
        ss = pa.take(NT * 4, F32)
        std = pa.take(NT * 4, F32)
        rstd = pa.take(NT * 4, F32)
        cw = pa.take(9 * 4, F32)
        aff_all = pa.take(NT * E * 4, F32, "p (i e) -> p i e", i=NT)
        sel_all = pa.take(NT * E * 4, F32, "p (i e) -> p i e", i=NT)
        sm = pa.take(16 * 4, F32)

        P = Prog()
        cnt = [0]

        def alt():
            cnt[0] += 1
            return "act" if cnt[0] % 2 == 0 else "dve"

        def copy_op(engname, out, in_):
            if engname == "act":
                return lambda e: e.copy(out, in_)
            return lambda e: e.tensor_copy(out, in_)

        def dump(name, src, reads, idx=None):
            if not debug or l != 0:
                return
            dst = dbg[name] if idx is None else dbg[name][idx]
            final_ops.append(P.add("sp", (lambda e: e.dma_start(out=dst, in_=src)), reads=reads, dma_chan="dbg", extra_deps=final_ops[-1:]))

        def dump_x(name):
            if not debug or l != 0:
                return
            for i in range(NT):
                final_ops.append(P.add("sp", (lambda e, i=i: e.dma_start(out=dbg[name][128 * i:128 * (i + 1), :], in_=X[:, i, :])),
                                       reads=[("X", i, 0), ("X", i, 1)], dma_chan="dbg", extra_deps=final_ops[-1:]))

        final_ops = []
        l = 0
        for i in range(NT):
            P.add("sp", (lambda e, i=i: e.dma_start(out=X[:, i, :], in_=x_d[128 * i:128 * (i + 1), :])),
                  writes=[("X", i, 0), ("X", i, 1)], dma_chan=("x", i))
        P.add("act", lambda e: e.dma_start(out=ident_b, in_=identb_d), writes=["ident_b"], dma_chan="c0")
        P.add("act", lambda e: e.dma_start(out=ident_f, in_=identf_d), writes=["ident_f"], dma_chan="c1")
        P.add("act", lambda e: e.dma_start(out=iota1, in_=iota1_d), writes=["iota1"], dma_chan="c2")
        P.add("dve", lambda e: e.memset(ones_f, 1.0), writes=["ones_f"])

        def load_gain(src_row):
            P.add("act", lambda e: e.dma_start(out=G, in_=src_row.partition_broadcast(128)), writes=["G"], dma_chan="g")

        def rms_stats(src_fn, keys_fn, n):
            for i in range(n):
                P.add("act", (lambda e, i=i: e.activation(junk, src_fn(i), AF.Square, accum_out=ss[:, i:i + 1])),
                      reads=keys_fn(i), writes=["junk", ("ss", i)])
            P.add("act", lambda e: e.activation(std[:, 0:n], ss[:, 0:n], AF.Sqrt, bias=eps_ap, scale=1.0 / D),
                  reads=[("ss", i) for i in range(n)] + ["eps"], writes=["std"])
            P.add("dve", lambda e: e.reciprocal(rstd[:, 0:n], std[:, 0:n]), reads=["std"], writes=["rstd"])

        eps_ap = pa.take(4, F32)
        P.add("dve", lambda e: e.memset(eps_ap, EPS), writes=["eps"])

        ph = Arena(ph_t, PH_BYTES)
        memx = ph.take(2 * D * 4, F32, "p (i d) -> p i d", i=2)
        for i in range(2):
            P.add("sp", (lambda e, i=i: e.dma_start(out=memx[:, i, :], in_=mem_d[128 * i:128 * (i + 1), :])),
                  writes=[("memx", i)], dma_chan=("mx", i))
        load_gain(mem_norm_d[0])
        rms_stats(lambda i: memx[:, i, :], lambda i: [("memx", i)], 2)
        for i in range(2):
            xb = XN[i % 2]
            P.add("dve", (lambda e, i=i, xb=xb: e.scalar_tensor_tensor(xb, memx[:, i, :], rstd[:, i:i + 1], G, ALU.mult, ALU.mult)),
                  reads=[("memx", i), "rstd", "G"], writes=[("XN", i % 2)])
            for c in range(KC):
                P.add("pe", (lambda e, c=c, xb=xb, i=i: e.transpose(PSB[i][:, 128 * c:128 * (c + 1)], xb[:, 128 * c:128 * (c + 1)], ident_b)),
                      reads=[("XN", i % 2), "ident_b"], writes=[("ps", i)])
            P.add("act", (lambda e, i=i: e.copy(memT[:, :, 128 * i:128 * (i + 1)], PSB[i].rearrange("p (k t) -> p k t", k=KC))),
                  reads=[("ps", i)], writes=[("memT", i)])
        P.barrier()

        for l in range(n_layers):
            ph = Arena(ph_t, PH_BYTES)
            WI = [ph.take(KC * 384 * 2, BF16, "p (k c) -> p k c", k=KC) for _ in range(2)]
            mix_base = ph.off
            w_in_v = w_in_d[l].rearrange("(k p) c -> p k c", p=128)
            w_out_v = w_out_d[l].rearrange("(k p) d -> p k d", p=128)

            load_gain(norm_mix_d[l])
            P.add("act", lambda e: e.dma_start(out=cw, in_=convw_d[l]), writes=["cw"], dma_chan="cw")

            def load_wi(buf, ranges):
                off = 0
                for (c0, n) in ranges:
                    P.add("pool", (lambda e, c0=c0, n=n, off=off: e.dma_start(out=WI[buf][:, :, off:off + n], in_=w_in_v[:, :, c0:c0 + n])),
                          writes=[("WI", buf, u) for u in range(off // 128, (off + n) // 128)], dma_chan=("wi", buf, off // 128))
                    off += n

            rms_stats(lambda i: X[:, i, :], lambda i: [("X", i, 0), ("X", i, 1)], NT)
            for i in range(NT):
                xb = XN[i % 2]
                pb = i % 2
                P.add("dve", (lambda e, i=i, xb=xb: e.scalar_tensor_tensor(xb, X[:, i, :], rstd[:, i:i + 1], G, ALU.mult, ALU.mult)),
                      reads=[("X", i, 0), ("X", i, 1), "rstd", "G"], writes=[("XN", i % 2)])
                for c in range(KC):
                    P.add("pe", (lambda e, c=c, xb=xb, pb=pb: e.transpose(PSB[pb][:, 128 * c:128 * (c + 1)], xb[:, 128 * c:128 * (c + 1)], ident_b)),
                          reads=[("XN", i % 2), "ident_b"], writes=[("ps", pb)])
                P.add("act", (lambda e, i=i, pb=pb: e.copy(xnT[:, :, 128 * i:128 * (i + 1)], PSB[pb].rearrange("p (k t) -> p k t", k=KC))),
                      reads=[("ps", pb)], writes=[("xnT", i // 4)])

            dump("d_xnT", A, [("xnT", tb) for tb in range(4)])

            def proj_feat(wi_ap, wi_keys, dst_fn, dst_key_fn, bank0, nbank=2, evac_scale=None):
                for tb in range(4):
                    bk = bank0 + tb % nbank
                    for k in range(KC):
                        P.add("pe", (lambda e, k=k, tb=tb, bk=bk: e.matmul(PS[bk][:, :], wi_ap[:, k, :], xnT[:, k, 512 * tb:512 * (tb + 1)],
                                                                         start=(k == 0), stop=(k == KC - 1))),
                              reads=wi_keys + [("xnT", tb)], writes=[("ps", bk)])
                    en = alt()
                    P.add(en, copy_op(en, dst_fn(tb), PS[bk][:, :]), reads=[("ps", bk)], writes=[dst_key_fn(tb)])

            ph.off = mix_base
            WOC = ph.take(3 * D * 2, BF16, "p (c d) -> p c d", c=3)
            P.add("pool", lambda e: e.dma_start(out=WOC, in_=w_out_v[:, 0:3, :]), writes=["WOC"], dma_chan="woc")
            u_pad = ph.take((S + 2) * 4 + 8, F32)
            tmpc = ph.take(S * 4, F32)
            bg_sb = ph.take(S * 4, F32)
            cg_sb = [ph.take(512 * 4, F32) for _ in range(2)]
            convT = ph.take(3 * S * 2, BF16, "p (c t) -> p c t", c=3)
            P.add("dve", lambda e: e.memset(u_pad[:, 0:1], 0.0), writes=["u_l"])
            P.add("dve", lambda e: e.memset(u_pad[:, S + 1:S + 2], 0.0), writes=["u_r"])
            for c in range(3):
                buf = c % 2
                load_wi(buf, [(128 * c, 128), (384 + 128 * c, 128), (768 + 128 * c, 128)])
                for tb in range(4):
                    for j in range(3):
                        bk = 2 + j
                        for k in range(KC):
                            P.add("pe", (lambda e, k=k, tb=tb, bk=bk, j=j, buf=buf: e.matmul(
                                PS[bk][:, :], WI[buf][:, k, 128 * j:128 * (j + 1)], xnT[:, k, 512 * tb:512 * (tb + 1)],
                                start=(k == 0), stop=(k == KC - 1))),
                                reads=[("WI", buf, j), ("xnT", tb)], writes=[("ps", bk)])
                    cb = cg_sb[tb % 2]
                    P.add("act", (lambda e, cb=cb: e.copy(cb, PS[4][:, :])), reads=[("ps", 4)], writes=[("cg", tb % 2)])
                    P.add("dve", (lambda e, cb=cb, tb=tb: e.tensor_tensor(u_pad[:, 1 + 512 * tb:1 + 512 * (tb + 1)], cb, PS[2][:, :], ALU.mult)),
                          reads=[("cg", tb % 2), ("ps", 2)], writes=[("u", tb)])
                    P.add("act", (lambda e, tb=tb: e.copy(bg_sb[:, 512 * tb:512 * (tb + 1)], PS[3][:, :])),
                          reads=[("ps", 3)], writes=[("bg", tb)])
                ukeys = [("u", tb) for tb in range(4)] + ["u_l", "u_r"]
                P.add("dve", (lambda e, c=c: e.tensor_scalar(tmpc, u_pad[:, 1:S + 1], cw[:, 3 * c + 1:3 * c + 2], None, ALU.mult)),
                      reads=ukeys + ["cw"], writes=["tmpc"])
                P.add("dve", (lambda e, c=c: e.scalar_tensor_tensor(tmpc, u_pad[:, 0:S], cw[:, 3 * c:3 * c + 1], tmpc, ALU.mult, ALU.add)),
                      reads=ukeys + ["cw", "tmpc"], writes=["tmpc"])
                P.add("dve", (lambda e, c=c: e.scalar_tensor_tensor(tmpc, u_pad[:, 2:S + 2], cw[:, 3 * c + 2:3 * c + 3], tmpc, ALU.mult, ALU.add)),
                      reads=ukeys + ["cw", "tmpc"], writes=["tmpc"])
                P.add("dve", (lambda e, c=c: e.tensor_tensor(convT[:, c, :], tmpc, bg_sb, ALU.mult)),
                      reads=["tmpc"] + [("bg", tb) for tb in range(4)], writes=[("convT", c)])
            for i in range(NT):
                for hf in range(2):
                    bk = 5 + (2 * i + hf) % 3
                    for c in range(3):
                        P.add("pe", (lambda e, i=i, hf=hf, c=c, bk=bk: e.matmul(
                            PS[bk][:, :], convT[:, c, 128 * i:128 * (i + 1)], WOC[:, c, 512 * hf:512 * (hf + 1)],
                            start=(c == 0), stop=(c == 2))),
                            reads=[("convT", c), "WOC"], writes=[("ps", bk)])
                    P.add("dve", (lambda e, i=i, hf=hf, bk=bk: e.tensor_tensor(
                        X[:, i, 512 * hf:512 * (hf + 1)], X[:, i, 512 * hf:512 * (hf + 1)], PS[bk][:, :], ALU.add)),
                        reads=[("ps", bk), ("X", i, hf)], writes=[("X", i, hf)])
            dump("d_convT", convT.rearrange("p c t -> p (c t)"), [("convT", c) for c in range(3)])
            dump_x("d_x_conv")
            P.barrier()

            pending = []

            import os as _os
            DEFER = _os.environ.get("K_DEFER", "1") == "1"

            def flush_pending():
                while pending:
                    pending.pop(0)()

            def attn_finish(par, hh, qb, oT_dst, att_key, r_row, o_sb):
                bo = 3 + par
                lrow = 64 if hh == 0 else 0
                olo = 0 if hh == 0 else 64
                lnl = o_sb[0]
                bcs = o_sb[1]
                P.add("act", (lambda e: e.activation(lnl[lrow:lrow + 1, :], PS[bo][lrow:lrow + 1, :], AF.Ln)), reads=[("ps", bo)], writes=["lnl"])
                P.add("act", (lambda e: e.activation(r_row[lrow:lrow + 1, :], lnl[lrow:lrow + 1, :], AF.Exp, scale=-1.0)), reads=["lnl"], writes=["r_row"])
                mo = 64 if hh == 0 else 128
                P.add("pe", lambda e: e.matmul(PS[5][0:mo, :], ones_f[lrow:lrow + 1, 0:mo], r_row[lrow:lrow + 1, :], start=True, stop=True),
                      reads=["r_row", "ones_f"], writes=[("ps", 5)])
                P.add("dve", (lambda e: e.tensor_copy(bcs[olo:olo + 64, :], PS[5][olo:olo + 64, :])), reads=[("ps", 5)], writes=["bcs"])
                P.add("dve", (lambda e: e.tensor_tensor(oT_dst[olo:olo + 64, 512 * qb:512 * (qb + 1)], PS[bo][olo:olo + 64, :], bcs[olo:olo + 64, :], ALU.mult)),
                      reads=[("ps", bo), "bcs"], writes=[att_key + (hh, qb)])

            def attn_outproj(oT_all, npair, att_key, WO_part):
                for i in range(NT):
                    for hf in range(2):
                        bk = 6 + (2 * i + hf) % 2
                        for hp_ in range(npair):
                            P.add("pe", (lambda e, hp_=hp_: e.matmul(
                                PS[bk][:, :], oT_all[:, hp_, 128 * i:128 * (i + 1)], WO_part[:, hp_, 512 * hf:512 * (hf + 1)],
                                start=(hp_ == 0), stop=(hp_ == npair - 1))),
                                reads=[(att_key, hp_, 0, i // 4), (att_key, hp_, 1, i // 4), "WO_part"], writes=[("ps", bk)])
                        P.add("dve", (lambda e: e.tensor_tensor(
                            X[:, i, 512 * hf:512 * (hf + 1)], X[:, i, 512 * hf:512 * (hf + 1)], PS[bk][:, :], ALU.add)),
                            reads=[("ps", bk), ("X", i, hf)], writes=[("X", i, hf)])

            def proj_q_pair(wi_ap, wi_key, qz_, zkey):
                for tb in range(4):
                    bk = 6 + tb % 2
                    for k in range(KC):
                        P.add("pe", (lambda e, k=k: e.matmul(PS[bk][:, :], wi_ap[:, k, :], xnT[:, k, 512 * tb:512 * (tb + 1)],
                                                             start=(k == 0), stop=(k == KC - 1))),
                              reads=[wi_key, ("xnT", tb)], writes=[("ps", bk)])
                    P.add("act", (lambda e: e.copy(qz_[0][0:64, 512 * tb:512 * (tb + 1)], PS[bk][0:64, :])),
                          reads=[("ps", bk), zkey + "0pad"], writes=[(zkey, 0, tb)])
                    P.add("dve", (lambda e: e.tensor_copy(qz_[1][64:128, 512 * tb:512 * (tb + 1)], PS[bk][64:128, :])),
                          reads=[("ps", bk), zkey + "1pad"], writes=[(zkey, 1, tb)])

            ph.off = mix_base
            VB = 192
            v_flat = ph.take(NT * 3 * VB * 2, BF16)
            v4 = v_flat.rearrange("p (i h c) -> p i h c", i=NT, h=3)
            qz = [ph.take(S * 2, BF16) for _ in range(2)]
            kT = [ph.take(S * 2, BF16) for _ in range(2)]
            ET = ph.take(2 * ET_W * 2, BF16, "p (h v) -> p h v", h=2)
            oT_all = ph.take(3 * S * 2, BF16, "p (h t) -> p h t", h=3)
            WOD = ph.take(3 * D * 2, BF16, "p (c d) -> p c d", c=3)
            e_sb = [ph.take(512 * 2, BF16) for _ in range(3)]
            p_sb = [ph.take(512 * 2, BF16) for _ in range(3)]
            o_sb = [ph.take(512 * 4, F32) for _ in range(2)]
            r_row = ph.take(512 * 4, F32)
            P.add("dve", lambda e: e.memset(qz[0][64:128, :], 0.0), writes=["qz0pad"])
            P.add("dve", lambda e: e.memset(qz[1][0:64, :], 0.0), writes=["qz1pad"])
            P.add("dve", lambda e: e.memset(v4[:, :, :, 64:128], 0.0), writes=["v_ones"])
            P.add("dve", lambda e: e.memset(v4[:, :, :, 64:65], 1.0), reads=["v_ones"], writes=["v_ones"])
            P.add("dve", lambda e: e.memset(v4[:, :, :, 96:97], 1.0), reads=["v_ones"], writes=["v_ones"])
            P.add("pool", lambda e: e.dma_start(out=WOD, in_=w_out_d[l][384:768, :].rearrange("(c p) d -> p c d", p=128)),
                  writes=["WO_part"], dma_chan="wod")
            load_wi(0, [(1920, 384)])
            for i in range(NT):
                bk = i % 2
                for k in range(KC):
                    P.add("pe", (lambda e, k=k: e.matmul(PS[bk][:, 0:384], xnT[:, k, 128 * i:128 * (i + 1)], WI[0][:, k, 0:384],
                                                         start=(k == 0), stop=(k == KC - 1))),
                          reads=[("WI", 0, 0), ("WI", 0, 1), ("WI", 0, 2), ("xnT", i // 4)], writes=[("ps", bk)])
                psv = PS[bk][:, 0:384].rearrange("p (h two c) -> p h two c", h=3, two=2)
                en = "act" if i % 2 == 0 else "dve"
                P.add(en, copy_op(en, v4[:, i, :, 0:64], psv[:, :, 0, :]), reads=[("ps", bk)], writes=[("vA", i)])
                P.add(en, copy_op(en, v4[:, i, :, 128:192], psv[:, :, 1, :]), reads=[("ps", bk)], writes=[("vB", i)])

            for hp in range(3):
                buf = (hp + 1) % 2
                load_wi(buf, [(1152 + 128 * hp, 128), (1536 + 128 * hp, 128)])
                P.add("sp", (lambda e: e.dma_start(out=ET, in_=etab_d[2 * hp:2 * hp + 2].rearrange("h p v -> p h v"))),
                      writes=["ET"], dma_chan="et")
                kb_ = kT[hp % 2]
                proj_q_pair(WI[buf][:, :, 0:128], ("WI", buf, 0), qz, "qz")
                proj_feat(WI[buf][:, :, 128:256], [("WI", buf, 1)], (lambda tb, kb_=kb_: kb_[:, 512 * tb:512 * (tb + 1)]),
                          (lambda tb, hp=hp: ("kT", hp % 2, tb)), 6)
                gi = 0
                for hh in range(2):
                    h = 2 * hp + hh
                    for qb in range(4):
                        kts = []
                        for kt in range(max(0, 4 * qb - 8), min(15, 4 * qb + 11) + 1):
                            v0 = ET_OFF - (128 * kt - 512 * qb)
                            if etab_nz[h][:, v0:v0 + 512].any():
                                kts.append(kt)
                        par = gi % 2
                        gi += 1
                        bo = 3 + par

                        def s_mm(kt, n):
                            bs = n % 3
                            P.add("pe", (lambda e: e.matmul(PS[bs][:, :], kb_[:, 128 * kt:128 * (kt + 1)],
                                                            qz[hh][:, 512 * qb:512 * (qb + 1)], start=True, stop=True)),
                                  reads=[("kT", hp % 2, kt // 4), ("qz", hh, qb), "qz%dpad" % hh], writes=[("ps", bs)])

                        s_mm(kts[0], 0)
                        if len(kts) > 1:
                            s_mm(kts[1], 1)
                        for n, kt in enumerate(kts):
                            bs = n % 3
                            v0 = ET_OFF - (128 * kt - 512 * qb)
                            P.add("act", (lambda e: e.activation(e_sb[bs], PS[bs][:, :], AF.Exp, scale=0.125)),
                                  reads=[("ps", bs)], writes=[("e_sb", bs)])
                            P.add("dve", (lambda e: e.tensor_tensor(p_sb[bs], e_sb[bs], ET[:, hh, v0:v0 + 512], ALU.mult)),
                                  reads=[("e_sb", bs), "ET"], writes=[("p_sb", bs)])
                            if n + 2 < len(kts):
                                s_mm(kts[n + 2], n + 2)
                            voff = (kt * 3 + hp) * VB + 64 * hh
                            P.add("pe", (lambda e: e.matmul(PS[bo][:, :], v_flat[:, voff:voff + 128], p_sb[bs],
                                                            start=(n == 0), stop=(n == len(kts) - 1))),
                                  reads=[("p_sb", bs), ("vA", kt), ("vB", kt), "v_ones"], writes=[("ps", bo)])
                            if n == min(1, len(kts) - 1):
                                flush_pending()
                        pending.append(lambda par=par, hh=hh, qb=qb, hp=hp: attn_finish(
                            par, hh, qb, oT_all[:, hp, :], ("oT", hp), r_row, o_sb))
                        if not DEFER:
                            flush_pending()
            flush_pending()
            attn_outproj(oT_all, 3, "oT", WOD)
            P.barrier()

            ph.off = mix_base
            if skip_mem:
                continue
            WMKV = ph.take(KC * 512 * 2, BF16, "p (k c) -> p k c", k=KC)
            kmT = ph.take(2 * MEM * 2, BF16, "p (h t) -> p h t", h=2)
            vm_flat = ph.take(2 * 2 * VB * 2, BF16)
            vm4 = vm_flat.rearrange("p (i h c) -> p i h c", i=2, h=2)
            qmz = [ph.take(S * 2, BF16) for _ in range(2)]
            oTm_all = ph.take(2 * S * 2, BF16, "p (h t) -> p h t", h=2)
            WOM = ph.take(2 * D * 2, BF16, "p (c d) -> p c d", c=2)
            p_sb = [ph.take(512 * 2, BF16) for _ in range(4)]
            o_sb = [ph.take(512 * 4, F32) for _ in range(2)]
            r_row = ph.take(512 * 4, F32)
            P.add("dve", lambda e: e.memset(qmz[0][64:128, :], 0.0), writes=["qmz0pad"])
            P.add("dve", lambda e: e.memset(qmz[1][0:64, :], 0.0), writes=["qmz1pad"])
            P.add("dve", lambda e: e.memset(vm4[:, :, :, 64:128], 0.0), writes=["vm_ones"])
            P.add("dve", lambda e: e.memset(vm4[:, :, :, 64:65], 1.0), reads=["vm_ones"], writes=["vm_ones"])
            P.add("dve", lambda e: e.memset(vm4[:, :, :, 96:97], 1.0), reads=["vm_ones"], writes=["vm_ones"])
            P.add("pool", lambda e: e.dma_start(out=WMKV, in_=w_mkv_d[l].rearrange("(k p) c -> p k c", p=128)), writes=["WMKV"], dma_chan="wmkv")
            P.add("pool", lambda e: e.dma_start(out=WOM, in_=w_out_d[l][768:1024, :].rearrange("(c p) d -> p c d", p=128)),
                  writes=["WO_part"], dma_chan="wod")
            for mp in range(2):
                for k in range(KC):
                    P.add("pe", (lambda e, k=k: e.matmul(PS[0][:, 0:MEM], WMKV[:, k, 128 * mp:128 * (mp + 1)], memT[:, k, :],
                                                         start=(k == 0), stop=(k == KC - 1))),
                          reads=["WMKV", ("memT", 0), ("memT", 1)], writes=[("ps", 0)])
                P.add("act", (lambda e: e.copy(kmT[:, mp, :], PS[0][:, 0:MEM])), reads=[("ps", 0)], writes=[("kmT", mp)])
            for i in range(2):
                for k in range(KC):
                    P.add("pe", (lambda e, k=k: e.matmul(PS[1][:, 0:256], memT[:, k, 128 * i:128 * (i + 1)], WMKV[:, k, 256:512],
                                                         start=(k == 0), stop=(k == KC - 1))),
                          reads=["WMKV", ("memT", 0), ("memT", 1)], writes=[("ps", 1)])
                psv = PS[1][:, 0:256].rearrange("p (h two c) -> p h two c", h=2, two=2)
                P.add("act", (lambda e: e.copy(vm4[:, i, :, 0:64], psv[:, :, 0, :])), reads=[("ps", 1)], writes=[("vmA", i)])
                P.add("act", (lambda e: e.copy(vm4[:, i, :, 128:192], psv[:, :, 1, :])), reads=[("ps", 1)], writes=[("vmB", i)])
            vmkeys = [("vmA", 0), ("vmA", 1), ("vmB", 0), ("vmB", 1), "vm_ones"]
            gi = 0
            for mp in range(2):
                buf = mp % 2
                load_wi(buf, [(2304 + 128 * mp, 128)])
                proj_q_pair(WI[buf][:, :, 0:128], ("WI", buf, 0), qmz, "qmz")
                for hh in range(2):
                    for qb in range(4):
                        par = gi % 2
                        sb0 = 2 * (gi % 2)
                        gi += 1
                        bo = 3 + par
                        for kt in range(2):
                            bs = sb0 + kt
                            P.add("pe", (lambda e: e.matmul(PS[bs if bs < 3 else 7][:, :], kmT[:, mp, 128 * kt:128 * (kt + 1)],
                                                            qmz[hh][:, 512 * qb:512 * (qb + 1)], start=True, stop=True)),
                                  reads=[("kmT", mp), ("qmz", hh, qb), "qmz%dpad" % hh], writes=[("ps", bs if bs < 3 else 7)])
                        for kt in range(2):
                            bs = sb0 + kt
                            pbk = bs if bs < 3 else 7
                            P.add("act", (lambda e: e.activation(p_sb[bs], PS[pbk][:, :], AF.Exp, scale=0.125)),
                                  reads=[("ps", pbk)], writes=[("p_sb", bs)])
                            if kt == 0:
                                flush_pending()
                            voff = (kt * 2 + mp) * VB + 64 * hh
                            P.add("pe", (lambda e: e.matmul(PS[bo][:, :], vm_flat[:, voff:voff + 128], p_sb[bs], start=(kt == 0), stop=(kt == 1))),
                                  reads=[("p_sb", bs)] + vmkeys, writes=[("ps", bo)])
                        pending.append(lambda par=par, hh=hh, qb=qb, mp=mp: attn_finish(
                            par, hh, qb, oTm_all[:, mp, :], ("oTm", mp), r_row, o_sb))
            flush_pending()
            attn_outproj(oTm_all, 2, "oTm", WOM)
            P.barrier()

            if not do_moe:
                continue
            ph = Arena(ph_t, PH_BYTES)
            WG = [ph.take(KC * 512 * 2, BF16, "p (k f) -> p k f", k=KC) for _ in range(2)]
            WU = [ph.take(KC * 512 * 2, BF16, "p (k f) -> p k f", k=KC) for _ in range(2)]
            WD = [ph.take(4 * D * 2, BF16, "p (c d) -> p c d", c=4) for _ in range(2)]
            WR = ph.take(KC * E * 4, F32, "p (k e) -> p k e", k=KC)
            moe_base = ph.off

            def load_expert_w(e_, fs):
                buf = (e_ * 4 + fs) % 2
                P.add("pool", (lambda e, e_=e_, fs=fs, buf=buf: e.dma_start(
                    out=WG[buf], in_=w_gate_d[l, e_].rearrange("(k p) f -> p k f", p=128)[:, :, 512 * fs:512 * (fs + 1)])),
                    writes=[("WG", buf)], dma_chan=("wg", buf))
                P.add("pool", (lambda e, e_=e_, fs=fs, buf=buf: e.dma_start(
                    out=WU[buf], in_=w_up_d[l, e_].rearrange("(k p) f -> p k f", p=128)[:, :, 512 * fs:512 * (fs + 1)])),
                    writes=[("WU", buf)], dma_chan=("wu", buf))
                P.add("pool", (lambda e, e_=e_, fs=fs, buf=buf: e.dma_start(
                    out=WD[buf], in_=w_down_d[l, e_][512 * fs:512 * (fs + 1), :].rearrange("(c p) d -> p c d", p=128))),
                    writes=[("WD", buf)], dma_chan=("wd", buf))

            load_gain(norm_ffn_d[l])
            P.add("act", lambda e: e.dma_start(out=WR, in_=w_router_d[l].rearrange("(k p) e -> p k e", p=128)), writes=["WR"], dma_chan="wr")
            load_expert_w(0, 0)
            load_expert_w(0, 1)

            affT = ph.take(S * 4, F32)
            cjunk = ph.take(S * 2, BF16)
            maskT = ph.take(S * 4, F32)
            r_base = ph.off
            xnf = [ph.take(D * 4, F32) for _ in range(2)]
            xnTf = [ph.take(KC * 128 * 4, F32, "p (k t) -> p k t", k=KC) for _ in range(2)]
            ph.off = r_base
            cums = ph.take(S * 4, F32)
            selp = ph.take(S * 4, F32)
            lg_sb = [ph.take(128 * 4, F32) for _ in range(2)]
            tq = ph.take(8 * 4, F32)
            rms_stats(lambda i: X[:, i, :], lambda i: [("X", i, 0), ("X", i, 1)], NT)

            def softmax_tile(i):
                lb = 3 + i % 2
                P.add("dve", lambda e: e.reduce_max(sm[:, 0:1], PS[lb][:, 0:E], axis=AX.X), reads=[("ps", lb)], writes=["sm0"])
                P.add("dve", lambda e: e.tensor_scalar(sm[:, 1:2], sm[:, 0:1], -1.0, None, ALU.mult), reads=["sm0"], writes=["sm1"])
                P.add("act", (lambda e: e.activation(aff_all[:, i, :], PS[lb][:, 0:E], AF.Exp, bias=sm[:, 1:2], scale=1.0, accum_out=sm[:, 2:3])),
                      reads=[("ps", lb), "sm1"], writes=[("aff", i), "sm2"])
                P.add("dve", lambda e: e.reciprocal(sm[:, 3:4], sm[:, 2:3]), reads=["sm2"], writes=["sm3"])
                P.add("dve", (lambda e: e.tensor_scalar(aff_all[:, i, :], aff_all[:, i, :], sm[:, 3:4], None, ALU.mult)),
                      reads=[("aff", i), "sm3"], writes=[("aff", i)])

            def stage_a(i):
                xf = xnf[i % 2]
                P.add("dve", (lambda e: e.scalar_tensor_tensor(xf, X[:, i, :], rstd[:, i:i + 1], G, ALU.mult, ALU.mult)),
                      reads=[("X", i, 0), ("X", i, 1), "rstd", "G"], writes=[("xnf", i % 2)])
                P.add("act", (lambda e: e.copy(xnTok[:, i, :], xf)), reads=[("xnf", i % 2)], writes=[("xnTok", i)])

            def stage_b(i):
                xf = xnf[i % 2]
                xt = xnTf[i % 2]
                lb = 3 + i % 2
                for c in range(KC):
                    bk = c // 4
                    P.add("pe", (lambda e, c=c, bk=bk: e.transpose(PS[bk][:, 128 * (c % 4):128 * (c % 4 + 1)], xf[:, 128 * c:128 * (c + 1)], ident_f)),
                          reads=[("xnf", i % 2), "ident_f"], writes=[("ps", bk)])
                P.add("act", lambda e: e.copy(xt[:, 0:4, :], PS[0][:, :].rearrange("p (k t) -> p k t", k=4)), reads=[("ps", 0)], writes=[("xnTf", i % 2, 0)])
                P.add("dve", lambda e: e.tensor_copy(xt[:, 4:8, :], PS[1][:, :].rearrange("p (k t) -> p k t", k=4)), reads=[("ps", 1)], writes=[("xnTf", i % 2, 1)])
                for k in range(KC):
                    P.add("pe", (lambda e, k=k: e.matmul(PS[2][0:E, 0:128], WR[:, k, :], xt[:, k, :], start=(k == 0), stop=(k == KC - 1))),
                          reads=[("xnTf", i % 2, k // 4), "WR"], writes=[("ps", 2)])
                P.add("act", (lambda e: e.copy(lg_sb[i % 2][0:E, :], PS[2][0:E, 0:128])), reads=[("ps", 2)], writes=[("lg", i % 2)])
                P.add("pe", (lambda e: e.transpose(PS[lb][:, 0:E], lg_sb[i % 2][0:E, :], ident_f[0:E, 0:E])),
                      reads=[("lg", i % 2), "ident_f"], writes=[("ps", lb)])

            stage_a(0)
            for i in range(NT):
                if i + 1 < NT:
                    stage_a(i + 1)
                if i > 0:
                    softmax_tile(i - 1)
                stage_b(i)
            softmax_tile(NT - 1)
            for i in range(NT):
                bk = 5 + (i // 4) % 2
                P.add("pe", (lambda e, i=i, bk=bk: e.transpose(PS[bk][0:E, 128 * (i % 4):128 * (i % 4 + 1)], aff_all[:, i, :], ident_f)),
                      reads=[("aff", i), "ident_f"], writes=[("ps", bk)])
                if i % 4 == 3:
                    j = i // 4
                    P.add("act", (lambda e, j=j, bk=bk: e.copy(affT[0:E, 512 * j:512 * (j + 1)], PS[bk][0:E, :])), reads=[("ps", bk)], writes=[("affT", j)])
            P.barrier()
            affT_keys = [("affT", j) for j in range(4)]
            P.add("dve", lambda e: e.memset(tq[0:E, 0:1], 0.0), writes=["tq_t"])
            for kbit in range(1, 31):
                step = 2.0 ** (-kbit)
                P.add("dve", lambda e: e.tensor_scalar(tq[0:E, 1:2], tq[0:E, 0:1], step, None, ALU.add), reads=["tq_t"], writes=["tq_c"])
                P.add("dve", lambda e: e.tensor_scalar(cjunk[0:E, :], affT[0:E, :], tq[0:E, 1:2], None, ALU.is_ge, ALU.add, accum_out=tq[0:E, 2:3]),
                      reads=affT_keys + ["tq_c"], writes=["cjunk", "tq_n"])
                P.add("dve", lambda e: e.tensor_scalar(tq[0:E, 3:4], tq[0:E, 2:3], CAP - 0.5, step, ALU.is_ge, ALU.mult), reads=["tq_n"], writes=["tq_g"])
                P.add("dve", lambda e: e.tensor_tensor(tq[0:E, 0:1], tq[0:E, 0:1], tq[0:E, 3:4], ALU.add), reads=["tq_t", "tq_g"], writes=["tq_t"])
            P.add("dve", lambda e: e.tensor_scalar(maskT[0:E, :], affT[0:E, :], tq[0:E, 0:1], None, ALU.is_ge), reads=affT_keys + ["tq_t"], writes=["maskT"])
            P.add("dve", lambda e: e.tensor_tensor_scan(cums[0:E, :], maskT[0:E, :], maskT[0:E, :], 0.0, ALU.add, ALU.max),
                  reads=["maskT"], writes=["cums"])
            P.add("dve", lambda e: e.tensor_tensor(selp[0:E, :], cums[0:E, :], maskT[0:E, :], ALU.mult), reads=["cums", "maskT"], writes=["selp"])
            for i in range(NT):
                P.add("pe", (lambda e, i=i: e.transpose(PS[7][:, E * i:E * (i + 1)], selp[0:E, 128 * i:128 * (i + 1)], ident_f[0:E, 0:E])),
                      reads=["selp", "ident_f"], writes=[("ps", 7)])
            P.add("act", lambda e: e.copy(sel_all, PS[7][:, 0:NT * E].rearrange("p (i e) -> p i e", i=NT)), reads=[("ps", 7)], writes=["sel"])
            P.barrier()

            ph.off = moe_base
            Ssel = ph.take(NT * CAP * 2, BF16, "p (i j) -> p i j", i=NT)
            Sg = ph.take(8 * CAP * 2, BF16, "p (i j) -> p i j", i=8)
            SgT = [ph.take(2 * S * 2, BF16, "p (j t) -> p j t", j=2) for _ in range(2)]
            xgT = ph.take(KC * CAP * 2, BF16, "p (k j) -> p k j", k=KC)
            hT = [ph.take(4 * CAP * 2, BF16, "p (c j) -> p c j", c=4) for _ in range(2)]
            sg_sb = [ph.take(CAP * 4, F32) for _ in range(2)]
            y_sb = ph.take(2 * D * 2, BF16, "p (j d) -> p j d", j=2)
            xg_keys = [("xgT", c) for c in range(KC)]

            def scatter_unit(es, u):
                i, hf = u // 2, u % 2
                bk = 6 + u % 2
                for jc in range(2):
                    P.add("pe", (lambda e, jc=jc: e.matmul(
                        PS[bk][:, :], SgT[es % 2][:, jc, 128 * i:128 * (i + 1)], y_sb[:, jc, 512 * hf:512 * (hf + 1)], start=(jc == 0), stop=(jc == 1))),
                        reads=[("SgT", es % 2, jc, i // 8), ("y", jc, hf)], writes=[("ps", bk)])
                P.add("dve", (lambda e: e.tensor_tensor(
                    X[:, i, 512 * hf:512 * (hf + 1)], X[:, i, 512 * hf:512 * (hf + 1)], PS[bk][:, :], ALU.add)),
                    reads=[("ps", bk), ("X", i, hf)], writes=[("X", i, hf)])

            for e_ in range(E):
                for half in range(2):
                    for ii in range(8):
                        i = 8 * half + ii
                        P.add("dve", (lambda e, i=i: e.tensor_scalar(Ssel[:, i, :], iota1, sel_all[:, i, e_:e_ + 1], None, ALU.is_equal)),
                              reads=["iota1", "sel"], writes=[("Ssel", i)])
                        P.add("dve", (lambda e, i=i, ii=ii: e.tensor_scalar(Sg[:, ii, :], iota1, sel_all[:, i, e_:e_ + 1], aff_all[:, i, e_:e_ + 1],
                                                                            ALU.is_equal, ALU.mult)),
                              reads=["iota1", "sel", ("aff", i)], writes=[("Sg", ii)])
                    for jc in range(2):
                        bk = 6 + jc
                        for ii in range(8):
                            P.add("pe", (lambda e, ii=ii, jc=jc, bk=bk: e.transpose(
                                PSB[bk][:, 128 * ii:128 * (ii + 1)], Sg[:, ii, 128 * jc:128 * (jc + 1)], ident_b)),
                                reads=[("Sg", ii), "ident_b"], writes=[("ps", bk)])
                        en = alt()
                        P.add(en, copy_op(en, SgT[e_ % 2][:, jc, 1024 * half:1024 * (half + 1)], PSB[bk]), reads=[("ps", bk)],
                              writes=[("SgT", e_ % 2, jc, half)])
                for c in range(KC):
                    bk = 6 + c % 2
                    for i in range(NT):
                        P.add("pe", (lambda e, c=c, i=i, bk=bk: e.matmul(PS[bk][:, 0:CAP], xnTok[:, i, 128 * c:128 * (c + 1)], Ssel[:, i, :],
                                                                         start=(i == 0), stop=(i == NT - 1))),
                              reads=[("xnTok", i), ("Ssel", i)], writes=[("ps", bk)])
                    en = alt()
                    P.add(en, copy_op(en, xgT[:, c, :], PS[bk][:, 0:CAP]), reads=[("ps", bk)], writes=[("xgT", c)])

                def gate_up(f):
                    fs, fc = f // 4, f % 4
                    buf = (e_ * 4 + fs) % 2
                    bk = 4 + f % 2
                    for k in range(KC):
                        P.add("pe", (lambda e, k=k: e.matmul(PS[bk][:, 0:CAP], WG[buf][:, k, 128 * fc:128 * (fc + 1)], xgT[:, k, :],
                                                             start=(k == 0), stop=(k == KC - 1))),
                              reads=[("WG", buf)] + xg_keys, writes=[("ps", bk)])
                    for k in range(KC):
                        P.add("pe", (lambda e, k=k: e.matmul(PS[bk][:, CAP:2 * CAP], WU[buf][:, k, 128 * fc:128 * (fc + 1)], xgT[:, k, :],
                                                             start=(k == 0), stop=(k == KC - 1))),
                              reads=[("WU", buf)] + xg_keys, writes=[("ps", bk)])

                gate_up(0)
                for f in range(16):
                    fs, fc = f // 4, f % 4
                    buf = (e_ * 4 + fs) % 2
                    bk = 4 + f % 2
                    hb = hT[fs % 2]
                    sgb = sg_sb[f % 2]
                    P.add("act", (lambda e: e.activation(sgb, PS[bk][:, 0:CAP], AF.Silu)), reads=[("ps", bk)], writes=[("sg", f % 2)])
                    P.add("dve", (lambda e: e.tensor_tensor(hb[:, fc, :], sgb, PS[bk][:, CAP:2 * CAP], ALU.mult)),
                          reads=[("sg", f % 2), ("ps", bk)], writes=[("hT", fs % 2, fc)])
                    if f + 1 < 16:
                        gate_up(f + 1)
                    for jc in range(2):
                        for hf in range(2):
                            by = 2 * jc + hf
                            P.add("pe", (lambda e, jc=jc, hf=hf, by=by: e.matmul(
                                PS[by][:, :], hb[:, fc, 128 * jc:128 * (jc + 1)], WD[buf][:, fc, 512 * hf:512 * (hf + 1)],
                                start=(f == 0), stop=(f == 15))),
                                reads=[("hT", fs % 2, fc), ("WD", buf)], writes=[("ps", by)])
                    if e_ > 0:
                        scatter_unit(e_ - 1, 2 * f)
                        scatter_unit(e_ - 1, 2 * f + 1)
                    if fc == 3:
                        nxt = e_ * 4 + fs + 2
                        if nxt < E * 4:
                            load_expert_w(nxt // 4, nxt % 4)
                for jc in range(2):
                    for hf in range(2):
                        by = 2 * jc + hf
                        en = alt()
                        P.add(en, copy_op(en, y_sb[:, jc, 512 * hf:512 * (hf + 1)], PS[by][:, :]), reads=[("ps", by)], writes=[("y", jc, hf)])
            for u in range(32):
                scatter_unit(E - 1, u)
            P.barrier()

        ph = Arena(ph_t, PH_BYTES)
        ob = [ph.take(D * 4, F32) for _ in range(4)]
        if debug:
            for i in range(NT):
                final_ops.append(P.add("sp", (lambda e, i=i: e.dma_start(out=out_d[128 * i:128 * (i + 1), :], in_=X[:, i, :])),
                                       reads=[("X", i, 0), ("X", i, 1)], dma_chan="dbg", extra_deps=final_ops[-1:]))
        else:
            load_gain(norm_final_d[0])
            rms_stats(lambda i: X[:, i, :], lambda i: [("X", i, 0), ("X", i, 1)], NT)
            for i in range(NT):
                P.add("dve", (lambda e, i=i: e.scalar_tensor_tensor(ob[i % 4], X[:, i, :], rstd[:, i:i + 1], G, ALU.mult, ALU.mult)),
                      reads=[("X", i, 0), ("X", i, 1), "rstd", "G"], writes=[("ob", i % 4)])
                final_ops.append(P.add("sp", (lambda e, i=i: e.dma_start(out=out_d[128 * i:128 * (i + 1), :], in_=ob[i % 4])),
                                       reads=[("ob", i % 4)], writes=[("obd", i % 4)], dma_chan=("o", i % 4)))
        P.emit(nc, final_waits=[("sp", o) for o in final_ops])
    return nc


def _etab():
    t = np.zeros((6, 128, ET_W), np.float64)
    p = np.arange(128)[:, None]
    v = np.arange(ET_W)[None, :]
    d = p - v + ET_OFF
    ad = np.abs(d)
    m = (ad <= 64).astype(np.float64) + ((d % 4 == 0) & (ad <= 256)) + ((d % 16 == 0) & (ad <= 1024))
    for h in range(6):
        slope = 2.0 ** (-8.0 * (h + 1) / 6)
        t[h] = m * np.exp(-slope * ad)
    return t.astype(np.float32).astype(ml_dtypes.bfloat16)


def _consts():
    return {
        "ident_bf": np.eye(128, dtype=np.float32).astype(ml_dtypes.bfloat16),
        "ident_f": np.eye(128, dtype=np.float32),
        "iota1": np.tile(np.arange(1, CAP + 1, dtype=np.float32)[None, :], (128, 1)),
        "etab": _etab(),
    }


def make_in_maps(x, mem, mem_norm, norm_mix, w_in, conv_w, w_mem_kv, w_out, norm_ffn,
                 w_router, w_gate, w_up, w_down, norm_final):
    f = lambda a: np.ascontiguousarray(np.asarray(a, dtype=np.float32))
    conv_w = f(conv_w)
    convw_t = np.ascontiguousarray(conv_w.reshape(L, 3, 3, 128).transpose(0, 3, 2, 1).reshape(L, 128, 9))
    shared = {
        "mem_norm": f(mem_norm).reshape(1, D), "norm_mix": f(norm_mix), "norm_ffn": f(norm_ffn),
        "norm_final": f(norm_final).reshape(1, D), "w_in": f(w_in), "convw_t": convw_t,
        "w_mem_kv": f(w_mem_kv), "w_out": f(w_out), "w_router": f(w_router),
        "w_gate": f(w_gate), "w_up": f(w_up), "w_down": f(w_down),
    }
    shared.update(_consts())
    x = f(x)
    mem = f(mem)
    return [dict(shared, x=x[b], mem=mem[b]) for b in range(N_CORES)]


def kernel(x, mem, mem_norm, norm_mix, w_in, conv_w, w_mem_kv, w_out, norm_ffn,
           w_router, w_gate, w_up, w_down, norm_final):
    in_maps = make_in_maps(x, mem, mem_norm, norm_mix, w_in, conv_w, w_mem_kv, w_out, norm_ffn,
                           w_router, w_gate, w_up, w_down, norm_final)
    nc = build_program()
    res = run_bass_kernel_spmd(nc, in_maps, core_ids=list(range(N_CORES)))
    return np.stack([np.asarray(r["out"], dtype=np.float32) for r in res.results], axis=0)
```

```python
import contextlib
import numpy as np
import ml_dtypes
import concourse.bass as bass
import concourse.mybir as mybir
from concourse.bass_utils import run_bass_kernel_spmd

F32 = mybir.dt.float32
BF16 = mybir.dt.bfloat16
AF = mybir.ActivationFunctionType
ALU = mybir.AluOpType
AX = mybir.AxisListType

D = 1024
S = 2048
NT = 16
KC = 8
L = 2
E = 16
FF = 2048
CAP = 256
MEM = 256
EPS = 1e-6
ET_W = 2944
ET_OFF = 1408
N_CORES = 8


class _Rec:
    def __init__(self):
        self.call = None

    def __getattr__(self, name):
        def f(*a, **k):
            self.call = (name, a, k)
            return self
        return f


class Prog:
    def __init__(self):
        self.ops = []
        self.last_writer = {}
        self.readers = {}
        self.chan_count = {}
        self.last_eng = {}
        self.last_chan = {}

    def add(self, eng, fn, reads=(), writes=(), dma_chan=None, extra_deps=()):
        idx = len(self.ops)
        deps = set(extra_deps)
        for k in reads:
            w = self.last_writer.get(k)
            if w is not None:
                deps.add(w)
        for k in writes:
            w = self.last_writer.get(k)
            if w is not None:
                deps.add(w)
            for r in self.readers.get(k, {}).values():
                deps.add(r)
        deps.discard(idx)
        rkey = eng if dma_chan is None else ("dma", idx)
        for k in reads:
            self.readers.setdefault(k, {})[rkey] = idx
        for k in writes:
            self.last_writer[k] = idx
            self.readers[k] = {}
        if fn is not None:
            rec = _Rec()
            fn(rec)
            call = rec.call
            assert call is not None
            fn = (lambda e, call=call: getattr(e, call[0])(*call[1], **call[2]))
        op = dict(eng=eng, fn=fn, deps=deps, chan=dma_chan, signal=False)
        if dma_chan is not None:
            self.chan_count[dma_chan] = self.chan_count.get(dma_chan, 0) + 16
            op["dmaval"] = self.chan_count[dma_chan]
            self.last_chan[dma_chan] = idx
        elif fn is not None:
            self.last_eng[eng] = idx
        self.ops.append(op)
        return idx

    def barrier(self):
        deps = set(self.last_eng.values()) | set(self.last_chan.values())
        for e in ["pe", "act", "dve", "pool", "sp"]:
            self.add(e, None, extra_deps=deps)

    def emit(self, nc, final_waits=()):
        ops = self.ops
        for op in ops:
            for d in op["deps"]:
                p = ops[d]
                if p["chan"] is None:
                    if p["eng"] == "pe" and op["eng"] == "pe" and op["chan"] is None and op["fn"] is not None:
                        continue
                    p["signal"] = True
        for (_, fo) in final_waits:
            if ops[fo]["chan"] is None:
                ops[fo]["signal"] = True
        engs = ["pe", "act", "dve", "pool", "sp"]
        seq = {e: 0 for e in engs}
        for op in ops:
            if op["chan"] is None and op["signal"]:
                seq[op["eng"]] += 1
                op["seqval"] = seq[op["eng"]]
        chans = sorted(self.chan_count.keys(), key=str)
        with contextlib.ExitStack() as st:
            esem = {e: st.enter_context(nc.semaphore("s_" + e)) for e in engs}
            csem = {c: st.enter_context(nc.semaphore("c_%d" % i)) for i, c in enumerate(chans)}
            block = st.enter_context(nc.Block())

            def run_engine(ename):
                def body(eng):
                    waited = {}
                    for op in ops:
                        if op["eng"] != ename:
                            continue
                        for d in sorted(op["deps"]):
                            p = ops[d]
                            if p["chan"] is not None:
                                key = ("c", p["chan"]); val = p["dmaval"]; sem = csem[p["chan"]]
                            else:
                                if p["eng"] == "pe" and ename == "pe" and op["chan"] is None and op["fn"] is not None:
                                    continue
                                key = ("e", p["eng"]); val = p["seqval"]; sem = esem[p["eng"]]
                            if waited.get(key, 0) >= val:
                                continue
                            eng.wait_ge(sem, val)
                            waited[key] = val
                        if op["fn"] is None:
                            continue
                        ins = op["fn"](eng)
                        if op["chan"] is not None:
                            ins.then_inc(csem[op["chan"]], 16)
                        elif op["signal"]:
                            ins.then_inc(esem[ename], 1)
                    for (e2, fo) in final_waits:
                        if e2 != ename:
                            continue
                        p = ops[fo]
                        if p["chan"] is not None:
                            eng.wait_ge(csem[p["chan"]], p["dmaval"])
                        else:
                            eng.wait_ge(esem[p["eng"]], p["seqval"])
                return body

            block.tensor(run_engine("pe"))
            block.scalar(run_engine("act"))
            block.vector(run_engine("dve"))
            block.gpsimd(run_engine("pool"))
            block.sync(run_engine("sp"))


class Arena:
    def __init__(self, t, nbytes):
        self.t = t
        self.n = nbytes
        self.off = 0
        self.mark = 0

    def take(self, nbytes, dt=F32, pattern=None, **kw):
        off = self.off
        self.off += (nbytes + 63) // 64 * 64
        assert self.off <= self.n, ("arena overflow", self.off, self.n)
        ap = self.t[:, off // 4:(off + nbytes) // 4]
        if dt != F32:
            ap = ap.bitcast(dt)
        if pattern:
            ap = ap.rearrange(pattern, **kw)
        return ap


def build_program(n_layers=L, do_moe=True, debug=False, skip_mem=False, skip_dil=False):
    nc = bass.Bass("TRN2", target_bir_lowering=False)

    def din(name, shape, dt=F32):
        return nc.dram_tensor(name, list(shape), dt, kind="ExternalInput").ap()

    x_d = din("x", [S, D])
    mem_d = din("mem", [MEM, D])
    mem_norm_d = din("mem_norm", [1, D])
    norm_mix_d = din("norm_mix", [L, D])
    norm_ffn_d = din("norm_ffn", [L, D])
    norm_final_d = din("norm_final", [1, D])
    w_in_d = din("w_in", [L, D, 2560])
    convw_d = din("convw_t", [L, 128, 9])
    w_mkv_d = din("w_mem_kv", [L, D, 512])
    w_out_d = din("w_out", [L, D, D])
    if do_moe:
        w_router_d = din("w_router", [L, D, E])
        w_gate_d = din("w_gate", [L, E, D, FF])
        w_up_d = din("w_up", [L, E, D, FF])
        w_down_d = din("w_down", [L, E, FF, D])
    identb_d = din("ident_bf", [128, 128], BF16)
    identf_d = din("ident_f", [128, 128])
    iota1_d = din("iota1", [128, CAP])
    etab_d = din("etab", [6, 128, ET_W], BF16)
    out_d = nc.dram_tensor("out", [S, D], F32, kind="ExternalOutput").ap()
    dbg = {}
    if debug:
        def dout(name, shape, dt=F32):
            dbg[name] = nc.dram_tensor(name, list(shape), dt, kind="ExternalOutput").ap()
        dout("d_xnT", [128, KC * S], BF16)
        dout("d_x_conv", [S, D])
        dout("d_convT", [128, 3 * S], BF16)
        dout("d_qT0", [128, S], BF16)
        dout("d_kT0", [128, S], BF16)
        dout("d_v", [128, NT * 6 * 65], BF16)
        dout("d_oT0", [64, 2 * S], BF16)
        dout("d_x_dil", [S, D])
        dout("d_osb", [65, 512])
        dout("d_psb", [128, 512], BF16)
        dout("d_ET", [128, 2 * ET_W], BF16)
        dout("d_esb", [128, 512], BF16)
        dout("d_S", [128, 512])
        dout("d_psb0", [128, 512], BF16)

    etab_nz = _etab().astype(np.float32) != 0
    PERS_BYTES = 114 * 1024 + 768
    PH_BYTES = 92 * 1024 - 768

    with contextlib.ExitStack() as st:
        pers_t = st.enter_context(nc.sbuf_tensor("pers", [128, PERS_BYTES // 4], F32))
        ph_t = st.enter_context(nc.sbuf_tensor("phase", [128, PH_BYTES // 4], F32))
        PS = [st.enter_context(nc.psum_tensor("ps%d" % i, [128, 512], F32)) for i in range(8)]
        PSB = [p[:].bitcast(BF16) for p in PS]

        pa = Arena(pers_t, PERS_BYTES)
        X = pa.take(NT * D * 4, F32, "p (i d) -> p i d", i=NT)
        A = pa.take(NT * D * 2, BF16)
        xnT = A.rearrange("p (k t) -> p k t", k=KC)
        xnTok = A.rearrange("p (i d) -> p i d", i=NT)
        ident_b = pa.take(128 * 2, BF16)
        ident_f = pa.take(128 * 4, F32)
        iota1 = pa.take(CAP * 4, F32)
        ones_f = pa.take(128 * 4, F32)
        memT = pa.take(KC * MEM * 2, BF16, "p (k t) -> p k t", k=KC)
        G = pa.take(D * 4, F32)
        junk = pa.take(D * 2, BF16)
        XN = [pa.take(D * 2, BF16), pa.take(D * 2, BF16)]
        ss = pa.take(NT * 4, F32)
        std = pa.take(NT * 4, F32)
        rstd = pa.take(NT * 4, F32)
        cw = pa.take(9 * 4, F32)
        aff_all = pa.take(NT * E * 4, F32, "p (i e) -> p i e", i=NT)
        sel_all = pa.take(NT * E * 4, F32, "p (i e) -> p i e", i=NT)
        sm = pa.take(16 * 4, F32)

        P = Prog()
        cnt = [0]

        def xk(tb):
            return [("xnT", tb, 0), ("xnT", tb, 1)]

        def alt():
            cnt[0] += 1
            return "act" if cnt[0] % 2 == 0 else "dve"

        def copy_op(engname, out, in_):
            if engname == "act":
                return lambda e: e.copy(out, in_)
            return lambda e: e.tensor_copy(out, in_)

        def dump(name, src, reads, idx=None):
            if not debug or l != 0:
                return
            dst = dbg[name] if idx is None else dbg[name][idx]
            final_ops.append(P.add("sp", (lambda e: e.dma_start(out=dst, in_=src)), reads=reads, dma_chan="dbg", extra_deps=final_ops[-1:]))

        def dump_x(name):
            if not debug or l != 0:
                return
            for i in range(NT):
                final_ops.append(P.add("sp", (lambda e, i=i: e.dma_start(out=dbg[name][128 * i:128 * (i + 1), :], in_=X[:, i, :])),
                                       reads=[("X", i, 0), ("X", i, 1)], dma_chan="dbg", extra_deps=final_ops[-1:]))

        final_ops = []
        l = 0
        for i in range(NT):
            P.add("sp", (lambda e, i=i: e.dma_start(out=X[:, i, :], in_=x_d[128 * i:128 * (i + 1), :])),
                  writes=[("X", i, 0), ("X", i, 1)], dma_chan=("x", i))
        P.add("act", lambda e: e.dma_start(out=ident_b, in_=identb_d), writes=["ident_b"], dma_chan="c0")
        P.add("act", lambda e: e.dma_start(out=ident_f, in_=identf_d), writes=["ident_f"], dma_chan="c1")
        P.add("act", lambda e: e.dma_start(out=iota1, in_=iota1_d), writes=["iota1"], dma_chan="c2")
        P.add("dve", lambda e: e.memset(ones_f, 1.0), writes=["ones_f"])

        def load_gain(src_row):
            P.add("act", lambda e: e.dma_start(out=G, in_=src_row.partition_broadcast(128)), writes=["G"], dma_chan="g")

        def rms_stats(src_fn, keys_fn, n):
            for i in range(n):
                P.add("act", (lambda e, i=i: e.activation(junk, src_fn(i), AF.Square, accum_out=ss[:, i:i + 1])),
                      reads=keys_fn(i), writes=["junk", ("ss", i)])
            P.add("act", lambda e: e.activation(std[:, 0:n], ss[:, 0:n], AF.Sqrt, bias=eps_ap, scale=1.0 / D),
                  reads=[("ss", i) for i in range(n)] + ["eps"], writes=["std"])
            P.add("dve", lambda e: e.reciprocal(rstd[:, 0:n], std[:, 0:n]), reads=["std"], writes=["rstd"])

        eps_ap = pa.take(4, F32)
        P.add("dve", lambda e: e.memset(eps_ap, EPS), writes=["eps"])

        ph = Arena(ph_t, PH_BYTES)
        memx = ph.take(2 * D * 4, F32, "p (i d) -> p i d", i=2)
        for i in range(2):
            P.add("sp", (lambda e, i=i: e.dma_start(out=memx[:, i, :], in_=mem_d[128 * i:128 * (i + 1), :])),
                  writes=[("memx", i)], dma_chan=("mx", i))
        load_gain(mem_norm_d[0])
        rms_stats(lambda i: memx[:, i, :], lambda i: [("memx", i)], 2)
        for i in range(2):
            xb = XN[i % 2]
            P.add("dve", (lambda e, i=i, xb=xb: e.scalar_tensor_tensor(xb, memx[:, i, :], rstd[:, i:i + 1], G, ALU.mult, ALU.mult)),
                  reads=[("memx", i), "rstd", "G"], writes=[("XN", i % 2)])
            for c in range(KC):
                P.add("pe", (lambda e, c=c, xb=xb, i=i: e.transpose(PSB[i][:, 128 * c:128 * (c + 1)], xb[:, 128 * c:128 * (c + 1)], ident_b)),
                      reads=[("XN", i % 2), "ident_b"], writes=[("ps", i)])
            P.add("act", (lambda e, i=i: e.copy(memT[:, :, 128 * i:128 * (i + 1)], PSB[i].rearrange("p (k t) -> p k t", k=KC))),
                  reads=[("ps", i)], writes=[("memT", i)])
        P.barrier()

        for l in range(n_layers):
            ph = Arena(ph_t, PH_BYTES)
            WI = [ph.take(KC * 384 * 2, BF16, "p (k c) -> p k c", k=KC) for _ in range(2)]
            mix_base = ph.off
            w_in_v = w_in_d[l].rearrange("(k p) c -> p k c", p=128)
            w_out_v = w_out_d[l].rearrange("(k p) d -> p k d", p=128)

            load_gain(norm_mix_d[l])
            P.add("act", lambda e: e.dma_start(out=cw, in_=convw_d[l]), writes=["cw"], dma_chan="cw")

            def load_wi(buf, ranges):
                off = 0
                for (c0, n) in ranges:
                    P.add("pool", (lambda e, c0=c0, n=n, off=off: e.dma_start(out=WI[buf][:, :, off:off + n], in_=w_in_v[:, :, c0:c0 + n])),
                          writes=[("WI", buf, u) for u in range(off // 128, (off + n) // 128)], dma_chan=("wi", buf, off // 128))
                    off += n

            rms_stats(lambda i: X[:, i, :], lambda i: [("X", i, 0), ("X", i, 1)], NT)
            for i in range(NT):
                xb = XN[i % 2]
                pb = i % 2
                P.add("dve", (lambda e, i=i, xb=xb: e.scalar_tensor_tensor(xb, X[:, i, :], rstd[:, i:i + 1], G, ALU.mult, ALU.mult)),
                      reads=[("X", i, 0), ("X", i, 1), "rstd", "G"], writes=[("XN", i % 2)])
                for q in range(2):
                    bk = 2 * pb + q
                    for c4 in range(4):
                        c = 4 * q + c4
                        P.add("pe", (lambda e, c=c, c4=c4: e.matmul(PS[bk][:, 128 * c4:128 * (c4 + 1)], xb[:, 128 * c:128 * (c + 1)], ident_b,
                                                                    start=True, stop=True)),
                              reads=[("XN", i % 2), "ident_b"], writes=[("ps", bk)])
                    en = "act" if q == 0 else "dve"
                    P.add(en, copy_op(en, xnT[:, 4 * q:4 * q + 4, 128 * i:128 * (i + 1)], PS[bk][:, :].rearrange("p (k t) -> p k t", k=4)),
                          reads=[("ps", bk)], writes=[("xnT", i // 4, q)])

            dump("d_xnT", A, [("xnT", tb, q) for tb in range(4) for q in range(2)])

            def proj_feat(wi_ap, wi_keys, dst_fn, dst_key_fn, bank0, nbank=2, evac_scale=None):
                for tb in range(4):
                    bk = bank0 + tb % nbank
                    for k in range(KC):
                        P.add("pe", (lambda e, k=k, tb=tb, bk=bk: e.matmul(PS[bk][:, :], wi_ap[:, k, :], xnT[:, k, 512 * tb:512 * (tb + 1)],
                                                                         start=(k == 0), stop=(k == KC - 1))),
                              reads=wi_keys + xk(tb), writes=[("ps", bk)])
                    en = alt()
                    P.add(en, copy_op(en, dst_fn(tb), PS[bk][:, :]), reads=[("ps", bk)], writes=[dst_key_fn(tb)])

            ph.off = mix_base
            WOC = ph.take(3 * D * 2, BF16, "p (c d) -> p c d", c=3)
            P.add("pool", lambda e: e.dma_start(out=WOC, in_=w_out_v[:, 0:3, :]), writes=["WOC"], dma_chan="woc")
            u_pad = ph.take((S + 2) * 4 + 8, F32)
            tmpc = ph.take(S * 4, F32)
            bg_sb = ph.take(S * 4, F32)
            cg_sb = [ph.take(512 * 4, F32) for _ in range(2)]
            convT = ph.take(3 * S * 2, BF16, "p (c t) -> p c t", c=3)
            P.add("dve", lambda e: e.memset(u_pad[:, 0:1], 0.0), writes=["u_l"])
            P.add("dve", lambda e: e.memset(u_pad[:, S + 1:S + 2], 0.0), writes=["u_r"])
            for c in range(3):
                buf = c % 2
                load_wi(buf, [(128 * c, 128), (384 + 128 * c, 128), (768 + 128 * c, 128)])
                for tb in range(4):
                    for j in range(3):
                        bk = 2 + j
                        for k in range(KC):
                            P.add("pe", (lambda e, k=k, tb=tb, bk=bk, j=j, buf=buf: e.matmul(
                                PS[bk][:, :], WI[buf][:, k, 128 * j:128 * (j + 1)], xnT[:, k, 512 * tb:512 * (tb + 1)],
                                start=(k == 0), stop=(k == KC - 1))),
                                reads=[("WI", buf, j)] + xk(tb), writes=[("ps", bk)])
                    cb = cg_sb[tb % 2]
                    P.add("act", (lambda e, cb=cb: e.copy(cb, PS[4][:, :])), reads=[("ps", 4)], writes=[("cg", tb % 2)])
                    P.add("dve", (lambda e, cb=cb, tb=tb: e.tensor_tensor(u_pad[:, 1 + 512 * tb:1 + 512 * (tb + 1)], cb, PS[2][:, :], ALU.mult)),
                          reads=[("cg", tb % 2), ("ps", 2)], writes=[("u", tb)])
                    P.add("act", (lambda e, tb=tb: e.copy(bg_sb[:, 512 * tb:512 * (tb + 1)], PS[3][:, :])),
                          reads=[("ps", 3)], writes=[("bg", tb)])
                ukeys = [("u", tb) for tb in range(4)] + ["u_l", "u_r"]
                P.add("dve", (lambda e, c=c: e.tensor_scalar(tmpc, u_pad[:, 1:S + 1], cw[:, 3 * c + 1:3 * c + 2], None, ALU.mult)),
                      reads=ukeys + ["cw"], writes=["tmpc"])
                P.add("dve", (lambda e, c=c: e.scalar_tensor_tensor(tmpc, u_pad[:, 0:S], cw[:, 3 * c:3 * c + 1], tmpc, ALU.mult, ALU.add)),
                      reads=ukeys + ["cw", "tmpc"], writes=["tmpc"])
                P.add("dve", (lambda e, c=c: e.scalar_tensor_tensor(tmpc, u_pad[:, 2:S + 2], cw[:, 3 * c + 2:3 * c + 3], tmpc, ALU.mult, ALU.add)),
                      reads=ukeys + ["cw", "tmpc"], writes=["tmpc"])
                P.add("dve", (lambda e, c=c: e.tensor_tensor(convT[:, c, :], tmpc, bg_sb, ALU.mult)),
                      reads=["tmpc"] + [("bg", tb) for tb in range(4)], writes=[("convT", c)])
            for i in range(NT):
                for hf in range(2):
                    bk = 5 + (2 * i + hf) % 3
                    for c in range(3):
                        P.add("pe", (lambda e, i=i, hf=hf, c=c, bk=bk: e.matmul(
                            PS[bk][:, :], convT[:, c, 128 * i:128 * (i + 1)], WOC[:, c, 512 * hf:512 * (hf + 1)],
                            start=(c == 0), stop=(c == 2))),
                            reads=[("convT", c), "WOC"], writes=[("ps", bk)])
                    P.add("dve", (lambda e, i=i, hf=hf, bk=bk: e.tensor_tensor(
                        X[:, i, 512 * hf:512 * (hf + 1)], X[:, i, 512 * hf:512 * (hf + 1)], PS[bk][:, :], ALU.add)),
                        reads=[("ps", bk), ("X", i, hf)], writes=[("X", i, hf)])
            dump("d_convT", convT.rearrange("p c t -> p (c t)"), [("convT", c) for c in range(3)])
            dump_x("d_x_conv")
            P.barrier()

            pending = []

            import os as _os
            DEFER = _os.environ.get("K_DEFER", "1") == "1"

            def flush_pending():
                while pending:
                    pending.pop(0)()

            def attn_finish(par, hh, qb, oT_dst, att_key, r_row, o_sb):
                bo = 3 + par
                lrow = 64 if hh == 0 else 0
                olo = 0 if hh == 0 else 64
                lnl = o_sb[0]
                bcs = o_sb[1]
                P.add("act", (lambda e: e.activation(lnl[lrow:lrow + 1, :], PS[bo][lrow:lrow + 1, :], AF.Ln)), reads=[("ps", bo)], writes=["lnl"])
                P.add("act", (lambda e: e.activation(r_row[lrow:lrow + 1, :], lnl[lrow:lrow + 1, :], AF.Exp, scale=-1.0)), reads=["lnl"], writes=["r_row"])
                mo = 64 if hh == 0 else 128
                P.add("pe", lambda e: e.matmul(PS[5][0:mo, :], ones_f[lrow:lrow + 1, 0:mo], r_row[lrow:lrow + 1, :], start=True, stop=True),
                      reads=["r_row", "ones_f"], writes=[("ps", 5)])
                P.add("dve", (lambda e: e.tensor_copy(bcs[olo:olo + 64, :], PS[5][olo:olo + 64, :])), reads=[("ps", 5)], writes=["bcs"])
                P.add("dve", (lambda e: e.tensor_tensor(oT_dst[olo:olo + 64, 512 * qb:512 * (qb + 1)], PS[bo][olo:olo + 64, :], bcs[olo:olo + 64, :], ALU.mult)),
                      reads=[("ps", bo), "bcs"], writes=[att_key + (hh, qb)])

            def attn_outproj(oT_all, npair, att_key, WO_part):
                for i in range(NT):
                    for hf in range(2):
                        bk = 6 + (2 * i + hf) % 2
                        for hp_ in range(npair):
                            P.add("pe", (lambda e, hp_=hp_: e.matmul(
                                PS[bk][:, :], oT_all[:, hp_, 128 * i:128 * (i + 1)], WO_part[:, hp_, 512 * hf:512 * (hf + 1)],
                                start=(hp_ == 0), stop=(hp_ == npair - 1))),
                                reads=[(att_key, hp_, 0, i // 4), (att_key, hp_, 1, i // 4), "WO_part"], writes=[("ps", bk)])
                        P.add("dve", (lambda e: e.tensor_tensor(
                            X[:, i, 512 * hf:512 * (hf + 1)], X[:, i, 512 * hf:512 * (hf + 1)], PS[bk][:, :], ALU.add)),
                            reads=[("ps", bk), ("X", i, hf)], writes=[("X", i, hf)])

            def proj_q_pair(wi_ap, wi_key, qz_, zkey):
                for tb in range(4):
                    bk = 6 + tb % 2
                    for k in range(KC):
                        P.add("pe", (lambda e, k=k: e.matmul(PS[bk][:, :], wi_ap[:, k, :], xnT[:, k, 512 * tb:512 * (tb + 1)],
                                                             start=(k == 0), stop=(k == KC - 1))),
                              reads=[wi_key] + xk(tb), writes=[("ps", bk)])
                    P.add("act", (lambda e: e.copy(qz_[0][0:64, 512 * tb:512 * (tb + 1)], PS[bk][0:64, :])),
                          reads=[("ps", bk), zkey + "0pad"], writes=[(zkey, 0, tb)])
                    P.add("dve", (lambda e: e.tensor_copy(qz_[1][64:128, 512 * tb:512 * (tb + 1)], PS[bk][64:128, :])),
                          reads=[("ps", bk), zkey + "1pad"], writes=[(zkey, 1, tb)])

            ph.off = mix_base
            VB = 192
            v_flat = ph.take(NT * 3 * VB * 2, BF16)
            v4 = v_flat.rearrange("p (i h c) -> p i h c", i=NT, h=3)
            qz = [ph.take(S * 2, BF16) for _ in range(2)]
            kT = [ph.take(S * 2, BF16) for _ in range(2)]
            ET = ph.take(2 * ET_W * 2, BF16, "p (h v) -> p h v", h=2)
            oT_all = ph.take(3 * S * 2, BF16, "p (h t) -> p h t", h=3)
            WOD = ph.take(3 * D * 2, BF16, "p (c d) -> p c d", c=3)
            e_sb = [ph.take(512 * 2, BF16) for _ in range(3)]
            p_sb = [ph.take(512 * 2, BF16) for _ in range(3)]
            o_sb = [ph.take(512 * 4, F32) for _ in range(2)]
            r_row = ph.take(512 * 4, F32)
            P.add("dve", lambda e: e.memset(qz[0][64:128, :], 0.0), writes=["qz0pad"])
            P.add("dve", lambda e: e.memset(qz[1][0:64, :], 0.0), writes=["qz1pad"])
            P.add("dve", lambda e: e.memset(v4[:, :, :, 64:128], 0.0), writes=["v_ones"])
            P.add("dve", lambda e: e.memset(v4[:, :, :, 64:65], 1.0), reads=["v_ones"], writes=["v_ones"])
            P.add("dve", lambda e: e.memset(v4[:, :, :, 96:97], 1.0), reads=["v_ones"], writes=["v_ones"])
            P.add("pool", lambda e: e.dma_start(out=WOD, in_=w_out_d[l][384:768, :].rearrange("(c p) d -> p c d", p=128)),
                  writes=["WO_part"], dma_chan="wod")
            load_wi(0, [(1920, 384)])
            for i in range(NT):
                bk = i % 2
                for k in range(KC):
                    P.add("pe", (lambda e, k=k: e.matmul(PS[bk][:, 0:384], xnT[:, k, 128 * i:128 * (i + 1)], WI[0][:, k, 0:384],
                                                         start=(k == 0), stop=(k == KC - 1))),
                          reads=[("WI", 0, 0), ("WI", 0, 1), ("WI", 0, 2)] + xk(i // 4), writes=[("ps", bk)])
                psv = PS[bk][:, 0:384].rearrange("p (h two c) -> p h two c", h=3, two=2)
                en = "act" if i % 2 == 0 else "dve"
                P.add(en, copy_op(en, v4[:, i, :, 0:64], psv[:, :, 0, :]), reads=[("ps", bk)], writes=[("vA", i)])
                P.add(en, copy_op(en, v4[:, i, :, 128:192], psv[:, :, 1, :]), reads=[("ps", bk)], writes=[("vB", i)])

            for hp in range(3):
                buf = (hp + 1) % 2
                load_wi(buf, [(1152 + 128 * hp, 128), (1536 + 128 * hp, 128)])
                P.add("sp", (lambda e: e.dma_start(out=ET, in_=etab_d[2 * hp:2 * hp + 2].rearrange("h p v -> p h v"))),
                      writes=["ET"], dma_chan="et")
                kb_ = kT[hp % 2]
                proj_q_pair(WI[buf][:, :, 0:128], ("WI", buf, 0), qz, "qz")
                proj_feat(WI[buf][:, :, 128:256], [("WI", buf, 1)], (lambda tb, kb_=kb_: kb_[:, 512 * tb:512 * (tb + 1)]),
                          (lambda tb, hp=hp: ("kT", hp % 2, tb)), 6)
                gi = 0
                for hh in range(2):
                    h = 2 * hp + hh
                    for qb in range(4):
                        kts = []
                        for kt in range(max(0, 4 * qb - 8), min(15, 4 * qb + 11) + 1):
                            v0 = ET_OFF - (128 * kt - 512 * qb)
                            if etab_nz[h][:, v0:v0 + 512].any():
                                kts.append(kt)
                        par = gi % 2
                        gi += 1
                        bo = 3 + par

                        def s_mm(kt, n):
                            bs = n % 3
                            P.add("pe", (lambda e: e.matmul(PS[bs][:, :], kb_[:, 128 * kt:128 * (kt + 1)],
                                                            qz[hh][:, 512 * qb:512 * (qb + 1)], start=True, stop=True)),
                                  reads=[("kT", hp % 2, kt // 4), ("qz", hh, qb), "qz%dpad" % hh], writes=[("ps", bs)])

                        s_mm(kts[0], 0)
                        if len(kts) > 1:
                            s_mm(kts[1], 1)
                        for n, kt in enumerate(kts):
                            bs = n % 3
                            v0 = ET_OFF - (128 * kt - 512 * qb)
                            P.add("act", (lambda e: e.activation(e_sb[bs], PS[bs][:, :], AF.Exp, scale=0.125)),
                                  reads=[("ps", bs)], writes=[("e_sb", bs)])
                            P.add("dve", (lambda e: e.tensor_tensor(p_sb[bs], e_sb[bs], ET[:, hh, v0:v0 + 512], ALU.mult)),
                                  reads=[("e_sb", bs), "ET"], writes=[("p_sb", bs)])
                            if n + 2 < len(kts):
                                s_mm(kts[n + 2], n + 2)
                            voff = (kt * 3 + hp) * VB + 64 * hh
                            P.add("pe", (lambda e: e.matmul(PS[bo][:, :], v_flat[:, voff:voff + 128], p_sb[bs],
                                                            start=(n == 0), stop=(n == len(kts) - 1))),
                                  reads=[("p_sb", bs), ("vA", kt), ("vB", kt), "v_ones"], writes=[("ps", bo)])
                            if n == min(1, len(kts) - 1):
                                flush_pending()
                        pending.append(lambda par=par, hh=hh, qb=qb, hp=hp: attn_finish(
                            par, hh, qb, oT_all[:, hp, :], ("oT", hp), r_row, o_sb))
                        if not DEFER:
                            flush_pending()
            flush_pending()
            attn_outproj(oT_all, 3, "oT", WOD)
            P.barrier()

            ph.off = mix_base
            if skip_mem:
                continue
            WMKV = ph.take(KC * 512 * 2, BF16, "p (k c) -> p k c", k=KC)
            kmT = ph.take(2 * MEM * 2, BF16, "p (h t) -> p h t", h=2)
            vm_flat = ph.take(2 * 2 * VB * 2, BF16)
            vm4 = vm_flat.rearrange("p (i h c) -> p i h c", i=2, h=2)
            qmz = [ph.take(S * 2, BF16) for _ in range(2)]
            oTm_all = ph.take(2 * S * 2, BF16, "p (h t) -> p h t", h=2)
            WOM = ph.take(2 * D * 2, BF16, "p (c d) -> p c d", c=2)
            p_sb = [ph.take(512 * 2, BF16) for _ in range(4)]
            o_sb = [ph.take(512 * 4, F32) for _ in range(2)]
            r_row = ph.take(512 * 4, F32)
            P.add("dve", lambda e: e.memset(qmz[0][64:128, :], 0.0), writes=["qmz0pad"])
            P.add("dve", lambda e: e.memset(qmz[1][0:64, :], 0.0), writes=["qmz1pad"])
            P.add("dve", lambda e: e.memset(vm4[:, :, :, 64:128], 0.0), writes=["vm_ones"])
            P.add("dve", lambda e: e.memset(vm4[:, :, :, 64:65], 1.0), reads=["vm_ones"], writes=["vm_ones"])
            P.add("dve", lambda e: e.memset(vm4[:, :, :, 96:97], 1.0), reads=["vm_ones"], writes=["vm_ones"])
            P.add("pool", lambda e: e.dma_start(out=WMKV, in_=w_mkv_d[l].rearrange("(k p) c -> p k c", p=128)), writes=["WMKV"], dma_chan="wmkv")
            P.add("pool", lambda e: e.dma_start(out=WOM, in_=w_out_d[l][768:1024, :].rearrange("(c p) d -> p c d", p=128)),
                  writes=["WO_part"], dma_chan="wod")
            for mp in range(2):
                for k in range(KC):
                    P.add("pe", (lambda e, k=k: e.matmul(PS[0][:, 0:MEM], WMKV[:, k, 128 * mp:128 * (mp + 1)], memT[:, k, :],
                                                         start=(k == 0), stop=(k == KC - 1))),
                          reads=["WMKV", ("memT", 0), ("memT", 1)], writes=[("ps", 0)])
                P.add("act", (lambda e: e.copy(kmT[:, mp, :], PS[0][:, 0:MEM])), reads=[("ps", 0)], writes=[("kmT", mp)])
            for i in range(2):
                for k in range(KC):
                    P.add("pe", (lambda e, k=k: e.matmul(PS[1][:, 0:256], memT[:, k, 128 * i:128 * (i + 1)], WMKV[:, k, 256:512],
                                                         start=(k == 0), stop=(k == KC - 1))),
                          reads=["WMKV", ("memT", 0), ("memT", 1)], writes=[("ps", 1)])
                psv = PS[1][:, 0:256].rearrange("p (h two c) -> p h two c", h=2, two=2)
                P.add("act", (lambda e: e.copy(vm4[:, i, :, 0:64], psv[:, :, 0, :])), reads=[("ps", 1)], writes=[("vmA", i)])
                P.add("act", (lambda e: e.copy(vm4[:, i, :, 128:192], psv[:, :, 1, :])), reads=[("ps", 1)], writes=[("vmB", i)])
            vmkeys = [("vmA", 0), ("vmA", 1), ("vmB", 0), ("vmB", 1), "vm_ones"]
            gi = 0
            for mp in range(2):
                buf = mp % 2
                load_wi(buf, [(2304 + 128 * mp, 128)])
                proj_q_pair(WI[buf][:, :, 0:128], ("WI", buf, 0), qmz, "qmz")
                for hh in range(2):
                    for qb in range(4):
                        par = gi % 2
                        sb0 = 2 * (gi % 2)
                        gi += 1
                        bo = 3 + par
                        for kt in range(2):
                            bs = sb0 + kt
                            P.add("pe", (lambda e: e.matmul(PS[bs if bs < 3 else 7][:, :], kmT[:, mp, 128 * kt:128 * (kt + 1)],
                                                            qmz[hh][:, 512 * qb:512 * (qb + 1)], start=True, stop=True)),
                                  reads=[("kmT", mp), ("qmz", hh, qb), "qmz%dpad" % hh], writes=[("ps", bs if bs < 3 else 7)])
                        for kt in range(2):
                            bs = sb0 + kt
                            pbk = bs if bs < 3 else 7
                            P.add("act", (lambda e: e.activation(p_sb[bs], PS[pbk][:, :], AF.Exp, scale=0.125)),
                                  reads=[("ps", pbk)], writes=[("p_sb", bs)])
                            if kt == 0:
                                flush_pending()
                            voff = (kt * 2 + mp) * VB + 64 * hh
                            P.add("pe", (lambda e: e.matmul(PS[bo][:, :], vm_flat[:, voff:voff + 128], p_sb[bs], start=(kt == 0), stop=(kt == 1))),
                                  reads=[("p_sb", bs)] + vmkeys, writes=[("ps", bo)])
                        pending.append(lambda par=par, hh=hh, qb=qb, mp=mp: attn_finish(
                            par, hh, qb, oTm_all[:, mp, :], ("oTm", mp), r_row, o_sb))
            flush_pending()
            attn_outproj(oTm_all, 2, "oTm", WOM)
            P.barrier()

            if not do_moe:
                continue
            ph = Arena(ph_t, PH_BYTES)
            WG = [ph.take(KC * 512 * 2, BF16, "p (k f) -> p k f", k=KC) for _ in range(2)]
            WU = [ph.take(KC * 512 * 2, BF16, "p (k f) -> p k f", k=KC) for _ in range(2)]
            WD = [ph.take(4 * D * 2, BF16, "p (c d) -> p c d", c=4) for _ in range(2)]
            WR = ph.take(KC * E * 4, F32, "p (k e) -> p k e", k=KC)
            moe_base = ph.off

            def load_expert_w(e_, fs):
                buf = (e_ * 4 + fs) % 2
                P.add("pool", (lambda e, e_=e_, fs=fs, buf=buf: e.dma_start(
                    out=WG[buf], in_=w_gate_d[l, e_].rearrange("(k p) f -> p k f", p=128)[:, :, 512 * fs:512 * (fs + 1)])),
                    writes=[("WG", buf)], dma_chan=("wg", buf))
                P.add("pool", (lambda e, e_=e_, fs=fs, buf=buf: e.dma_start(
                    out=WU[buf], in_=w_up_d[l, e_].rearrange("(k p) f -> p k f", p=128)[:, :, 512 * fs:512 * (fs + 1)])),
                    writes=[("WU", buf)], dma_chan=("wu", buf))
                P.add("pool", (lambda e, e_=e_, fs=fs, buf=buf: e.dma_start(
                    out=WD[buf], in_=w_down_d[l, e_][512 * fs:512 * (fs + 1), :].rearrange("(c p) d -> p c d", p=128))),
                    writes=[("WD", buf)], dma_chan=("wd", buf))

            load_gain(norm_ffn_d[l])
            P.add("act", lambda e: e.dma_start(out=WR, in_=w_router_d[l].rearrange("(k p) e -> p k e", p=128)), writes=["WR"], dma_chan="wr")
            load_expert_w(0, 0)
            load_expert_w(0, 1)

            affT = ph.take(S * 4, F32)
            cjunk = ph.take(S * 2, BF16)
            maskT = ph.take(S * 4, F32)
            r_base = ph.off
            xnf = [ph.take(D * 4, F32) for _ in range(2)]
            xnTf = [ph.take(KC * 128 * 4, F32, "p (k t) -> p k t", k=KC) for _ in range(2)]
            ph.off = r_base
            cums = ph.take(S * 4, F32)
            selp = ph.take(S * 4, F32)
            lg_sb = [ph.take(128 * 4, F32) for _ in range(2)]
            tq = ph.take(8 * 4, F32)
            rms_stats(lambda i: X[:, i, :], lambda i: [("X", i, 0), ("X", i, 1)], NT)

            def softmax_tile(i):
                lb = 3 + i % 2
                P.add("dve", lambda e: e.reduce_max(sm[:, 0:1], PS[lb][:, 0:E], axis=AX.X), reads=[("ps", lb)], writes=["sm0"])
                P.add("dve", lambda e: e.tensor_scalar(sm[:, 1:2], sm[:, 0:1], -1.0, None, ALU.mult), reads=["sm0"], writes=["sm1"])
                P.add("act", (lambda e: e.activation(aff_all[:, i, :], PS[lb][:, 0:E], AF.Exp, bias=sm[:, 1:2], scale=1.0, accum_out=sm[:, 2:3])),
                      reads=[("ps", lb), "sm1"], writes=[("aff", i), "sm2"])
                P.add("dve", lambda e: e.reciprocal(sm[:, 3:4], sm[:, 2:3]), reads=["sm2"], writes=["sm3"])
                P.add("dve", (lambda e: e.tensor_scalar(aff_all[:, i, :], aff_all[:, i, :], sm[:, 3:4], None, ALU.mult)),
                      reads=[("aff", i), "sm3"], writes=[("aff", i)])

            def stage_a(i):
                xf = xnf[i % 2]
                P.add("dve", (lambda e: e.scalar_tensor_tensor(xf, X[:, i, :], rstd[:, i:i + 1], G, ALU.mult, ALU.mult)),
                      reads=[("X", i, 0), ("X", i, 1), "rstd", "G"], writes=[("xnf", i % 2)])
                P.add("act", (lambda e: e.copy(xnTok[:, i, :], xf)), reads=[("xnf", i % 2)], writes=[("xnTok", i)])

            def stage_b(i):
                xf = xnf[i % 2]
                xt = xnTf[i % 2]
                lb = 3 + i % 2
                for c in range(KC):
                    bk = c // 4
                    P.add("pe", (lambda e, c=c, bk=bk: e.transpose(PS[bk][:, 128 * (c % 4):128 * (c % 4 + 1)], xf[:, 128 * c:128 * (c + 1)], ident_f)),
                          reads=[("xnf", i % 2), "ident_f"], writes=[("ps", bk)])
                P.add("act", lambda e: e.copy(xt[:, 0:4, :], PS[0][:, :].rearrange("p (k t) -> p k t", k=4)), reads=[("ps", 0)], writes=[("xnTf", i % 2, 0)])
                P.add("dve", lambda e: e.tensor_copy(xt[:, 4:8, :], PS[1][:, :].rearrange("p (k t) -> p k t", k=4)), reads=[("ps", 1)], writes=[("xnTf", i % 2, 1)])
                for k in range(KC):
                    P.add("pe", (lambda e, k=k: e.matmul(PS[2][0:E, 0:128], WR[:, k, :], xt[:, k, :], start=(k == 0), stop=(k == KC - 1))),
                          reads=[("xnTf", i % 2, k // 4), "WR"], writes=[("ps", 2)])
                P.add("act", (lambda e: e.copy(lg_sb[i % 2][0:E, :], PS[2][0:E, 0:128])), reads=[("ps", 2)], writes=[("lg", i % 2)])
                P.add("pe", (lambda e: e.transpose(PS[lb][:, 0:E], lg_sb[i % 2][0:E, :], ident_f[0:E, 0:E])),
                      reads=[("lg", i % 2), "ident_f"], writes=[("ps", lb)])

            stage_a(0)
            for i in range(NT):
                if i + 1 < NT:
                    stage_a(i + 1)
                if i > 0:
                    softmax_tile(i - 1)
                stage_b(i)
            softmax_tile(NT - 1)
            for i in range(NT):
                bk = 5 + (i // 4) % 2
                P.add("pe", (lambda e, i=i, bk=bk: e.transpose(PS[bk][0:E, 128 * (i % 4):128 * (i % 4 + 1)], aff_all[:, i, :], ident_f)),
                      reads=[("aff", i), "ident_f"], writes=[("ps", bk)])
                if i % 4 == 3:
                    j = i // 4
                    P.add("act", (lambda e, j=j, bk=bk: e.copy(affT[0:E, 512 * j:512 * (j + 1)], PS[bk][0:E, :])), reads=[("ps", bk)], writes=[("affT", j)])
            P.barrier()
            affT_keys = [("affT", j) for j in range(4)]
            P.add("dve", lambda e: e.memset(tq[0:E, 0:1], 0.0), writes=["tq_t"])
            for kbit in range(1, 31):
                step = 2.0 ** (-kbit)
                P.add("dve", lambda e: e.tensor_scalar(tq[0:E, 1:2], tq[0:E, 0:1], step, None, ALU.add), reads=["tq_t"], writes=["tq_c"])
                P.add("dve", lambda e: e.tensor_scalar(cjunk[0:E, :], affT[0:E, :], tq[0:E, 1:2], None, ALU.is_ge, ALU.add, accum_out=tq[0:E, 2:3]),
                      reads=affT_keys + ["tq_c"], writes=["cjunk", "tq_n"])
                P.add("dve", lambda e: e.tensor_scalar(tq[0:E, 3:4], tq[0:E, 2:3], CAP - 0.5, step, ALU.is_ge, ALU.mult), reads=["tq_n"], writes=["tq_g"])
                P.add("dve", lambda e: e.tensor_tensor(tq[0:E, 0:1], tq[0:E, 0:1], tq[0:E, 3:4], ALU.add), reads=["tq_t", "tq_g"], writes=["tq_t"])
            P.add("dve", lambda e: e.tensor_scalar(maskT[0:E, :], affT[0:E, :], tq[0:E, 0:1], None, ALU.is_ge), reads=affT_keys + ["tq_t"], writes=["maskT"])
            P.add("dve", lambda e: e.tensor_tensor_scan(cums[0:E, :], maskT[0:E, :], maskT[0:E, :], 0.0, ALU.add, ALU.max),
                  reads=["maskT"], writes=["cums"])
            P.add("dve", lambda e: e.tensor_tensor(selp[0:E, :], cums[0:E, :], maskT[0:E, :], ALU.mult), reads=["cums", "maskT"], writes=["selp"])
            for i in range(NT):
                P.add("pe", (lambda e, i=i: e.transpose(PS[7][:, E * i:E * (i + 1)], selp[0:E, 128 * i:128 * (i + 1)], ident_f[0:E, 0:E])),
                      reads=["selp", "ident_f"], writes=[("ps", 7)])
            P.add("act", lambda e: e.copy(sel_all, PS[7][:, 0:NT * E].rearrange("p (i e) -> p i e", i=NT)), reads=[("ps", 7)], writes=["sel"])
            P.barrier()

            ph.off = moe_base
            Ssel = ph.take(NT * CAP * 2, BF16, "p (i j) -> p i j", i=NT)
            Sg = ph.take(8 * CAP * 2, BF16, "p (i j) -> p i j", i=8)
            SgT = [ph.take(2 * S * 2, BF16, "p (j t) -> p j t", j=2) for _ in range(2)]
            xgT = ph.take(KC * CAP * 2, BF16, "p (k j) -> p k j", k=KC)
            hT = [ph.take(4 * CAP * 2, BF16, "p (c j) -> p c j", c=4) for _ in range(2)]
            sg_sb = [ph.take(CAP * 4, F32) for _ in range(2)]
            y_sb = ph.take(2 * D * 2, BF16, "p (j d) -> p j d", j=2)
            xg_keys = [("xgT", c) for c in range(KC)]

            def scatter_unit(es, u):
                i, hf = u // 2, u % 2
                bk = 6 + u % 2
                for jc in range(2):
                    P.add("pe", (lambda e, jc=jc: e.matmul(
                        PS[bk][:, :], SgT[es % 2][:, jc, 128 * i:128 * (i + 1)], y_sb[:, jc, 512 * hf:512 * (hf + 1)], start=(jc == 0), stop=(jc == 1))),
                        reads=[("SgT", es % 2, jc, i // 8, (i % 8) // 4), ("y", jc, hf)], writes=[("ps", bk)])
                P.add("dve", (lambda e: e.tensor_tensor(
                    X[:, i, 512 * hf:512 * (hf + 1)], X[:, i, 512 * hf:512 * (hf + 1)], PS[bk][:, :], ALU.add)),
                    reads=[("ps", bk), ("X", i, hf)], writes=[("X", i, hf)])

            def build_A(e_):
                th = []
                for i in range(NT):
                    th.append(lambda i=i: P.add("dve", (lambda e: e.tensor_scalar(Ssel[:, i, :], iota1, sel_all[:, i, e_:e_ + 1], None, ALU.is_equal)),
                                                reads=["iota1", "sel"], writes=[("Ssel", i)]))
                for ii in range(8):
                    th.append(lambda ii=ii: P.add("dve", (lambda e: e.tensor_scalar(Sg[:, ii, :], iota1, sel_all[:, ii, e_:e_ + 1], aff_all[:, ii, e_:e_ + 1],
                                                                                    ALU.is_equal, ALU.mult)),
                                                  reads=["iota1", "sel", ("aff", ii)], writes=[("Sg", ii)]))
                return th

            def build_B(e_):
                for ii in range(8):
                    i = 8 + ii
                    P.add("dve", (lambda e: e.tensor_scalar(Sg[:, ii, :], iota1, sel_all[:, i, e_:e_ + 1], aff_all[:, i, e_:e_ + 1],
                                                            ALU.is_equal, ALU.mult)),
                          reads=["iota1", "sel", ("aff", i)], writes=[("Sg", ii)])

            def sgt_half(e_, half):
                for jc in range(2):
                    for q in range(2):
                        bk = 6 + q
                        for i4 in range(4):
                            ii = 4 * q + i4
                            P.add("pe", (lambda e: e.matmul(PS[bk][:, 128 * i4:128 * (i4 + 1)], Sg[:, ii, 128 * jc:128 * (jc + 1)], ident_b,
                                                            start=True, stop=True)),
                                  reads=[("Sg", ii), "ident_b"], writes=[("ps", bk)])
                        en = alt()
                        c0 = 1024 * half + 512 * q
                        P.add(en, copy_op(en, SgT[e_ % 2][:, jc, c0:c0 + 512], PS[bk][:, :]), reads=[("ps", bk)],
                              writes=[("SgT", e_ % 2, jc, half, q)])

            for t_ in build_A(0):
                t_()
            for e_ in range(E):
                sgt_half(e_, 0)
                build_B(e_)
                for c in range(KC):
                    bk = 6 + c % 2
                    for i in range(NT):
                        P.add("pe", (lambda e, c=c, i=i, bk=bk: e.matmul(PS[bk][:, 0:CAP], xnTok[:, i, 128 * c:128 * (c + 1)], Ssel[:, i, :],
                                                                         start=(i == 0), stop=(i == NT - 1))),
                              reads=[("xnTok", i), ("Ssel", i)], writes=[("ps", bk)])
                    en = alt()
                    P.add(en, copy_op(en, xgT[:, c, :], PS[bk][:, 0:CAP]), reads=[("ps", bk)], writes=[("xgT", c)])
                sgt_half(e_, 1)
                nextA = build_A(e_ + 1) if e_ + 1 < E else []

                def gate_up(f):
                    fs, fc = f // 4, f % 4
                    buf = (e_ * 4 + fs) % 2
                    bk = 4 + f % 2
                    for k in range(KC):
                        P.add("pe", (lambda e, k=k: e.matmul(PS[bk][:, 0:CAP], WG[buf][:, k, 128 * fc:128 * (fc + 1)], xgT[:, k, :],
                                                             start=(k == 0), stop=(k == KC - 1))),
                              reads=[("WG", buf)] + xg_keys, writes=[("ps", bk)])
                    for k in range(KC):
                        P.add("pe", (lambda e, k=k: e.matmul(PS[bk][:, CAP:2 * CAP], WU[buf][:, k, 128 * fc:128 * (fc + 1)], xgT[:, k, :],
                                                             start=(k == 0), stop=(k == KC - 1))),
                              reads=[("WU", buf)] + xg_keys, writes=[("ps", bk)])

                gate_up(0)
                for f in range(16):
                    fs, fc = f // 4, f % 4
                    buf = (e_ * 4 + fs) % 2
                    bk = 4 + f % 2
                    hb = hT[fs % 2]
                    sgb = sg_sb[f % 2]
                    P.add("act", (lambda e: e.activation(sgb, PS[bk][:, 0:CAP], AF.Silu)), reads=[("ps", bk)], writes=[("sg", f % 2)])
                    P.add("dve", (lambda e: e.tensor_tensor(hb[:, fc, :], sgb, PS[bk][:, CAP:2 * CAP], ALU.mult)),
                          reads=[("sg", f % 2), ("ps", bk)], writes=[("hT", fs % 2, fc)])
                    if f + 1 < 16:
                        gate_up(f + 1)
                    for jc in range(2):
                        for hf in range(2):
                            by = 2 * jc + hf
                            P.add("pe", (lambda e, jc=jc, hf=hf, by=by: e.matmul(
                                PS[by][:, :], hb[:, fc, 128 * jc:128 * (jc + 1)], WD[buf][:, fc, 512 * hf:512 * (hf + 1)],
                                start=(f == 0), stop=(f == 15))),
                                reads=[("hT", fs % 2, fc), ("WD", buf)], writes=[("ps", by)])
                    if e_ > 0:
                        scatter_unit(e_ - 1, 2 * f)
                        scatter_unit(e_ - 1, 2 * f + 1)
                    for _ in range(2):
                        if nextA:
                            nextA.pop(0)()
                    if fc == 3:
                        nxt = e_ * 4 + fs + 2
                        if nxt < E * 4:
                            load_expert_w(nxt // 4, nxt % 4)
                for jc in range(2):
                    for hf in range(2):
                        by = 2 * jc + hf
                        en = alt()
                        P.add(en, copy_op(en, y_sb[:, jc, 512 * hf:512 * (hf + 1)], PS[by][:, :]), reads=[("ps", by)], writes=[("y", jc, hf)])
            for u in range(32):
                scatter_unit(E - 1, u)
            P.barrier()

        ph = Arena(ph_t, PH_BYTES)
        ob = [ph.take(D * 4, F32) for _ in range(4)]
        if debug:
            for i in range(NT):
                final_ops.append(P.add("sp", (lambda e, i=i: e.dma_start(out=out_d[128 * i:128 * (i + 1), :], in_=X[:, i, :])),
                                       reads=[("X", i, 0), ("X", i, 1)], dma_chan="dbg", extra_deps=final_ops[-1:]))
        else:
            load_gain(norm_final_d[0])
            rms_stats(lambda i: X[:, i, :], lambda i: [("X", i, 0), ("X", i, 1)], NT)
            for i in range(NT):
                P.add("dve", (lambda e, i=i: e.scalar_tensor_tensor(ob[i % 4], X[:, i, :], rstd[:, i:i + 1], G, ALU.mult, ALU.mult)),
                      reads=[("X", i, 0), ("X", i, 1), "rstd", "G"], writes=[("ob", i % 4)])
                final_ops.append(P.add("sp", (lambda e, i=i: e.dma_start(out=out_d[128 * i:128 * (i + 1), :], in_=ob[i % 4])),
                                       reads=[("ob", i % 4)], writes=[("obd", i % 4)], dma_chan=("o", i % 4)))
        P.emit(nc, final_waits=[("sp", o) for o in final_ops])
    return nc


def _etab():
    t = np.zeros((6, 128, ET_W), np.float64)
    p = np.arange(128)[:, None]
    v = np.arange(ET_W)[None, :]
    d = p - v + ET_OFF
    ad = np.abs(d)
    m = (ad <= 64).astype(np.float64) + ((d % 4 == 0) & (ad <= 256)) + ((d % 16 == 0) & (ad <= 1024))
    for h in range(6):
        slope = 2.0 ** (-8.0 * (h + 1) / 6)
        t[h] = m * np.exp(-slope * ad)
    return t.astype(np.float32).astype(ml_dtypes.bfloat16)


def _consts():
    return {
        "ident_bf": np.eye(128, dtype=np.float32).astype(ml_dtypes.bfloat16),
        "ident_f": np.eye(128, dtype=np.float32),
        "iota1": np.tile(np.arange(1, CAP + 1, dtype=np.float32)[None, :], (128, 1)),
        "etab": _etab(),
    }


def make_in_maps(x, mem, mem_norm, norm_mix, w_in, conv_w, w_mem_kv, w_out, norm_ffn,
                 w_router, w_gate, w_up, w_down, norm_final):
    f = lambda a: np.ascontiguousarray(np.asarray(a, dtype=np.float32))
    conv_w = f(conv_w)
    convw_t = np.ascontiguousarray(conv_w.reshape(L, 3, 3, 128).transpose(0, 3, 2, 1).reshape(L, 128, 9))
    shared = {
        "mem_norm": f(mem_norm).reshape(1, D), "norm_mix": f(norm_mix), "norm_ffn": f(norm_ffn),
        "norm_final": f(norm_final).reshape(1, D), "w_in": f(w_in), "convw_t": convw_t,
        "w_mem_kv": f(w_mem_kv), "w_out": f(w_out), "w_router": f(w_router),
        "w_gate": f(w_gate), "w_up": f(w_up), "w_down": f(w_down),
    }
    shared.update(_consts())
    x = f(x)
    mem = f(mem)
    return [dict(shared, x=x[b], mem=mem[b]) for b in range(N_CORES)]


def kernel(x, mem, mem_norm, norm_mix, w_in, conv_w, w_mem_kv, w_out, norm_ffn,
           w_router, w_gate, w_up, w_down, norm_final):
    in_maps = make_in_maps(x, mem, mem_norm, norm_mix, w_in, conv_w, w_mem_kv, w_out, norm_ffn,
                           w_router, w_gate, w_up, w_down, norm_final)
    nc = build_program()
    res = run_bass_kernel_spmd(nc, in_maps, core_ids=list(range(N_CORES)))
    return np.stack([np.asarray(r["out"], dtype=np.float32) for r in res.results], axis=0)
```

```python
import contextlib
import numpy as np
import ml_dtypes
import concourse.bass as bass
import concourse.mybir as mybir
from concourse.bass_utils import run_bass_kernel_spmd

F32 = mybir.dt.float32
BF16 = mybir.dt.bfloat16
AF = mybir.ActivationFunctionType
ALU = mybir.AluOpType
AX = mybir.AxisListType

D = 1024
S = 2048
NT = 16
KC = 8
L = 2
E = 16
FF = 2048
CAP = 256
MEM = 256
EPS = 1e-6
ET_W = 2944
ET_OFF = 1408
N_CORES = 8


class _Rec:
    def __init__(self):
        self.call = None

    def __getattr__(self, name):
        def f(*a, **k):
            self.call = (name, a, k)
            return self
        return f


class Prog:
    def __init__(self):
        self.ops = []
        self.last_writer = {}
        self.readers = {}
        self.chan_count = {}
        self.last_eng = {}
        self.last_chan = {}

    def add(self, eng, fn, reads=(), writes=(), dma_chan=None, extra_deps=()):
        idx = len(self.ops)
        deps = set(extra_deps)
        for k in reads:
            w = self.last_writer.get(k)
            if w is not None:
                deps.add(w)
        for k in writes:
            w = self.last_writer.get(k)
            if w is not None:
                deps.add(w)
            for r in self.readers.get(k, {}).values():
                deps.add(r)
        deps.discard(idx)
        rkey = eng if dma_chan is None else ("dma", idx)
        for k in reads:
            self.readers.setdefault(k, {})[rkey] = idx
        for k in writes:
            self.last_writer[k] = idx
            self.readers[k] = {}
        if fn is not None:
            rec = _Rec()
            fn(rec)
            call = rec.call
            assert call is not None
            fn = (lambda e, call=call: getattr(e, call[0])(*call[1], **call[2]))
        op = dict(eng=eng, fn=fn, deps=deps, chan=dma_chan, signal=False)
        if dma_chan is not None:
            self.chan_count[dma_chan] = self.chan_count.get(dma_chan, 0) + 16
            op["dmaval"] = self.chan_count[dma_chan]
            self.last_chan[dma_chan] = idx
        elif fn is not None:
            self.last_eng[eng] = idx
        self.ops.append(op)
        return idx

    def barrier(self):
        deps = set(self.last_eng.values()) | set(self.last_chan.values())
        for e in ["pe", "act", "dve", "pool", "sp"]:
            self.add(e, None, extra_deps=deps)

    def emit(self, nc, final_waits=()):
        ops = self.ops
        for op in ops:
            for d in op["deps"]:
                p = ops[d]
                if p["chan"] is None:
                    if p["eng"] == "pe" and op["eng"] == "pe" and op["chan"] is None and op["fn"] is not None:
                        continue
                    p["signal"] = True
        for (_, fo) in final_waits:
            if ops[fo]["chan"] is None:
                ops[fo]["signal"] = True
        engs = ["pe", "act", "dve", "pool", "sp"]
        seq = {e: 0 for e in engs}
        for op in ops:
            if op["chan"] is None and op["signal"]:
                seq[op["eng"]] += 1
                op["seqval"] = seq[op["eng"]]
        chans = sorted(self.chan_count.keys(), key=str)
        with contextlib.ExitStack() as st:
            esem = {e: st.enter_context(nc.semaphore("s_" + e)) for e in engs}
            csem = {c: st.enter_context(nc.semaphore("c_%d" % i)) for i, c in enumerate(chans)}
            block = st.enter_context(nc.Block())

            def run_engine(ename):
                def body(eng):
                    waited = {}
                    for op in ops:
                        if op["eng"] != ename:
                            continue
                        for d in sorted(op["deps"]):
                            p = ops[d]
                            if p["chan"] is not None:
                                key = ("c", p["chan"]); val = p["dmaval"]; sem = csem[p["chan"]]
                            else:
                                if p["eng"] == "pe" and ename == "pe" and op["chan"] is None and op["fn"] is not None:
                                    continue
                                key = ("e", p["eng"]); val = p["seqval"]; sem = esem[p["eng"]]
                            if waited.get(key, 0) >= val:
                                continue
                            eng.wait_ge(sem, val)
                            waited[key] = val
                        if op["fn"] is None:
                            continue
                        ins = op["fn"](eng)
                        if op["chan"] is not None:
                            ins.then_inc(csem[op["chan"]], 16)
                        elif op["signal"]:
                            ins.then_inc(esem[ename], 1)
                    for (e2, fo) in final_waits:
                        if e2 != ename:
                            continue
                        p = ops[fo]
                        if p["chan"] is not None:
                            eng.wait_ge(csem[p["chan"]], p["dmaval"])
                        else:
                            eng.wait_ge(esem[p["eng"]], p["seqval"])
                return body

            block.tensor(run_engine("pe"))
            block.scalar(run_engine("act"))
            block.vector(run_engine("dve"))
            block.gpsimd(run_engine("pool"))
            block.sync(run_engine("sp"))


class Arena:
    def __init__(self, t, nbytes):
        self.t = t
        self.n = nbytes
        self.off = 0
        self.mark = 0

    def take(self, nbytes, dt=F32, pattern=None, **kw):
        off = self.off
        self.off += (nbytes + 63) // 64 * 64
        assert self.off <= self.n, ("arena overflow", self.off, self.n)
        ap = self.t[:, off // 4:(off + nbytes) // 4]
        if dt != F32:
            ap = ap.bitcast(dt)
        if pattern:
            ap = ap.rearrange(pattern, **kw)
        return ap


def build_program(n_layers=L, do_moe=True, debug=False, skip_mem=False, skip_dil=False):
    nc = bass.Bass("TRN2", target_bir_lowering=False)

    def din(name, shape, dt=F32):
        return nc.dram_tensor(name, list(shape), dt, kind="ExternalInput").ap()

    x_d = din("x", [S, D])
    mem_d = din("mem", [MEM, D])
    mem_norm_d = din("mem_norm", [1, D])
    norm_mix_d = din("norm_mix", [L, D])
    norm_ffn_d = din("norm_ffn", [L, D])
    norm_final_d = din("norm_final", [1, D])
    w_in_d = din("w_in", [L, D, 2560])
    convw_d = din("convw_t", [L, 128, 9])
    w_mkv_d = din("w_mem_kv", [L, D, 512])
    w_out_d = din("w_out", [L, D, D])
    if do_moe:
        w_router_d = din("w_router", [L, D, E])
        w_gate_d = din("w_gate", [L, E, D, FF])
        w_up_d = din("w_up", [L, E, D, FF])
        w_down_d = din("w_down", [L, E, FF, D])
    identb_d = din("ident_bf", [128, 128], BF16)
    identf_d = din("ident_f", [128, 128])
    iota1_d = din("iota1", [128, CAP])
    etab_d = din("etab", [6, 128, ET_W], BF16)
    out_d = nc.dram_tensor("out", [S, D], F32, kind="ExternalOutput").ap()
    dbg = {}
    if debug:
        def dout(name, shape, dt=F32):
            dbg[name] = nc.dram_tensor(name, list(shape), dt, kind="ExternalOutput").ap()
        dout("d_xnT", [128, KC * S], BF16)
        dout("d_x_conv", [S, D])
        dout("d_convT", [128, 3 * S], BF16)
        dout("d_qT0", [128, S], BF16)
        dout("d_kT0", [128, S], BF16)
        dout("d_v", [128, NT * 6 * 65], BF16)
        dout("d_oT0", [64, 2 * S], BF16)
        dout("d_x_dil", [S, D])
        dout("d_osb", [65, 512])
        dout("d_psb", [128, 512], BF16)
        dout("d_ET", [128, 2 * ET_W], BF16)
        dout("d_esb", [128, 512], BF16)
        dout("d_S", [128, 512])
        dout("d_psb0", [128, 512], BF16)

    etab_nz = _etab().astype(np.float32) != 0
    PERS_BYTES = 114 * 1024 + 768
    PH_BYTES = 92 * 1024 - 768

    with contextlib.ExitStack() as st:
        pers_t = st.enter_context(nc.sbuf_tensor("pers", [128, PERS_BYTES // 4], F32))
        ph_t = st.enter_context(nc.sbuf_tensor("phase", [128, PH_BYTES // 4], F32))
        PS = [st.enter_context(nc.psum_tensor("ps%d" % i, [128, 512], F32)) for i in range(8)]
        PSB = [p[:].bitcast(BF16) for p in PS]

        pa = Arena(pers_t, PERS_BYTES)
        X = pa.take(NT * D * 4, F32, "p (i d) -> p i d", i=NT)
        A = pa.take(NT * D * 2, BF16)
        xnT = A.rearrange("p (k t) -> p k t", k=KC)
        xnTok = A.rearrange("p (i d) -> p i d", i=NT)
        ident_b = pa.take(128 * 2, BF16)
        ident_f = pa.take(128 * 4, F32)
        iota1 = pa.take(CAP * 4, F32)
        ones_f = pa.take(128 * 4, F32)
        memT = pa.take(KC * MEM * 2, BF16, "p (k t) -> p k t", k=KC)
        G = pa.take(D * 4, F32)
        junk = pa.take(D * 2, BF16)
        XN = [pa.take(D * 2, BF16), pa.take(D * 2, BF16)]
        ss = pa.take(NT * 4, F32)
        std = pa.take(NT * 4, F32)
        rstd = pa.take(NT * 4, F32)
        cw = pa.take(9 * 4, F32)
        aff_all = pa.take(NT * E * 4, F32, "p (i e) -> p i e", i=NT)
        sel_all = pa.take(NT * E * 4, F32, "p (i e) -> p i e", i=NT)
        sm = pa.take(16 * 4, F32)

        P = Prog()
        cnt = [0]

        def xk(tb):
            return [("xnT", tb, 0), ("xnT", tb, 1)]

        def alt():
            cnt[0] += 1
            return "act" if cnt[0] % 2 == 0 else "dve"

        def copy_op(engname, out, in_):
            if engname == "act":
                return lambda e: e.copy(out, in_)
            return lambda e: e.tensor_copy(out, in_)

        def dump(name, src, reads, idx=None):
            if not debug or l != 0:
                return
            dst = dbg[name] if idx is None else dbg[name][idx]
            final_ops.append(P.add("sp", (lambda e: e.dma_start(out=dst, in_=src)), reads=reads, dma_chan="dbg", extra_deps=final_ops[-1:]))

        def dump_x(name):
            if not debug or l != 0:
                return
            for i in range(NT):
                final_ops.append(P.add("sp", (lambda e, i=i: e.dma_start(out=dbg[name][128 * i:128 * (i + 1), :], in_=X[:, i, :])),
                                       reads=[("X", i, 0), ("X", i, 1)], dma_chan="dbg", extra_deps=final_ops[-1:]))

        final_ops = []
        l = 0
        for i in range(NT):
            P.add("sp", (lambda e, i=i: e.dma_start(out=X[:, i, :], in_=x_d[128 * i:128 * (i + 1), :])),
                  writes=[("X", i, 0), ("X", i, 1)], dma_chan=("x", i))
        P.add("act", lambda e: e.dma_start(out=ident_b, in_=identb_d), writes=["ident_b"], dma_chan="c0")
        P.add("act", lambda e: e.dma_start(out=ident_f, in_=identf_d), writes=["ident_f"], dma_chan="c1")
        P.add("act", lambda e: e.dma_start(out=iota1, in_=iota1_d), writes=["iota1"], dma_chan="c2")
        P.add("dve", lambda e: e.memset(ones_f, 1.0), writes=["ones_f"])

        def load_gain(src_row):
            P.add("act", lambda e: e.dma_start(out=G, in_=src_row.partition_broadcast(128)), writes=["G"], dma_chan="g")

        def rms_stats(src_fn, keys_fn, n):
            for i in range(n):
                P.add("act", (lambda e, i=i: e.activation(junk, src_fn(i), AF.Square, accum_out=ss[:, i:i + 1])),
                      reads=keys_fn(i), writes=["junk", ("ss", i)])
            P.add("act", lambda e: e.activation(std[:, 0:n], ss[:, 0:n], AF.Sqrt, bias=eps_ap, scale=1.0 / D),
                  reads=[("ss", i) for i in range(n)] + ["eps"], writes=["std"])
            P.add("dve", lambda e: e.reciprocal(rstd[:, 0:n], std[:, 0:n]), reads=["std"], writes=["rstd"])

        eps_ap = pa.take(4, F32)
        P.add("dve", lambda e: e.memset(eps_ap, EPS), writes=["eps"])

        ph = Arena(ph_t, PH_BYTES)
        memx = ph.take(2 * D * 4, F32, "p (i d) -> p i d", i=2)
        for i in range(2):
            P.add("sp", (lambda e, i=i: e.dma_start(out=memx[:, i, :], in_=mem_d[128 * i:128 * (i + 1), :])),
                  writes=[("memx", i)], dma_chan=("mx", i))
        load_gain(mem_norm_d[0])
        rms_stats(lambda i: memx[:, i, :], lambda i: [("memx", i)], 2)
        for i in range(2):
            xb = XN[i % 2]
            P.add("dve", (lambda e, i=i, xb=xb: e.scalar_tensor_tensor(xb, memx[:, i, :], rstd[:, i:i + 1], G, ALU.mult, ALU.mult)),
                  reads=[("memx", i), "rstd", "G"], writes=[("XN", i % 2)])
            for c in range(KC):
                P.add("pe", (lambda e, c=c, xb=xb, i=i: e.transpose(PSB[i][:, 128 * c:128 * (c + 1)], xb[:, 128 * c:128 * (c + 1)], ident_b)),
                      reads=[("XN", i % 2), "ident_b"], writes=[("ps", i)])
            P.add("act", (lambda e, i=i: e.copy(memT[:, :, 128 * i:128 * (i + 1)], PSB[i].rearrange("p (k t) -> p k t", k=KC))),
                  reads=[("ps", i)], writes=[("memT", i)])
        P.barrier()

        for l in range(n_layers):
            ph = Arena(ph_t, PH_BYTES)
            WI = [ph.take(KC * 384 * 2, BF16, "p (k c) -> p k c", k=KC) for _ in range(2)]
            mix_base = ph.off
            w_in_v = w_in_d[l].rearrange("(k p) c -> p k c", p=128)
            w_out_v = w_out_d[l].rearrange("(k p) d -> p k d", p=128)

            load_gain(norm_mix_d[l])
            P.add("act", lambda e: e.dma_start(out=cw, in_=convw_d[l]), writes=["cw"], dma_chan="cw")

            def load_wi(buf, ranges):
                off = 0
                for (c0, n) in ranges:
                    P.add("pool", (lambda e, c0=c0, n=n, off=off: e.dma_start(out=WI[buf][:, :, off:off + n], in_=w_in_v[:, :, c0:c0 + n])),
                          writes=[("WI", buf, u) for u in range(off // 128, (off + n) // 128)], dma_chan=("wi", buf, off // 128))
                    off += n

            rms_stats(lambda i: X[:, i, :], lambda i: [("X", i, 0), ("X", i, 1)], NT)
            for i in range(NT):
                xb = XN[i % 2]
                pb = i % 2
                P.add("dve", (lambda e, i=i, xb=xb: e.scalar_tensor_tensor(xb, X[:, i, :], rstd[:, i:i + 1], G, ALU.mult, ALU.mult)),
                      reads=[("X", i, 0), ("X", i, 1), "rstd", "G"], writes=[("XN", i % 2)])
                for q in range(2):
                    bk = 2 * pb + q
                    for c4 in range(4):
                        c = 4 * q + c4
                        P.add("pe", (lambda e, c=c, c4=c4: e.matmul(PS[bk][:, 128 * c4:128 * (c4 + 1)], xb[:, 128 * c:128 * (c + 1)], ident_b,
                                                                    start=True, stop=True)),
                              reads=[("XN", i % 2), "ident_b"], writes=[("ps", bk)])
                    en = "act" if q == 0 else "dve"
                    P.add(en, copy_op(en, xnT[:, 4 * q:4 * q + 4, 128 * i:128 * (i + 1)], PS[bk][:, :].rearrange("p (k t) -> p k t", k=4)),
                          reads=[("ps", bk)], writes=[("xnT", i // 4, q)])

            dump("d_xnT", A, [("xnT", tb, q) for tb in range(4) for q in range(2)])

            def proj_feat(wi_ap, wi_keys, dst_fn, dst_key_fn, bank0, nbank=2, evac_scale=None):
                for tb in range(4):
                    bk = bank0 + tb % nbank
                    for k in range(KC):
                        P.add("pe", (lambda e, k=k, tb=tb, bk=bk: e.matmul(PS[bk][:, :], wi_ap[:, k, :], xnT[:, k, 512 * tb:512 * (tb + 1)],
                                                                         start=(k == 0), stop=(k == KC - 1))),
                              reads=wi_keys + xk(tb), writes=[("ps", bk)])
                    en = alt()
                    P.add(en, copy_op(en, dst_fn(tb), PS[bk][:, :]), reads=[("ps", bk)], writes=[dst_key_fn(tb)])

            ph.off = mix_base
            WOC = ph.take(3 * D * 2, BF16, "p (c d) -> p c d", c=3)
            P.add("pool", lambda e: e.dma_start(out=WOC, in_=w_out_v[:, 0:3, :]), writes=["WOC"], dma_chan="woc")
            u_pad = ph.take((S + 2) * 4 + 8, F32)
            tmpc = ph.take(S * 4, F32)
            bg_sb = ph.take(S * 4, F32)
            cg_sb = [ph.take(512 * 4, F32) for _ in range(2)]
            convT = ph.take(3 * S * 2, BF16, "p (c t) -> p c t", c=3)
            P.add("dve", lambda e: e.memset(u_pad[:, 0:1], 0.0), writes=["u_l"])
            P.add("dve", lambda e: e.memset(u_pad[:, S + 1:S + 2], 0.0), writes=["u_r"])
            for c in range(3):
                buf = c % 2
                load_wi(buf, [(128 * c, 128), (384 + 128 * c, 128), (768 + 128 * c, 128)])
                for tb in range(4):
                    for j in range(3):
                        bk = 2 + j
                        for k in range(KC):
                            P.add("pe", (lambda e, k=k, tb=tb, bk=bk, j=j, buf=buf: e.matmul(
                                PS[bk][:, :], WI[buf][:, k, 128 * j:128 * (j + 1)], xnT[:, k, 512 * tb:512 * (tb + 1)],
                                start=(k == 0), stop=(k == KC - 1))),
                                reads=[("WI", buf, j)] + xk(tb), writes=[("ps", bk)])
                    cb = cg_sb[tb % 2]
                    P.add("act", (lambda e, cb=cb: e.copy(cb, PS[4][:, :])), reads=[("ps", 4)], writes=[("cg", tb % 2)])
                    P.add("dve", (lambda e, cb=cb, tb=tb: e.tensor_tensor(u_pad[:, 1 + 512 * tb:1 + 512 * (tb + 1)], cb, PS[2][:, :], ALU.mult)),
                          reads=[("cg", tb % 2), ("ps", 2)], writes=[("u", tb)])
                    P.add("act", (lambda e, tb=tb: e.copy(bg_sb[:, 512 * tb:512 * (tb + 1)], PS[3][:, :])),
                          reads=[("ps", 3)], writes=[("bg", tb)])
                ukeys = [("u", tb) for tb in range(4)] + ["u_l", "u_r"]
                P.add("dve", (lambda e, c=c: e.tensor_scalar(tmpc, u_pad[:, 1:S + 1], cw[:, 3 * c + 1:3 * c + 2], None, ALU.mult)),
                      reads=ukeys + ["cw"], writes=["tmpc"])
                P.add("dve", (lambda e, c=c: e.scalar_tensor_tensor(tmpc, u_pad[:, 0:S], cw[:, 3 * c:3 * c + 1], tmpc, ALU.mult, ALU.add)),
                      reads=ukeys + ["cw", "tmpc"], writes=["tmpc"])
                P.add("dve", (lambda e, c=c: e.scalar_tensor_tensor(tmpc, u_pad[:, 2:S + 2], cw[:, 3 * c + 2:3 * c + 3], tmpc, ALU.mult, ALU.add)),
                      reads=ukeys + ["cw", "tmpc"], writes=["tmpc"])
                P.add("dve", (lambda e, c=c: e.tensor_tensor(convT[:, c, :], tmpc, bg_sb, ALU.mult)),
                      reads=["tmpc"] + [("bg", tb) for tb in range(4)], writes=[("convT", c)])
            for i in range(NT):
                for hf in range(2):
                    bk = 5 + (2 * i + hf) % 3
                    for c in range(3):
                        P.add("pe", (lambda e, i=i, hf=hf, c=c, bk=bk: e.matmul(
                            PS[bk][:, :], convT[:, c, 128 * i:128 * (i + 1)], WOC[:, c, 512 * hf:512 * (hf + 1)],
                            start=(c == 0), stop=(c == 2))),
                            reads=[("convT", c), "WOC"], writes=[("ps", bk)])
                    P.add("dve", (lambda e, i=i, hf=hf, bk=bk: e.tensor_tensor(
                        X[:, i, 512 * hf:512 * (hf + 1)], X[:, i, 512 * hf:512 * (hf + 1)], PS[bk][:, :], ALU.add)),
                        reads=[("ps", bk), ("X", i, hf)], writes=[("X", i, hf)])
            dump("d_convT", convT.rearrange("p c t -> p (c t)"), [("convT", c) for c in range(3)])
            dump_x("d_x_conv")
            P.barrier()

            pending = []

            import os as _os
            DEFER = _os.environ.get("K_DEFER", "1") == "1"

            def flush_pending():
                while pending:
                    pending.pop(0)()

            def attn_finish(par, hh, qb, oT_dst, att_key, r_row, o_sb):
                bo = 3 + par
                lrow = 64 if hh == 0 else 0
                olo = 0 if hh == 0 else 64
                lnl = o_sb[0]
                bcs = o_sb[1]
                P.add("act", (lambda e: e.activation(lnl[lrow:lrow + 1, :], PS[bo][lrow:lrow + 1, :], AF.Ln)), reads=[("ps", bo)], writes=["lnl"])
                P.add("act", (lambda e: e.activation(r_row[lrow:lrow + 1, :], lnl[lrow:lrow + 1, :], AF.Exp, scale=-1.0)), reads=["lnl"], writes=["r_row"])
                mo = 64 if hh == 0 else 128
                P.add("pe", lambda e: e.matmul(PS[5][0:mo, :], ones_f[lrow:lrow + 1, 0:mo], r_row[lrow:lrow + 1, :], start=True, stop=True),
                      reads=["r_row", "ones_f"], writes=[("ps", 5)])
                P.add("dve", (lambda e: e.tensor_copy(bcs[olo:olo + 64, :], PS[5][olo:olo + 64, :])), reads=[("ps", 5)], writes=["bcs"])
                P.add("dve", (lambda e: e.tensor_tensor(oT_dst[olo:olo + 64, 512 * qb:512 * (qb + 1)], PS[bo][olo:olo + 64, :], bcs[olo:olo + 64, :], ALU.mult)),
                      reads=[("ps", bo), "bcs"], writes=[att_key + (hh, qb)])

            def attn_outproj(oT_all, npair, att_key, WO_part):
                for i in range(NT):
                    for hf in range(2):
                        bk = 6 + (2 * i + hf) % 2
                        for hp_ in range(npair):
                            P.add("pe", (lambda e, hp_=hp_: e.matmul(
                                PS[bk][:, :], oT_all[:, hp_, 128 * i:128 * (i + 1)], WO_part[:, hp_, 512 * hf:512 * (hf + 1)],
                                start=(hp_ == 0), stop=(hp_ == npair - 1))),
                                reads=[(att_key, hp_, 0, i // 4), (att_key, hp_, 1, i // 4), "WO_part"], writes=[("ps", bk)])
                        P.add("dve", (lambda e: e.tensor_tensor(
                            X[:, i, 512 * hf:512 * (hf + 1)], X[:, i, 512 * hf:512 * (hf + 1)], PS[bk][:, :], ALU.add)),
                            reads=[("ps", bk), ("X", i, hf)], writes=[("X", i, hf)])

            def proj_q_pair(wi_ap, wi_key, qz_, zkey):
                for tb in range(4):
                    bk = 6 + tb % 2
                    for k in range(KC):
                        P.add("pe", (lambda e, k=k: e.matmul(PS[bk][:, :], wi_ap[:, k, :], xnT[:, k, 512 * tb:512 * (tb + 1)],
                                                             start=(k == 0), stop=(k == KC - 1))),
                              reads=[wi_key] + xk(tb), writes=[("ps", bk)])
                    P.add("act", (lambda e: e.copy(qz_[0][0:64, 512 * tb:512 * (tb + 1)], PS[bk][0:64, :])),
                          reads=[("ps", bk), zkey + "0pad"], writes=[(zkey, 0, tb)])
                    P.add("dve", (lambda e: e.tensor_copy(qz_[1][64:128, 512 * tb:512 * (tb + 1)], PS[bk][64:128, :])),
                          reads=[("ps", bk), zkey + "1pad"], writes=[(zkey, 1, tb)])

            ph.off = mix_base
            VB = 192
            v_flat = ph.take(NT * 3 * VB * 2, BF16)
            v4 = v_flat.rearrange("p (i h c) -> p i h c", i=NT, h=3)
            qz = [ph.take(S * 2, BF16) for _ in range(2)]
            kT = [ph.take(S * 2, BF16) for _ in range(2)]
            ET = ph.take(2 * ET_W * 2, BF16, "p (h v) -> p h v", h=2)
            oT_all = ph.take(3 * S * 2, BF16, "p (h t) -> p h t", h=3)
            WOD = ph.take(3 * D * 2, BF16, "p (c d) -> p c d", c=3)
            e_sb = [ph.take(512 * 2, BF16) for _ in range(3)]
            p_sb = [ph.take(512 * 2, BF16) for _ in range(3)]
            o_sb = [ph.take(512 * 4, F32) for _ in range(2)]
            r_row = ph.take(512 * 4, F32)
            P.add("dve", lambda e: e.memset(qz[0][64:128, :], 0.0), writes=["qz0pad"])
            P.add("dve", lambda e: e.memset(qz[1][0:64, :], 0.0), writes=["qz1pad"])
            P.add("dve", lambda e: e.memset(v4[:, :, :, 64:128], 0.0), writes=["v_ones"])
            P.add("dve", lambda e: e.memset(v4[:, :, :, 64:65], 1.0), reads=["v_ones"], writes=["v_ones"])
            P.add("dve", lambda e: e.memset(v4[:, :, :, 96:97], 1.0), reads=["v_ones"], writes=["v_ones"])
            P.add("pool", lambda e: e.dma_start(out=WOD, in_=w_out_d[l][384:768, :].rearrange("(c p) d -> p c d", p=128)),
                  writes=["WO_part"], dma_chan="wod")
            load_wi(0, [(1920, 384)])
            for i in range(NT):
                bk = i % 2
                for k in range(KC):
                    P.add("pe", (lambda e, k=k: e.matmul(PS[bk][:, 0:384], xnT[:, k, 128 * i:128 * (i + 1)], WI[0][:, k, 0:384],
                                                         start=(k == 0), stop=(k == KC - 1))),
                          reads=[("WI", 0, 0), ("WI", 0, 1), ("WI", 0, 2)] + xk(i // 4), writes=[("ps", bk)])
                psv = PS[bk][:, 0:384].rearrange("p (h two c) -> p h two c", h=3, two=2)
                en = "act" if i % 2 == 0 else "dve"
                P.add(en, copy_op(en, v4[:, i, :, 0:64], psv[:, :, 0, :]), reads=[("ps", bk)], writes=[("vA", i)])
                P.add(en, copy_op(en, v4[:, i, :, 128:192], psv[:, :, 1, :]), reads=[("ps", bk)], writes=[("vB", i)])

            for hp in range(3):
                buf = (hp + 1) % 2
                load_wi(buf, [(1152 + 128 * hp, 128), (1536 + 128 * hp, 128)])
                P.add("sp", (lambda e: e.dma_start(out=ET, in_=etab_d[2 * hp:2 * hp + 2].rearrange("h p v -> p h v"))),
                      writes=["ET"], dma_chan="et")
                kb_ = kT[hp % 2]
                proj_q_pair(WI[buf][:, :, 0:128], ("WI", buf, 0), qz, "qz")
                proj_feat(WI[buf][:, :, 128:256], [("WI", buf, 1)], (lambda tb, kb_=kb_: kb_[:, 512 * tb:512 * (tb + 1)]),
                          (lambda tb, hp=hp: ("kT", hp % 2, tb)), 6)
                gi = 0
                for hh in range(2):
                    h = 2 * hp + hh
                    for qb in range(4):
                        kts = []
                        for kt in range(max(0, 4 * qb - 8), min(15, 4 * qb + 11) + 1):
                            v0 = ET_OFF - (128 * kt - 512 * qb)
                            if etab_nz[h][:, v0:v0 + 512].any():
                                kts.append(kt)
                        par = gi % 2
                        gi += 1
                        bo = 3 + par

                        def s_mm(kt, n):
                            bs = n % 3
                            P.add("pe", (lambda e: e.matmul(PS[bs][:, :], kb_[:, 128 * kt:128 * (kt + 1)],
                                                            qz[hh][:, 512 * qb:512 * (qb + 1)], start=True, stop=True)),
                                  reads=[("kT", hp % 2, kt // 4), ("qz", hh, qb), "qz%dpad" % hh], writes=[("ps", bs)])

                        s_mm(kts[0], 0)
                        if len(kts) > 1:
                            s_mm(kts[1], 1)
                        for n, kt in enumerate(kts):
                            bs = n % 3
                            v0 = ET_OFF - (128 * kt - 512 * qb)
                            P.add("act", (lambda e: e.activation(e_sb[bs], PS[bs][:, :], AF.Exp, scale=0.125)),
                                  reads=[("ps", bs)], writes=[("e_sb", bs)])
                            P.add("dve", (lambda e: e.tensor_tensor(p_sb[bs], e_sb[bs], ET[:, hh, v0:v0 + 512], ALU.mult)),
                                  reads=[("e_sb", bs), "ET"], writes=[("p_sb", bs)])
                            if n + 2 < len(kts):
                                s_mm(kts[n + 2], n + 2)
                            voff = (kt * 3 + hp) * VB + 64 * hh
                            P.add("pe", (lambda e: e.matmul(PS[bo][:, :], v_flat[:, voff:voff + 128], p_sb[bs],
                                                            start=(n == 0), stop=(n == len(kts) - 1))),
                                  reads=[("p_sb", bs), ("vA", kt), ("vB", kt), "v_ones"], writes=[("ps", bo)])
                            if n == min(1, len(kts) - 1):
                                flush_pending()
                        pending.append(lambda par=par, hh=hh, qb=qb, hp=hp: attn_finish(
                            par, hh, qb, oT_all[:, hp, :], ("oT", hp), r_row, o_sb))
                        if not DEFER:
                            flush_pending()
            flush_pending()
            attn_outproj(oT_all, 3, "oT", WOD)
            P.barrier()

            ph.off = mix_base
            if skip_mem:
                continue
            WMKV = ph.take(KC * 512 * 2, BF16, "p (k c) -> p k c", k=KC)
            kmT = ph.take(2 * MEM * 2, BF16, "p (h t) -> p h t", h=2)
            vm_flat = ph.take(2 * 2 * VB * 2, BF16)
            vm4 = vm_flat.rearrange("p (i h c) -> p i h c", i=2, h=2)
            qmz = [ph.take(S * 2, BF16) for _ in range(2)]
            oTm_all = ph.take(2 * S * 2, BF16, "p (h t) -> p h t", h=2)
            WOM = ph.take(2 * D * 2, BF16, "p (c d) -> p c d", c=2)
            p_sb = [ph.take(512 * 2, BF16) for _ in range(4)]
            o_sb = [ph.take(512 * 4, F32) for _ in range(2)]
            r_row = ph.take(512 * 4, F32)
            P.add("dve", lambda e: e.memset(qmz[0][64:128, :], 0.0), writes=["qmz0pad"])
            P.add("dve", lambda e: e.memset(qmz[1][0:64, :], 0.0), writes=["qmz1pad"])
            P.add("dve", lambda e: e.memset(vm4[:, :, :, 64:128], 0.0), writes=["vm_ones"])
            P.add("dve", lambda e: e.memset(vm4[:, :, :, 64:65], 1.0), reads=["vm_ones"], writes=["vm_ones"])
            P.add("dve", lambda e: e.memset(vm4[:, :, :, 96:97], 1.0), reads=["vm_ones"], writes=["vm_ones"])
            P.add("pool", lambda e: e.dma_start(out=WMKV, in_=w_mkv_d[l].rearrange("(k p) c -> p k c", p=128)), writes=["WMKV"], dma_chan="wmkv")
            P.add("pool", lambda e: e.dma_start(out=WOM, in_=w_out_d[l][768:1024, :].rearrange("(c p) d -> p c d", p=128)),
                  writes=["WO_part"], dma_chan="wod")
            for mp in range(2):
                for k in range(KC):
                    P.add("pe", (lambda e, k=k: e.matmul(PS[0][:, 0:MEM], WMKV[:, k, 128 * mp:128 * (mp + 1)], memT[:, k, :],
                                                         start=(k == 0), stop=(k == KC - 1))),
                          reads=["WMKV", ("memT", 0), ("memT", 1)], writes=[("ps", 0)])
                P.add("act", (lambda e: e.copy(kmT[:, mp, :], PS[0][:, 0:MEM])), reads=[("ps", 0)], writes=[("kmT", mp)])
            for i in range(2):
                for k in range(KC):
                    P.add("pe", (lambda e, k=k: e.matmul(PS[1][:, 0:256], memT[:, k, 128 * i:128 * (i + 1)], WMKV[:, k, 256:512],
                                                         start=(k == 0), stop=(k == KC - 1))),
                          reads=["WMKV", ("memT", 0), ("memT", 1)], writes=[("ps", 1)])
                psv = PS[1][:, 0:256].rearrange("p (h two c) -> p h two c", h=2, two=2)
                P.add("act", (lambda e: e.copy(vm4[:, i, :, 0:64], psv[:, :, 0, :])), reads=[("ps", 1)], writes=[("vmA", i)])
                P.add("act", (lambda e: e.copy(vm4[:, i, :, 128:192], psv[:, :, 1, :])), reads=[("ps", 1)], writes=[("vmB", i)])
            vmkeys = [("vmA", 0), ("vmA", 1), ("vmB", 0), ("vmB", 1), "vm_ones"]
            gi = 0
            for mp in range(2):
                buf = mp % 2
                load_wi(buf, [(2304 + 128 * mp, 128)])
                proj_q_pair(WI[buf][:, :, 0:128], ("WI", buf, 0), qmz, "qmz")
                for hh in range(2):
                    for qb in range(4):
                        par = gi % 2
                        sb0 = 2 * (gi % 2)
                        gi += 1
                        bo = 3 + par
                        for kt in range(2):
                            bs = sb0 + kt
                            P.add("pe", (lambda e: e.matmul(PS[bs if bs < 3 else 7][:, :], kmT[:, mp, 128 * kt:128 * (kt + 1)],
                                                            qmz[hh][:, 512 * qb:512 * (qb + 1)], start=True, stop=True)),
                                  reads=[("kmT", mp), ("qmz", hh, qb), "qmz%dpad" % hh], writes=[("ps", bs if bs < 3 else 7)])
                        for kt in range(2):
                            bs = sb0 + kt
                            pbk = bs if bs < 3 else 7
                            P.add("act", (lambda e: e.activation(p_sb[bs], PS[pbk][:, :], AF.Exp, scale=0.125)),
                                  reads=[("ps", pbk)], writes=[("p_sb", bs)])
                            if kt == 0:
                                flush_pending()
                            voff = (kt * 2 + mp) * VB + 64 * hh
                            P.add("pe", (lambda e: e.matmul(PS[bo][:, :], vm_flat[:, voff:voff + 128], p_sb[bs], start=(kt == 0), stop=(kt == 1))),
                                  reads=[("p_sb", bs)] + vmkeys, writes=[("ps", bo)])
                        pending.append(lambda par=par, hh=hh, qb=qb, mp=mp: attn_finish(
                            par, hh, qb, oTm_all[:, mp, :], ("oTm", mp), r_row, o_sb))
            flush_pending()
            attn_outproj(oTm_all, 2, "oTm", WOM)
            P.barrier()

            if not do_moe:
                continue
            ph = Arena(ph_t, PH_BYTES)
            WG = [ph.take(KC * 512 * 2, BF16, "p (k f) -> p k f", k=KC) for _ in range(2)]
            WU = [ph.take(KC * 512 * 2, BF16, "p (k f) -> p k f", k=KC) for _ in range(2)]
            WD = [ph.take(4 * D * 2, BF16, "p (c d) -> p c d", c=4) for _ in range(2)]
            WR = ph.take(KC * E * 4, F32, "p (k e) -> p k e", k=KC)
            moe_base = ph.off

            def load_expert_gu(e_, fs):
                buf = (e_ * 4 + fs) % 2
                P.add("pool", (lambda e: e.dma_start(
                    out=WG[buf], in_=w_gate_d[l, e_].rearrange("(k p) f -> p k f", p=128)[:, :, 512 * fs:512 * (fs + 1)])),
                    writes=[("WG", buf)], dma_chan=("wg", buf))
                P.add("pool", (lambda e: e.dma_start(
                    out=WU[buf], in_=w_up_d[l, e_].rearrange("(k p) f -> p k f", p=128)[:, :, 512 * fs:512 * (fs + 1)])),
                    writes=[("WU", buf)], dma_chan=("wu", buf))

            def load_expert_d(e_, fs):
                buf = (e_ * 4 + fs) % 2
                P.add("pool", (lambda e: e.dma_start(
                    out=WD[buf], in_=w_down_d[l, e_][512 * fs:512 * (fs + 1), :].rearrange("(c p) d -> p c d", p=128))),
                    writes=[("WD", buf)], dma_chan=("wd", buf))

            def load_expert_w(e_, fs):
                load_expert_gu(e_, fs)
                load_expert_d(e_, fs)

            load_gain(norm_ffn_d[l])
            P.add("act", lambda e: e.dma_start(out=WR, in_=w_router_d[l].rearrange("(k p) e -> p k e", p=128)), writes=["WR"], dma_chan="wr")
            load_expert_w(0, 0)
            load_expert_w(0, 1)

            affT = ph.take(S * 4, F32)
            cjunk = ph.take(S * 2, BF16)
            maskT = ph.take(S * 4, F32)
            r_base = ph.off
            xnf = [ph.take(D * 4, F32) for _ in range(2)]
            xnTf = [ph.take(KC * 128 * 4, F32, "p (k t) -> p k t", k=KC) for _ in range(2)]
            ph.off = r_base
            cums = ph.take(S * 4, F32)
            selp = ph.take(S * 4, F32)
            lg_sb = [ph.take(128 * 4, F32) for _ in range(2)]
            tq = ph.take(8 * 4, F32)
            rms_stats(lambda i: X[:, i, :], lambda i: [("X", i, 0), ("X", i, 1)], NT)

            def softmax_tile(i):
                lb = 3 + i % 2
                P.add("dve", lambda e: e.reduce_max(sm[:, 0:1], PS[lb][:, 0:E], axis=AX.X), reads=[("ps", lb)], writes=["sm0"])
                P.add("dve", lambda e: e.tensor_scalar(sm[:, 1:2], sm[:, 0:1], -1.0, None, ALU.mult), reads=["sm0"], writes=["sm1"])
                P.add("act", (lambda e: e.activation(aff_all[:, i, :], PS[lb][:, 0:E], AF.Exp, bias=sm[:, 1:2], scale=1.0, accum_out=sm[:, 2:3])),
                      reads=[("ps", lb), "sm1"], writes=[("aff", i), "sm2"])
                P.add("dve", lambda e: e.reciprocal(sm[:, 3:4], sm[:, 2:3]), reads=["sm2"], writes=["sm3"])
                P.add("dve", (lambda e: e.tensor_scalar(aff_all[:, i, :], aff_all[:, i, :], sm[:, 3:4], None, ALU.mult)),
                      reads=[("aff", i), "sm3"], writes=[("aff", i)])

            def stage_a(i):
                xf = xnf[i % 2]
                P.add("dve", (lambda e: e.scalar_tensor_tensor(xf, X[:, i, :], rstd[:, i:i + 1], G, ALU.mult, ALU.mult)),
                      reads=[("X", i, 0), ("X", i, 1), "rstd", "G"], writes=[("xnf", i % 2)])
                P.add("act", (lambda e: e.copy(xnTok[:, i, :], xf)), reads=[("xnf", i % 2)], writes=[("xnTok", i)])

            def stage_b(i):
                xf = xnf[i % 2]
                xt = xnTf[i % 2]
                lb = 3 + i % 2
                for c in range(KC):
                    bk = c // 4
                    P.add("pe", (lambda e, c=c, bk=bk: e.transpose(PS[bk][:, 128 * (c % 4):128 * (c % 4 + 1)], xf[:, 128 * c:128 * (c + 1)], ident_f)),
                          reads=[("xnf", i % 2), "ident_f"], writes=[("ps", bk)])
                P.add("act", lambda e: e.copy(xt[:, 0:4, :], PS[0][:, :].rearrange("p (k t) -> p k t", k=4)), reads=[("ps", 0)], writes=[("xnTf", i % 2, 0)])
                P.add("dve", lambda e: e.tensor_copy(xt[:, 4:8, :], PS[1][:, :].rearrange("p (k t) -> p k t", k=4)), reads=[("ps", 1)], writes=[("xnTf", i % 2, 1)])
                for k in range(KC):
                    P.add("pe", (lambda e, k=k: e.matmul(PS[2][0:E, 0:128], WR[:, k, :], xt[:, k, :], start=(k == 0), stop=(k == KC - 1))),
                          reads=[("xnTf", i % 2, k // 4), "WR"], writes=[("ps", 2)])
                P.add("act", (lambda e: e.copy(lg_sb[i % 2][0:E, :], PS[2][0:E, 0:128])), reads=[("ps", 2)], writes=[("lg", i % 2)])
                P.add("pe", (lambda e: e.transpose(PS[lb][:, 0:E], lg_sb[i % 2][0:E, :], ident_f[0:E, 0:E])),
                      reads=[("lg", i % 2), "ident_f"], writes=[("ps", lb)])

            stage_a(0)
            for i in range(NT):
                if i + 1 < NT:
                    stage_a(i + 1)
                if i > 0:
                    softmax_tile(i - 1)
                stage_b(i)
            softmax_tile(NT - 1)
            for i in range(NT):
                bk = 5 + (i // 4) % 2
                P.add("pe", (lambda e, i=i, bk=bk: e.transpose(PS[bk][0:E, 128 * (i % 4):128 * (i % 4 + 1)], aff_all[:, i, :], ident_f)),
                      reads=[("aff", i), "ident_f"], writes=[("ps", bk)])
                if i % 4 == 3:
                    j = i // 4
                    P.add("act", (lambda e, j=j, bk=bk: e.copy(affT[0:E, 512 * j:512 * (j + 1)], PS[bk][0:E, :])), reads=[("ps", bk)], writes=[("affT", j)])
            P.barrier()
            affT_keys = [("affT", j) for j in range(4)]
            P.add("dve", lambda e: e.memset(tq[0:E, 0:1], 0.0), writes=["tq_t"])
            for kbit in range(1, 31):
                step = 2.0 ** (-kbit)
                P.add("dve", lambda e: e.tensor_scalar(tq[0:E, 1:2], tq[0:E, 0:1], step, None, ALU.add), reads=["tq_t"], writes=["tq_c"])
                P.add("dve", lambda e: e.tensor_scalar(cjunk[0:E, :], affT[0:E, :], tq[0:E, 1:2], None, ALU.is_ge, ALU.add, accum_out=tq[0:E, 2:3]),
                      reads=affT_keys + ["tq_c"], writes=["cjunk", "tq_n"])
                P.add("dve", lambda e: e.tensor_scalar(tq[0:E, 3:4], tq[0:E, 2:3], CAP - 0.5, step, ALU.is_ge, ALU.mult), reads=["tq_n"], writes=["tq_g"])
                P.add("dve", lambda e: e.tensor_tensor(tq[0:E, 0:1], tq[0:E, 0:1], tq[0:E, 3:4], ALU.add), reads=["tq_t", "tq_g"], writes=["tq_t"])
            P.add("dve", lambda e: e.tensor_scalar(maskT[0:E, :], affT[0:E, :], tq[0:E, 0:1], None, ALU.is_ge), reads=affT_keys + ["tq_t"], writes=["maskT"])
            P.add("dve", lambda e: e.tensor_tensor_scan(cums[0:E, :], maskT[0:E, :], maskT[0:E, :], 0.0, ALU.add, ALU.max),
                  reads=["maskT"], writes=["cums"])
            P.add("dve", lambda e: e.tensor_tensor(selp[0:E, :], cums[0:E, :], maskT[0:E, :], ALU.mult), reads=["cums", "maskT"], writes=["selp"])
            for i in range(NT):
                P.add("pe", (lambda e, i=i: e.transpose(PS[7][:, E * i:E * (i + 1)], selp[0:E, 128 * i:128 * (i + 1)], ident_f[0:E, 0:E])),
                      reads=["selp", "ident_f"], writes=[("ps", 7)])
            P.add("act", lambda e: e.copy(sel_all, PS[7][:, 0:NT * E].rearrange("p (i e) -> p i e", i=NT)), reads=[("ps", 7)], writes=["sel"])
            P.barrier()

            ph.off = moe_base
            Ssel = ph.take(NT * CAP * 2, BF16, "p (i j) -> p i j", i=NT)
            Sg = ph.take(8 * CAP * 2, BF16, "p (i j) -> p i j", i=8)
            SgT = [ph.take(2 * S * 2, BF16, "p (j t) -> p j t", j=2) for _ in range(2)]
            xgT = ph.take(KC * CAP * 2, BF16, "p (k j) -> p k j", k=KC)
            hT = [ph.take(4 * CAP * 2, BF16, "p (c j) -> p c j", c=4) for _ in range(2)]
            sg_sb = [ph.take(CAP * 4, F32) for _ in range(2)]
            y_sb = ph.take(2 * D * 2, BF16, "p (j d) -> p j d", j=2)
            xg_keys = [("xgT", c) for c in range(KC)]

            def scatter_unit(es, u):
                i, hf = u // 2, u % 2
                bk = 6 + u % 2
                for jc in range(2):
                    P.add("pe", (lambda e, jc=jc: e.matmul(
                        PS[bk][:, :], SgT[es % 2][:, jc, 128 * i:128 * (i + 1)], y_sb[:, jc, 512 * hf:512 * (hf + 1)], start=(jc == 0), stop=(jc == 1))),
                        reads=[("SgT", es % 2, jc, i // 8, (i % 8) // 4), ("y", jc, hf)], writes=[("ps", bk)])
                P.add("dve", (lambda e: e.tensor_tensor(
                    X[:, i, 512 * hf:512 * (hf + 1)], X[:, i, 512 * hf:512 * (hf + 1)], PS[bk][:, :], ALU.add)),
                    reads=[("ps", bk), ("X", i, hf)], writes=[("X", i, hf)])

            def build_A(e_):
                th = []
                for i in range(NT):
                    th.append(lambda i=i: P.add("dve", (lambda e: e.tensor_scalar(Ssel[:, i, :], iota1, sel_all[:, i, e_:e_ + 1], None, ALU.is_equal)),
                                                reads=["iota1", "sel"], writes=[("Ssel", i)]))
                for ii in range(8):
                    th.append(lambda ii=ii: P.add("dve", (lambda e: e.tensor_scalar(Sg[:, ii, :], iota1, sel_all[:, ii, e_:e_ + 1], aff_all[:, ii, e_:e_ + 1],
                                                                                    ALU.is_equal, ALU.mult)),
                                                  reads=["iota1", "sel", ("aff", ii)], writes=[("Sg", ii)]))
                return th

            def build_B(e_):
                for ii in range(8):
                    i = 8 + ii
                    P.add("dve", (lambda e: e.tensor_scalar(Sg[:, ii, :], iota1, sel_all[:, i, e_:e_ + 1], aff_all[:, i, e_:e_ + 1],
                                                            ALU.is_equal, ALU.mult)),
                          reads=["iota1", "sel", ("aff", i)], writes=[("Sg", ii)])

            def sgt_half(e_, half):
                for jc in range(2):
                    for q in range(2):
                        bk = 6 + q
                        for i4 in range(4):
                            ii = 4 * q + i4
                            P.add("pe", (lambda e: e.matmul(PS[bk][:, 128 * i4:128 * (i4 + 1)], Sg[:, ii, 128 * jc:128 * (jc + 1)], ident_b,
                                                            start=True, stop=True)),
                                  reads=[("Sg", ii), "ident_b"], writes=[("ps", bk)])
                        en = alt()
                        c0 = 1024 * half + 512 * q
                        P.add(en, copy_op(en, SgT[e_ % 2][:, jc, c0:c0 + 512], PS[bk][:, :]), reads=[("ps", bk)],
                              writes=[("SgT", e_ % 2, jc, half, q)])

            for t_ in build_A(0):
                t_()
            for e_ in range(E):
                sgt_half(e_, 0)
                build_B(e_)
                for c in range(KC):
                    bk = 6 + c % 2
                    for i in range(NT):
                        P.add("pe", (lambda e, c=c, i=i, bk=bk: e.matmul(PS[bk][:, 0:CAP], xnTok[:, i, 128 * c:128 * (c + 1)], Ssel[:, i, :],
                                                                         start=(i == 0), stop=(i == NT - 1))),
                              reads=[("xnTok", i), ("Ssel", i)], writes=[("ps", bk)])
                    en = alt()
                    P.add(en, copy_op(en, xgT[:, c, :], PS[bk][:, 0:CAP]), reads=[("ps", bk)], writes=[("xgT", c)])
                sgt_half(e_, 1)
                nextA = build_A(e_ + 1) if e_ + 1 < E else []

                def gate_up(f):
                    fs, fc = f // 4, f % 4
                    buf = (e_ * 4 + fs) % 2
                    bk = 4 + f % 2
                    for k in range(KC):
                        P.add("pe", (lambda e, k=k: e.matmul(PS[bk][:, 0:CAP], WG[buf][:, k, 128 * fc:128 * (fc + 1)], xgT[:, k, :],
                                                             start=(k == 0), stop=(k == KC - 1))),
                              reads=[("WG", buf)] + xg_keys, writes=[("ps", bk)])
                    for k in range(KC):
                        P.add("pe", (lambda e, k=k: e.matmul(PS[bk][:, CAP:2 * CAP], WU[buf][:, k, 128 * fc:128 * (fc + 1)], xgT[:, k, :],
                                                             start=(k == 0), stop=(k == KC - 1))),
                              reads=[("WU", buf)] + xg_keys, writes=[("ps", bk)])

                gate_up(0)
                for f in range(16):
                    fs, fc = f // 4, f % 4
                    buf = (e_ * 4 + fs) % 2
                    bk = 4 + f % 2
                    hb = hT[fs % 2]
                    sgb = sg_sb[f % 2]
                    P.add("act", (lambda e: e.activation(sgb, PS[bk][:, 0:CAP], AF.Silu)), reads=[("ps", bk)], writes=[("sg", f % 2)])
                    P.add("dve", (lambda e: e.tensor_tensor(hb[:, fc, :], sgb, PS[bk][:, CAP:2 * CAP], ALU.mult)),
                          reads=[("sg", f % 2), ("ps", bk)], writes=[("hT", fs % 2, fc)])
                    if f + 1 < 16:
                        gate_up(f + 1)
                        if (f + 1) % 4 == 3:
                            nxt = e_ * 4 + (f + 1) // 4 + 2
                            if nxt < E * 4:
                                load_expert_gu(nxt // 4, nxt % 4)
                    for jc in range(2):
                        for hf in range(2):
                            by = 2 * jc + hf
                            P.add("pe", (lambda e, jc=jc, hf=hf, by=by: e.matmul(
                                PS[by][:, :], hb[:, fc, 128 * jc:128 * (jc + 1)], WD[buf][:, fc, 512 * hf:512 * (hf + 1)],
                                start=(f == 0), stop=(f == 15))),
                                reads=[("hT", fs % 2, fc), ("WD", buf)], writes=[("ps", by)])
                    if e_ > 0:
                        scatter_unit(e_ - 1, 2 * f)
                        scatter_unit(e_ - 1, 2 * f + 1)
                    for _ in range(2):
                        if nextA:
                            nextA.pop(0)()
                    if fc == 3:
                        nxt = e_ * 4 + fs + 2
                        if nxt < E * 4:
                            load_expert_d(nxt // 4, nxt % 4)
                for jc in range(2):
                    for hf in range(2):
                        by = 2 * jc + hf
                        en = alt()
                        P.add(en, copy_op(en, y_sb[:, jc, 512 * hf:512 * (hf + 1)], PS[by][:, :]), reads=[("ps", by)], writes=[("y", jc, hf)])
            for u in range(32):
                scatter_unit(E - 1, u)
            P.barrier()

        ph = Arena(ph_t, PH_BYTES)
        ob = [ph.take(D * 4, F32) for _ in range(4)]
        if debug:
            for i in range(NT):
                final_ops.append(P.add("sp", (lambda e, i=i: e.dma_start(out=out_d[128 * i:128 * (i + 1), :], in_=X[:, i, :])),
                                       reads=[("X", i, 0), ("X", i, 1)], dma_chan="dbg", extra_deps=final_ops[-1:]))
        else:
            load_gain(norm_final_d[0])
            rms_stats(lambda i: X[:, i, :], lambda i: [("X", i, 0), ("X", i, 1)], NT)
            for i in range(NT):
                P.add("dve", (lambda e, i=i: e.scalar_tensor_tensor(ob[i % 4], X[:, i, :], rstd[:, i:i + 1], G, ALU.mult, ALU.mult)),
                      reads=[("X", i, 0), ("X", i, 1), "rstd", "G"], writes=[("ob", i % 4)])
                final_ops.append(P.add("sp", (lambda e, i=i: e.dma_start(out=out_d[128 * i:128 * (i + 1), :], in_=ob[i % 4])),
                                       reads=[("ob", i % 4)], writes=[("obd", i % 4)], dma_chan=("o", i % 4)))
        P.emit(nc, final_waits=[("sp", o) for o in final_ops])
    return nc


def _etab():
    t = np.zeros((6, 128, ET_W), np.float64)
    p = np.arange(128)[:, None]
    v = np.arange(ET_W)[None, :]
    d = p - v + ET_OFF
    ad = np.abs(d)
    m = (ad <= 64).astype(np.float64) + ((d % 4 == 0) & (ad <= 256)) + ((d % 16 == 0) & (ad <= 1024))
    for h in range(6):
        slope = 2.0 ** (-8.0 * (h + 1) / 6)
        t[h] = m * np.exp(-slope * ad)
    return t.astype(np.float32).astype(ml_dtypes.bfloat16)


def _consts():
    return {
        "ident_bf": np.eye(128, dtype=np.float32).astype(ml_dtypes.bfloat16),
        "ident_f": np.eye(128, dtype=np.float32),
        "iota1": np.tile(np.arange(1, CAP + 1, dtype=np.float32)[None, :], (128, 1)),
        "etab": _etab(),
    }


def make_in_maps(x, mem, mem_norm, norm_mix, w_in, conv_w, w_mem_kv, w_out, norm_ffn,
                 w_router, w_gate, w_up, w_down, norm_final):
    f = lambda a: np.ascontiguousarray(np.asarray(a, dtype=np.float32))
    conv_w = f(conv_w)
    convw_t = np.ascontiguousarray(conv_w.reshape(L, 3, 3, 128).transpose(0, 3, 2, 1).reshape(L, 128, 9))
    shared = {
        "mem_norm": f(mem_norm).reshape(1, D), "norm_mix": f(norm_mix), "norm_ffn": f(norm_ffn),
        "norm_final": f(norm_final).reshape(1, D), "w_in": f(w_in), "convw_t": convw_t,
        "w_mem_kv": f(w_mem_kv), "w_out": f(w_out), "w_router": f(w_router),
        "w_gate": f(w_gate), "w_up": f(w_up), "w_down": f(w_down),
    }
    shared.update(_consts())
    x = f(x)
    mem = f(mem)
    return [dict(shared, x=x[b], mem=mem[b]) for b in range(N_CORES)]


def kernel(x, mem, mem_norm, norm_mix, w_in, conv_w, w_mem_kv, w_out, norm_ffn,
           w_router, w_gate, w_up, w_down, norm_final):
    in_maps = make_in_maps(x, mem, mem_norm, norm_mix, w_in, conv_w, w_mem_kv, w_out, norm_ffn,
                           w_router, w_gate, w_up, w_down, norm_final)
    nc = build_program()
    res = run_bass_kernel_spmd(nc, in_maps, core_ids=list(range(N_CORES)))
    return np.stack([np.asarray(r["out"], dtype=np.float32) for r in res.results], axis=0)
```

```python
import contextlib
import numpy as np
import ml_dtypes
import concourse.bass as bass
import concourse.mybir as mybir
from concourse.bass_utils import run_bass_kernel_spmd

F32 = mybir.dt.float32
BF16 = mybir.dt.bfloat16
AF = mybir.ActivationFunctionType
ALU = mybir.AluOpType
AX = mybir.AxisListType

D = 1024
S = 2048
NT = 16
KC = 8
L = 2
E = 16
FF = 2048
CAP = 256
MEM = 256
EPS = 1e-6
ET_W = 2944
ET_OFF = 1408
N_CORES = 8


class _Rec:
    def __init__(self):
        self.call = None

    def __getattr__(self, name):
        def f(*a, **k):
            self.call = (name, a, k)
            return self
        return f


class Prog:
    def __init__(self):
        self.ops = []
        self.last_writer = {}
        self.readers = {}
        self.chan_count = {}
        self.last_eng = {}
        self.last_chan = {}

    def add(self, eng, fn, reads=(), writes=(), dma_chan=None, extra_deps=()):
        idx = len(self.ops)
        deps = set(extra_deps)
        for k in reads:
            w = self.last_writer.get(k)
            if w is not None:
                deps.add(w)
        for k in writes:
            w = self.last_writer.get(k)
            if w is not None:
                deps.add(w)
            for r in self.readers.get(k, {}).values():
                deps.add(r)
        deps.discard(idx)
        rkey = eng if dma_chan is None else ("dma", idx)
        for k in reads:
            self.readers.setdefault(k, {})[rkey] = idx
        for k in writes:
            self.last_writer[k] = idx
            self.readers[k] = {}
        if fn is not None:
            rec = _Rec()
            fn(rec)
            call = rec.call
            assert call is not None
            fn = (lambda e, call=call: getattr(e, call[0])(*call[1], **call[2]))
        op = dict(eng=eng, fn=fn, deps=deps, chan=dma_chan, signal=False)
        if dma_chan is not None:
            self.chan_count[dma_chan] = self.chan_count.get(dma_chan, 0) + 16
            op["dmaval"] = self.chan_count[dma_chan]
            self.last_chan[dma_chan] = idx
        elif fn is not None:
            self.last_eng[eng] = idx
        self.ops.append(op)
        return idx

    def barrier(self):
        deps = set(self.last_eng.values()) | set(self.last_chan.values())
        for e in ["pe", "act", "dve", "pool", "sp"]:
            self.add(e, None, extra_deps=deps)

    def emit(self, nc, final_waits=()):
        ops = self.ops
        for op in ops:
            for d in op["deps"]:
                p = ops[d]
                if p["chan"] is None:
                    if p["eng"] == "pe" and op["eng"] == "pe" and op["chan"] is None and op["fn"] is not None:
                        continue
                    p["signal"] = True
        for (_, fo) in final_waits:
            if ops[fo]["chan"] is None:
                ops[fo]["signal"] = True
        engs = ["pe", "act", "dve", "pool", "sp"]
        seq = {e: 0 for e in engs}
        for op in ops:
            if op["chan"] is None and op["signal"]:
                seq[op["eng"]] += 1
                op["seqval"] = seq[op["eng"]]
        chans = sorted(self.chan_count.keys(), key=str)
        with contextlib.ExitStack() as st:
            esem = {e: st.enter_context(nc.semaphore("s_" + e)) for e in engs}
            csem = {c: st.enter_context(nc.semaphore("c_%d" % i)) for i, c in enumerate(chans)}
            block = st.enter_context(nc.Block())

            def run_engine(ename):
                def body(eng):
                    waited = {}
                    for op in ops:
                        if op["eng"] != ename:
                            continue
                        for d in sorted(op["deps"]):
                            p = ops[d]
                            if p["chan"] is not None:
                                key = ("c", p["chan"]); val = p["dmaval"]; sem = csem[p["chan"]]
                            else:
                                if p["eng"] == "pe" and ename == "pe" and op["chan"] is None and op["fn"] is not None:
                                    continue
                                key = ("e", p["eng"]); val = p["seqval"]; sem = esem[p["eng"]]
                            if waited.get(key, 0) >= val:
                                continue
                            eng.wait_ge(sem, val)
                            waited[key] = val
                        if op["fn"] is None:
                            continue
                        ins = op["fn"](eng)
                        if op["chan"] is not None:
                            ins.then_inc(csem[op["chan"]], 16)
                        elif op["signal"]:
                            ins.then_inc(esem[ename], 1)
                    for (e2, fo) in final_waits:
                        if e2 != ename:
                            continue
                        p = ops[fo]
                        if p["chan"] is not None:
                            eng.wait_ge(csem[p["chan"]], p["dmaval"])
                        else:
                            eng.wait_ge(esem[p["eng"]], p["seqval"])
                return body

            block.tensor(run_engine("pe"))
            block.scalar(run_engine("act"))
            block.vector(run_engine("dve"))
            block.gpsimd(run_engine("pool"))
            block.sync(run_engine("sp"))


class Arena:
    def __init__(self, t, nbytes):
        self.t = t
        self.n = nbytes
        self.off = 0
        self.mark = 0

    def take(self, nbytes, dt=F32, pattern=None, **kw):
        off = self.off
        self.off += (nbytes + 63) // 64 * 64
        assert self.off <= self.n, ("arena overflow", self.off, self.n)
        ap = self.t[:, off // 4:(off + nbytes) // 4]
        if dt != F32:
            ap = ap.bitcast(dt)
        if pattern:
            ap = ap.rearrange(pattern, **kw)
        return ap


def build_program(n_layers=L, do_moe=True, debug=False, skip_mem=False, skip_dil=False):
    nc = bass.Bass("TRN2", target_bir_lowering=False)

    def din(name, shape, dt=F32):
        return nc.dram_tensor(name, list(shape), dt, kind="ExternalInput").ap()

    x_d = din("x", [S, D])
    mem_d = din("mem", [MEM, D])
    mem_norm_d = din("mem_norm", [1, D])
    norm_mix_d = din("norm_mix", [L, D])
    norm_ffn_d = din("norm_ffn", [L, D])
    norm_final_d = din("norm_final", [1, D])
    w_in_d = din("w_in", [L, D, 2560])
    convw_d = din("convw_t", [L, 128, 9])
    w_mkv_d = din("w_mem_kv", [L, D, 512])
    w_out_d = din("w_out", [L, D, D])
    if do_moe:
        w_router_d = din("w_router", [L, D, E])
        w_gate_d = din("w_gate", [L, E, D, FF])
        w_up_d = din("w_up", [L, E, D, FF])
        w_down_d = din("w_down", [L, E, FF, D])
    identb_d = din("ident_bf", [128, 128], BF16)
    identf_d = din("ident_f", [128, 128])
    iota1_d = din("iota1", [128, CAP])
    etab_d = din("etab", [6, 128, ET_W], BF16)
    out_d = nc.dram_tensor("out", [S, D], F32, kind="ExternalOutput").ap()
    dbg = {}
    if debug:
        def dout(name, shape, dt=F32):
            dbg[name] = nc.dram_tensor(name, list(shape), dt, kind="ExternalOutput").ap()
        dout("d_xnT", [128, KC * S], BF16)
        dout("d_x_conv", [S, D])
        dout("d_convT", [128, 3 * S], BF16)
        dout("d_qT0", [128, S], BF16)
        dout("d_kT0", [128, S], BF16)
        dout("d_v", [128, NT * 6 * 65], BF16)
        dout("d_oT0", [64, 2 * S], BF16)
        dout("d_x_dil", [S, D])
        dout("d_osb", [65, 512])
        dout("d_psb", [128, 512], BF16)
        dout("d_ET", [128, 2 * ET_W], BF16)
        dout("d_esb", [128, 512], BF16)
        dout("d_S", [128, 512])
        dout("d_psb0", [128, 512], BF16)

    etab_nz = _etab().astype(np.float32) != 0
    PERS_BYTES = 114 * 1024 + 768
    PH_BYTES = 92 * 1024 - 768

    with contextlib.ExitStack() as st:
        pers_t = st.enter_context(nc.sbuf_tensor("pers", [128, PERS_BYTES // 4], F32))
        ph_t = st.enter_context(nc.sbuf_tensor("phase", [128, PH_BYTES // 4], F32))
        PS = [st.enter_context(nc.psum_tensor("ps%d" % i, [128, 512], F32)) for i in range(8)]
        PSB = [p[:].bitcast(BF16) for p in PS]

        pa = Arena(pers_t, PERS_BYTES)
        X = pa.take(NT * D * 4, F32, "p (i d) -> p i d", i=NT)
        A = pa.take(NT * D * 2, BF16)
        xnT = A.rearrange("p (k t) -> p k t", k=KC)
        xnTok = A.rearrange("p (i d) -> p i d", i=NT)
        ident_b = pa.take(128 * 2, BF16)
        ident_f = pa.take(128 * 4, F32)
        iota1 = pa.take(CAP * 4, F32)
        ones_f = pa.take(128 * 4, F32)
        memT = pa.take(KC * MEM * 2, BF16, "p (k t) -> p k t", k=KC)
        G = pa.take(D * 4, F32)
        junk = pa.take(D * 2, BF16)
        XN = [pa.take(D * 2, BF16), pa.take(D * 2, BF16)]
        ss = pa.take(NT * 4, F32)
        std = pa.take(NT * 4, F32)
        rstd = pa.take(NT * 4, F32)
        cw = pa.take(9 * 4, F32)
        aff_all = pa.take(NT * E * 4, F32, "p (i e) -> p i e", i=NT)
        sel_all = pa.take(NT * E * 4, F32, "p (i e) -> p i e", i=NT)
        sm = pa.take(16 * 4, F32)

        P = Prog()
        cnt = [0]

        def xk(tb):
            return [("xnT", tb, 0), ("xnT", tb, 1)]

        def alt():
            cnt[0] += 1
            return "act" if cnt[0] % 2 == 0 else "dve"

        def copy_op(engname, out, in_):
            if engname == "act":
                return lambda e: e.copy(out, in_)
            return lambda e: e.tensor_copy(out, in_)

        def dump(name, src, reads, idx=None):
            if not debug or l != 0:
                return
            dst = dbg[name] if idx is None else dbg[name][idx]
            final_ops.append(P.add("sp", (lambda e: e.dma_start(out=dst, in_=src)), reads=reads, dma_chan="dbg", extra_deps=final_ops[-1:]))

        def dump_x(name):
            if not debug or l != 0:
                return
            for i in range(NT):
                final_ops.append(P.add("sp", (lambda e, i=i: e.dma_start(out=dbg[name][128 * i:128 * (i + 1), :], in_=X[:, i, :])),
                                       reads=[("X", i, 0), ("X", i, 1)], dma_chan="dbg", extra_deps=final_ops[-1:]))

        final_ops = []
        l = 0
        P.add("act", lambda e: e.dma_start(out=ident_b, in_=identb_d), writes=["ident_b"], dma_chan="c0")
        P.add("act", lambda e: e.dma_start(out=ident_f, in_=identf_d), writes=["ident_f"], dma_chan="c1")
        P.add("act", lambda e: e.dma_start(out=iota1, in_=iota1_d), writes=["iota1"], dma_chan="c2")
        P.add("dve", lambda e: e.memset(ones_f, 1.0), writes=["ones_f"])

        def load_gain(src_row):
            P.add("act", lambda e: e.dma_start(out=G, in_=src_row.partition_broadcast(128)), writes=["G"], dma_chan="g")

        def rms_stats(src_fn, keys_fn, n):
            for i in range(n):
                P.add("act", (lambda e, i=i: e.activation(junk, src_fn(i), AF.Square, accum_out=ss[:, i:i + 1])),
                      reads=keys_fn(i), writes=["junk", ("ss", i)])
            P.add("act", lambda e: e.activation(std[:, 0:n], ss[:, 0:n], AF.Sqrt, bias=eps_ap, scale=1.0 / D),
                  reads=[("ss", i) for i in range(n)] + ["eps"], writes=["std"])
            P.add("dve", lambda e: e.reciprocal(rstd[:, 0:n], std[:, 0:n]), reads=["std"], writes=["rstd"])

        eps_ap = pa.take(4, F32)
        P.add("dve", lambda e: e.memset(eps_ap, EPS), writes=["eps"])

        ph = Arena(ph_t, PH_BYTES)
        memx = ph.take(2 * D * 4, F32, "p (i d) -> p i d", i=2)
        for i in range(2):
            P.add("sp", (lambda e, i=i: e.dma_start(out=memx[:, i, :], in_=mem_d[128 * i:128 * (i + 1), :])),
                  writes=[("memx", i)], dma_chan=("mx", i))
        for i in range(NT):
            P.add("sp", (lambda e, i=i: e.dma_start(out=X[:, i, :], in_=x_d[128 * i:128 * (i + 1), :])),
                  writes=[("X", i, 0), ("X", i, 1)], dma_chan=("x", i))
        load_gain(mem_norm_d[0])
        rms_stats(lambda i: memx[:, i, :], lambda i: [("memx", i)], 2)
        for i in range(2):
            xb = XN[i % 2]
            P.add("dve", (lambda e, i=i, xb=xb: e.scalar_tensor_tensor(xb, memx[:, i, :], rstd[:, i:i + 1], G, ALU.mult, ALU.mult)),
                  reads=[("memx", i), "rstd", "G"], writes=[("XN", i % 2)])
            for c in range(KC):
                P.add("pe", (lambda e, c=c, xb=xb, i=i: e.transpose(PSB[i][:, 128 * c:128 * (c + 1)], xb[:, 128 * c:128 * (c + 1)], ident_b)),
                      reads=[("XN", i % 2), "ident_b"], writes=[("ps", i)])
            P.add("act", (lambda e, i=i: e.copy(memT[:, :, 128 * i:128 * (i + 1)], PSB[i].rearrange("p (k t) -> p k t", k=KC))),
                  reads=[("ps", i)], writes=[("memT", i)])
        P.barrier()

        for l in range(n_layers):
            ph = Arena(ph_t, PH_BYTES)
            WI = [ph.take(KC * 384 * 2, BF16, "p (k c) -> p k c", k=KC) for _ in range(2)]
            mix_base = ph.off
            w_in_v = w_in_d[l].rearrange("(k p) c -> p k c", p=128)
            w_out_v = w_out_d[l].rearrange("(k p) d -> p k d", p=128)

            load_gain(norm_mix_d[l])
            P.add("act", lambda e: e.dma_start(out=cw, in_=convw_d[l]), writes=["cw"], dma_chan="cw")

            def load_wi(buf, ranges):
                off = 0
                for (c0, n) in ranges:
                    P.add("pool", (lambda e, c0=c0, n=n, off=off: e.dma_start(out=WI[buf][:, :, off:off + n], in_=w_in_v[:, :, c0:c0 + n])),
                          writes=[("WI", buf, u) for u in range(off // 128, (off + n) // 128)], dma_chan=("wi", buf, off // 128))
                    off += n

            rms_stats(lambda i: X[:, i, :], lambda i: [("X", i, 0), ("X", i, 1)], NT)
            for i in range(NT):
                xb = XN[i % 2]
                pb = i % 2
                P.add("dve", (lambda e, i=i, xb=xb: e.scalar_tensor_tensor(xb, X[:, i, :], rstd[:, i:i + 1], G, ALU.mult, ALU.mult)),
                      reads=[("X", i, 0), ("X", i, 1), "rstd", "G"], writes=[("XN", i % 2)])
                for q in range(2):
                    bk = 2 * pb + q
                    for c4 in range(4):
                        c = 4 * q + c4
                        P.add("pe", (lambda e, c=c, c4=c4: e.matmul(PS[bk][:, 128 * c4:128 * (c4 + 1)], xb[:, 128 * c:128 * (c + 1)], ident_b,
                                                                    start=True, stop=True)),
                              reads=[("XN", i % 2), "ident_b"], writes=[("ps", bk)])
                    en = "act" if q == 0 else "dve"
                    P.add(en, copy_op(en, xnT[:, 4 * q:4 * q + 4, 128 * i:128 * (i + 1)], PS[bk][:, :].rearrange("p (k t) -> p k t", k=4)),
                          reads=[("ps", bk)], writes=[("xnT", i // 4, q)])

            dump("d_xnT", A, [("xnT", tb, q) for tb in range(4) for q in range(2)])

            def proj_feat(wi_ap, wi_keys, dst_fn, dst_key_fn, bank0, nbank=2, evac_scale=None):
                for tb in range(4):
                    bk = bank0 + tb % nbank
                    for k in range(KC):
                        P.add("pe", (lambda e, k=k, tb=tb, bk=bk: e.matmul(PS[bk][:, :], wi_ap[:, k, :], xnT[:, k, 512 * tb:512 * (tb + 1)],
                                                                         start=(k == 0), stop=(k == KC - 1))),
                              reads=wi_keys + xk(tb), writes=[("ps", bk)])
                    en = alt()
                    P.add(en, copy_op(en, dst_fn(tb), PS[bk][:, :]), reads=[("ps", bk)], writes=[dst_key_fn(tb)])

            ph.off = mix_base
            WOC = ph.take(3 * D * 2, BF16, "p (c d) -> p c d", c=3)
            P.add("pool", lambda e: e.dma_start(out=WOC, in_=w_out_v[:, 0:3, :]), writes=["WOC"], dma_chan="woc")
            u_pad = ph.take((S + 2) * 4 + 8, F32)
            tmpc = ph.take(S * 4, F32)
            bg_sb = ph.take(S * 4, F32)
            cg_sb = [ph.take(512 * 4, F32) for _ in range(2)]
            convT = ph.take(3 * S * 2, BF16, "p (c t) -> p c t", c=3)
            P.add("dve", lambda e: e.memset(u_pad[:, 0:1], 0.0), writes=["u_l"])
            P.add("dve", lambda e: e.memset(u_pad[:, S + 1:S + 2], 0.0), writes=["u_r"])
            for c in range(3):
                buf = c % 2
                load_wi(buf, [(128 * c, 128), (384 + 128 * c, 128), (768 + 128 * c, 128)])
                for tb in range(4):
                    for j in range(3):
                        bk = 2 + j
                        for k in range(KC):
                            P.add("pe", (lambda e, k=k, tb=tb, bk=bk, j=j, buf=buf: e.matmul(
                                PS[bk][:, :], WI[buf][:, k, 128 * j:128 * (j + 1)], xnT[:, k, 512 * tb:512 * (tb + 1)],
                                start=(k == 0), stop=(k == KC - 1))),
                                reads=[("WI", buf, j)] + xk(tb), writes=[("ps", bk)])
                    cb = cg_sb[tb % 2]
                    P.add("act", (lambda e, cb=cb: e.copy(cb, PS[4][:, :])), reads=[("ps", 4)], writes=[("cg", tb % 2)])
                    P.add("dve", (lambda e, cb=cb, tb=tb: e.tensor_tensor(u_pad[:, 1 + 512 * tb:1 + 512 * (tb + 1)], cb, PS[2][:, :], ALU.mult)),
                          reads=[("cg", tb % 2), ("ps", 2)], writes=[("u", tb)])
                    P.add("act", (lambda e, tb=tb: e.copy(bg_sb[:, 512 * tb:512 * (tb + 1)], PS[3][:, :])),
                          reads=[("ps", 3)], writes=[("bg", tb)])
                ukeys = [("u", tb) for tb in range(4)] + ["u_l", "u_r"]
                P.add("dve", (lambda e, c=c: e.tensor_scalar(tmpc, u_pad[:, 1:S + 1], cw[:, 3 * c + 1:3 * c + 2], None, ALU.mult)),
                      reads=ukeys + ["cw"], writes=["tmpc"])
                P.add("dve", (lambda e, c=c: e.scalar_tensor_tensor(tmpc, u_pad[:, 0:S], cw[:, 3 * c:3 * c + 1], tmpc, ALU.mult, ALU.add)),
                      reads=ukeys + ["cw", "tmpc"], writes=["tmpc"])
                P.add("dve", (lambda e, c=c: e.scalar_tensor_tensor(tmpc, u_pad[:, 2:S + 2], cw[:, 3 * c + 2:3 * c + 3], tmpc, ALU.mult, ALU.add)),
                      reads=ukeys + ["cw", "tmpc"], writes=["tmpc"])
                P.add("dve", (lambda e, c=c: e.tensor_tensor(convT[:, c, :], tmpc, bg_sb, ALU.mult)),
                      reads=["tmpc"] + [("bg", tb) for tb in range(4)], writes=[("convT", c)])
            for i in range(NT):
                for hf in range(2):
                    bk = 5 + (2 * i + hf) % 3
                    for c in range(3):
                        P.add("pe", (lambda e, i=i, hf=hf, c=c, bk=bk: e.matmul(
                            PS[bk][:, :], convT[:, c, 128 * i:128 * (i + 1)], WOC[:, c, 512 * hf:512 * (hf + 1)],
                            start=(c == 0), stop=(c == 2))),
                            reads=[("convT", c), "WOC"], writes=[("ps", bk)])
                    P.add("dve", (lambda e, i=i, hf=hf, bk=bk: e.tensor_tensor(
                        X[:, i, 512 * hf:512 * (hf + 1)], X[:, i, 512 * hf:512 * (hf + 1)], PS[bk][:, :], ALU.add)),
                        reads=[("ps", bk), ("X", i, hf)], writes=[("X", i, hf)])
            dump("d_convT", convT.rearrange("p c t -> p (c t)"), [("convT", c) for c in range(3)])
            dump_x("d_x_conv")
            P.barrier()

            pending = []

            import os as _os
            DEFER = _os.environ.get("K_DEFER", "1") == "1"

            def flush_pending():
                while pending:
                    pending.pop(0)()

            def attn_finish(par, hh, qb, oT_dst, att_key, r_row, o_sb):
                bo = 3 + par
                lrow = 64 if hh == 0 else 0
                olo = 0 if hh == 0 else 64
                lnl = o_sb[0]
                bcs = o_sb[1]
                P.add("act", (lambda e: e.activation(lnl[lrow:lrow + 1, :], PS[bo][lrow:lrow + 1, :], AF.Ln)), reads=[("ps", bo)], writes=["lnl"])
                P.add("act", (lambda e: e.activation(r_row[lrow:lrow + 1, :], lnl[lrow:lrow + 1, :], AF.Exp, scale=-1.0)), reads=["lnl"], writes=["r_row"])
                mo = 64 if hh == 0 else 128
                P.add("pe", lambda e: e.matmul(PS[5][0:mo, :], ones_f[lrow:lrow + 1, 0:mo], r_row[lrow:lrow + 1, :], start=True, stop=True),
                      reads=["r_row", "ones_f"], writes=[("ps", 5)])
                P.add("dve", (lambda e: e.tensor_copy(bcs[olo:olo + 64, :], PS[5][olo:olo + 64, :])), reads=[("ps", 5)], writes=["bcs"])
                P.add("dve", (lambda e: e.tensor_tensor(oT_dst[olo:olo + 64, 512 * qb:512 * (qb + 1)], PS[bo][olo:olo + 64, :], bcs[olo:olo + 64, :], ALU.mult)),
                      reads=[("ps", bo), "bcs"], writes=[att_key + (hh, qb)])

            def attn_outproj(oT_all, npair, att_key, WO_part):
                for i in range(NT):
                    for hf in range(2):
                        bk = 6 + (2 * i + hf) % 2
                        for hp_ in range(npair):
                            P.add("pe", (lambda e, hp_=hp_: e.matmul(
                                PS[bk][:, :], oT_all[:, hp_, 128 * i:128 * (i + 1)], WO_part[:, hp_, 512 * hf:512 * (hf + 1)],
                                start=(hp_ == 0), stop=(hp_ == npair - 1))),
                                reads=[(att_key, hp_, 0, i // 4), (att_key, hp_, 1, i // 4), "WO_part"], writes=[("ps", bk)])
                        P.add("dve", (lambda e: e.tensor_tensor(
                            X[:, i, 512 * hf:512 * (hf + 1)], X[:, i, 512 * hf:512 * (hf + 1)], PS[bk][:, :], ALU.add)),
                            reads=[("ps", bk), ("X", i, hf)], writes=[("X", i, hf)])

            def proj_q_pair(wi_ap, wi_key, qz_, zkey):
                for tb in range(4):
                    bk = 6 + tb % 2
                    for k in range(KC):
                        P.add("pe", (lambda e, k=k: e.matmul(PS[bk][:, :], wi_ap[:, k, :], xnT[:, k, 512 * tb:512 * (tb + 1)],
                                                             start=(k == 0), stop=(k == KC - 1))),
                              reads=[wi_key] + xk(tb), writes=[("ps", bk)])
                    P.add("act", (lambda e: e.copy(qz_[0][0:64, 512 * tb:512 * (tb + 1)], PS[bk][0:64, :])),
                          reads=[("ps", bk), zkey + "0pad"], writes=[(zkey, 0, tb)])
                    P.add("dve", (lambda e: e.tensor_copy(qz_[1][64:128, 512 * tb:512 * (tb + 1)], PS[bk][64:128, :])),
                          reads=[("ps", bk), zkey + "1pad"], writes=[(zkey, 1, tb)])

            ph.off = mix_base
            VB = 192
            v_flat = ph.take(NT * 3 * VB * 2, BF16)
            v4 = v_flat.rearrange("p (i h c) -> p i h c", i=NT, h=3)
            qz = [ph.take(S * 2, BF16) for _ in range(2)]
            kT = [ph.take(S * 2, BF16) for _ in range(2)]
            ET = ph.take(2 * ET_W * 2, BF16, "p (h v) -> p h v", h=2)
            oT_all = ph.take(3 * S * 2, BF16, "p (h t) -> p h t", h=3)
            WOD = ph.take(3 * D * 2, BF16, "p (c d) -> p c d", c=3)
            e_sb = [ph.take(512 * 2, BF16) for _ in range(3)]
            p_sb = [ph.take(512 * 2, BF16) for _ in range(3)]
            o_sb = [ph.take(512 * 4, F32) for _ in range(2)]
            r_row = ph.take(512 * 4, F32)
            P.add("dve", lambda e: e.memset(qz[0][64:128, :], 0.0), writes=["qz0pad"])
            P.add("dve", lambda e: e.memset(qz[1][0:64, :], 0.0), writes=["qz1pad"])
            P.add("dve", lambda e: e.memset(v4[:, :, :, 64:128], 0.0), writes=["v_ones"])
            P.add("dve", lambda e: e.memset(v4[:, :, :, 64:65], 1.0), reads=["v_ones"], writes=["v_ones"])
            P.add("dve", lambda e: e.memset(v4[:, :, :, 96:97], 1.0), reads=["v_ones"], writes=["v_ones"])
            P.add("pool", lambda e: e.dma_start(out=WOD, in_=w_out_d[l][384:768, :].rearrange("(c p) d -> p c d", p=128)),
                  writes=["WO_part"], dma_chan="wod")
            load_wi(0, [(1920, 384)])
            for i in range(NT):
                bk = i % 2
                for k in range(KC):
                    P.add("pe", (lambda e, k=k: e.matmul(PS[bk][:, 0:384], xnT[:, k, 128 * i:128 * (i + 1)], WI[0][:, k, 0:384],
                                                         start=(k == 0), stop=(k == KC - 1))),
                          reads=[("WI", 0, 0), ("WI", 0, 1), ("WI", 0, 2)] + xk(i // 4), writes=[("ps", bk)])
                psv = PS[bk][:, 0:384].rearrange("p (h two c) -> p h two c", h=3, two=2)
                en = "act" if i % 2 == 0 else "dve"
                P.add(en, copy_op(en, v4[:, i, :, 0:64], psv[:, :, 0, :]), reads=[("ps", bk)], writes=[("vA", i)])
                P.add(en, copy_op(en, v4[:, i, :, 128:192], psv[:, :, 1, :]), reads=[("ps", bk)], writes=[("vB", i)])

            for hp in range(3):
                buf = (hp + 1) % 2
                load_wi(buf, [(1152 + 128 * hp, 128), (1536 + 128 * hp, 128)])
                P.add("sp", (lambda e: e.dma_start(out=ET, in_=etab_d[2 * hp:2 * hp + 2].rearrange("h p v -> p h v"))),
                      writes=["ET"], dma_chan="et")
                kb_ = kT[hp % 2]
                proj_q_pair(WI[buf][:, :, 0:128], ("WI", buf, 0), qz, "qz")
                proj_feat(WI[buf][:, :, 128:256], [("WI", buf, 1)], (lambda tb, kb_=kb_: kb_[:, 512 * tb:512 * (tb + 1)]),
                          (lambda tb, hp=hp: ("kT", hp % 2, tb)), 6)
                gi = 0
                for hh in range(2):
                    h = 2 * hp + hh
                    for qb in range(4):
                        kts = []
                        for kt in range(max(0, 4 * qb - 8), min(15, 4 * qb + 11) + 1):
                            v0 = ET_OFF - (128 * kt - 512 * qb)
                            if etab_nz[h][:, v0:v0 + 512].any():
                                kts.append(kt)
                        par = gi % 2
                        gi += 1
                        bo = 3 + par

                        def s_mm(kt, n):
                            bs = n % 3
                            P.add("pe", (lambda e: e.matmul(PS[bs][:, :], kb_[:, 128 * kt:128 * (kt + 1)],
                                                            qz[hh][:, 512 * qb:512 * (qb + 1)], start=True, stop=True)),
                                  reads=[("kT", hp % 2, kt // 4), ("qz", hh, qb), "qz%dpad" % hh], writes=[("ps", bs)])

                        s_mm(kts[0], 0)
                        if len(kts) > 1:
                            s_mm(kts[1], 1)
                        for n, kt in enumerate(kts):
                            bs = n % 3
                            v0 = ET_OFF - (128 * kt - 512 * qb)
                            P.add("act", (lambda e: e.activation(e_sb[bs], PS[bs][:, :], AF.Exp, scale=0.125)),
                                  reads=[("ps", bs)], writes=[("e_sb", bs)])
                            P.add("dve", (lambda e: e.tensor_tensor(p_sb[bs], e_sb[bs], ET[:, hh, v0:v0 + 512], ALU.mult)),
                                  reads=[("e_sb", bs), "ET"], writes=[("p_sb", bs)])
                            if n + 2 < len(kts):
                                s_mm(kts[n + 2], n + 2)
                            voff = (kt * 3 + hp) * VB + 64 * hh
                            P.add("pe", (lambda e: e.matmul(PS[bo][:, :], v_flat[:, voff:voff + 128], p_sb[bs],
                                                            start=(n == 0), stop=(n == len(kts) - 1))),
                                  reads=[("p_sb", bs), ("vA", kt), ("vB", kt), "v_ones"], writes=[("ps", bo)])
                            if n == min(1, len(kts) - 1):
                                flush_pending()
                        pending.append(lambda par=par, hh=hh, qb=qb, hp=hp: attn_finish(
                            par, hh, qb, oT_all[:, hp, :], ("oT", hp), r_row, o_sb))
                        if not DEFER:
                            flush_pending()
            flush_pending()
            attn_outproj(oT_all, 3, "oT", WOD)
            P.barrier()

            ph.off = mix_base
            if skip_mem:
                continue
            WMKV = ph.take(KC * 512 * 2, BF16, "p (k c) -> p k c", k=KC)
            kmT = ph.take(2 * MEM * 2, BF16, "p (h t) -> p h t", h=2)
            vm_flat = ph.take(2 * 2 * VB * 2, BF16)
            vm4 = vm_flat.rearrange("p (i h c) -> p i h c", i=2, h=2)
            qmz = [ph.take(S * 2, BF16) for _ in range(2)]
            oTm_all = ph.take(2 * S * 2, BF16, "p (h t) -> p h t", h=2)
            WOM = ph.take(2 * D * 2, BF16, "p (c d) -> p c d", c=2)
            p_sb = [ph.take(512 * 2, BF16) for _ in range(4)]
            o_sb = [ph.take(512 * 4, F32) for _ in range(2)]
            r_row = ph.take(512 * 4, F32)
            P.add("dve", lambda e: e.memset(qmz[0][64:128, :], 0.0), writes=["qmz0pad"])
            P.add("dve", lambda e: e.memset(qmz[1][0:64, :], 0.0), writes=["qmz1pad"])
            P.add("dve", lambda e: e.memset(vm4[:, :, :, 64:128], 0.0), writes=["vm_ones"])
            P.add("dve", lambda e: e.memset(vm4[:, :, :, 64:65], 1.0), reads=["vm_ones"], writes=["vm_ones"])
            P.add("dve", lambda e: e.memset(vm4[:, :, :, 96:97], 1.0), reads=["vm_ones"], writes=["vm_ones"])
            P.add("pool", lambda e: e.dma_start(out=WMKV, in_=w_mkv_d[l].rearrange("(k p) c -> p k c", p=128)), writes=["WMKV"], dma_chan="wmkv")
            P.add("pool", lambda e: e.dma_start(out=WOM, in_=w_out_d[l][768:1024, :].rearrange("(c p) d -> p c d", p=128)),
                  writes=["WO_part"], dma_chan="wod")
            for mp in range(2):
                for k in range(KC):
                    P.add("pe", (lambda e, k=k: e.matmul(PS[0][:, 0:MEM], WMKV[:, k, 128 * mp:128 * (mp + 1)], memT[:, k, :],
                                                         start=(k == 0), stop=(k == KC - 1))),
                          reads=["WMKV", ("memT", 0), ("memT", 1)], writes=[("ps", 0)])
                P.add("act", (lambda e: e.copy(kmT[:, mp, :], PS[0][:, 0:MEM])), reads=[("ps", 0)], writes=[("kmT", mp)])
            for i in range(2):
                for k in range(KC):
                    P.add("pe", (lambda e, k=k: e.matmul(PS[1][:, 0:256], memT[:, k, 128 * i:128 * (i + 1)], WMKV[:, k, 256:512],
                                                         start=(k == 0), stop=(k == KC - 1))),
                          reads=["WMKV", ("memT", 0), ("memT", 1)], writes=[("ps", 1)])
                psv = PS[1][:, 0:256].rearrange("p (h two c) -> p h two c", h=2, two=2)
                P.add("act", (lambda e: e.copy(vm4[:, i, :, 0:64], psv[:, :, 0, :])), reads=[("ps", 1)], writes=[("vmA", i)])
                P.add("act", (lambda e: e.copy(vm4[:, i, :, 128:192], psv[:, :, 1, :])), reads=[("ps", 1)], writes=[("vmB", i)])
            vmkeys = [("vmA", 0), ("vmA", 1), ("vmB", 0), ("vmB", 1), "vm_ones"]
            gi = 0
            for mp in range(2):
                buf = mp % 2
                load_wi(buf, [(2304 + 128 * mp, 128)])
                proj_q_pair(WI[buf][:, :, 0:128], ("WI", buf, 0), qmz, "qmz")
                for hh in range(2):
                    for qb in range(4):
                        par = gi % 2
                        sb0 = 2 * (gi % 2)
                        gi += 1
                        bo = 3 + par
                        for kt in range(2):
                            bs = sb0 + kt
                            P.add("pe", (lambda e: e.matmul(PS[bs if bs < 3 else 7][:, :], kmT[:, mp, 128 * kt:128 * (kt + 1)],
                                                            qmz[hh][:, 512 * qb:512 * (qb + 1)], start=True, stop=True)),
                                  reads=[("kmT", mp), ("qmz", hh, qb), "qmz%dpad" % hh], writes=[("ps", bs if bs < 3 else 7)])
                        for kt in range(2):
                            bs = sb0 + kt
                            pbk = bs if bs < 3 else 7
                            P.add("act", (lambda e: e.activation(p_sb[bs], PS[pbk][:, :], AF.Exp, scale=0.125)),
                                  reads=[("ps", pbk)], writes=[("p_sb", bs)])
                            if kt == 0:
                                flush_pending()
                            voff = (kt * 2 + mp) * VB + 64 * hh
                            P.add("pe", (lambda e: e.matmul(PS[bo][:, :], vm_flat[:, voff:voff + 128], p_sb[bs], start=(kt == 0), stop=(kt == 1))),
                                  reads=[("p_sb", bs)] + vmkeys, writes=[("ps", bo)])
                        pending.append(lambda par=par, hh=hh, qb=qb, mp=mp: attn_finish(
                            par, hh, qb, oTm_all[:, mp, :], ("oTm", mp), r_row, o_sb))
            flush_pending()
            attn_outproj(oTm_all, 2, "oTm", WOM)
            P.barrier()

            if not do_moe:
                continue
            ph = Arena(ph_t, PH_BYTES)
            WG = [ph.take(KC * 512 * 2, BF16, "p (k f) -> p k f", k=KC) for _ in range(2)]
            WU = [ph.take(KC * 512 * 2, BF16, "p (k f) -> p k f", k=KC) for _ in range(2)]
            WD = [ph.take(4 * D * 2, BF16, "p (c d) -> p c d", c=4) for _ in range(2)]
            WR = ph.take(KC * E * 4, F32, "p (k e) -> p k e", k=KC)
            moe_base = ph.off

            def load_expert_gu(e_, fs):
                buf = (e_ * 4 + fs) % 2
                P.add("pool", (lambda e: e.dma_start(
                    out=WG[buf], in_=w_gate_d[l, e_].rearrange("(k p) f -> p k f", p=128)[:, :, 512 * fs:512 * (fs + 1)])),
                    writes=[("WG", buf)], dma_chan=("wg", buf))
                P.add("pool", (lambda e: e.dma_start(
                    out=WU[buf], in_=w_up_d[l, e_].rearrange("(k p) f -> p k f", p=128)[:, :, 512 * fs:512 * (fs + 1)])),
                    writes=[("WU", buf)], dma_chan=("wu", buf))

            def load_expert_d(e_, fs):
                buf = (e_ * 4 + fs) % 2
                P.add("pool", (lambda e: e.dma_start(
                    out=WD[buf], in_=w_down_d[l, e_][512 * fs:512 * (fs + 1), :].rearrange("(c p) d -> p c d", p=128))),
                    writes=[("WD", buf)], dma_chan=("wd", buf))

            def load_expert_w(e_, fs):
                load_expert_gu(e_, fs)
                load_expert_d(e_, fs)

            load_gain(norm_ffn_d[l])
            P.add("act", lambda e: e.dma_start(out=WR, in_=w_router_d[l].rearrange("(k p) e -> p k e", p=128)), writes=["WR"], dma_chan="wr")
            load_expert_w(0, 0)
            load_expert_w(0, 1)

            affT = ph.take(S * 4, F32)
            cjunk = ph.take(S * 2, BF16)
            maskT = ph.take(S * 4, F32)
            r_base = ph.off
            xnf = [ph.take(D * 4, F32) for _ in range(2)]
            xnTf = [ph.take(KC * 128 * 4, F32, "p (k t) -> p k t", k=KC) for _ in range(2)]
            ph.off = r_base
            cums = ph.take(S * 4, F32)
            selp = ph.take(S * 4, F32)
            lg_sb = [ph.take(128 * 4, F32) for _ in range(2)]
            tq = ph.take(8 * 4, F32)
            rms_stats(lambda i: X[:, i, :], lambda i: [("X", i, 0), ("X", i, 1)], NT)

            def softmax_tile(i):
                lb = 3 + i % 2
                P.add("dve", lambda e: e.reduce_max(sm[:, 0:1], PS[lb][:, 0:E], axis=AX.X), reads=[("ps", lb)], writes=["sm0"])
                P.add("dve", lambda e: e.tensor_scalar(sm[:, 1:2], sm[:, 0:1], -1.0, None, ALU.mult), reads=["sm0"], writes=["sm1"])
                P.add("act", (lambda e: e.activation(aff_all[:, i, :], PS[lb][:, 0:E], AF.Exp, bias=sm[:, 1:2], scale=1.0, accum_out=sm[:, 2:3])),
                      reads=[("ps", lb), "sm1"], writes=[("aff", i), "sm2"])
                P.add("dve", lambda e: e.reciprocal(sm[:, 3:4], sm[:, 2:3]), reads=["sm2"], writes=["sm3"])
                P.add("dve", (lambda e: e.tensor_scalar(aff_all[:, i, :], aff_all[:, i, :], sm[:, 3:4], None, ALU.mult)),
                      reads=[("aff", i), "sm3"], writes=[("aff", i)])

            def stage_a(i):
                xf = xnf[i % 2]
                P.add("dve", (lambda e: e.scalar_tensor_tensor(xf, X[:, i, :], rstd[:, i:i + 1], G, ALU.mult, ALU.mult)),
                      reads=[("X", i, 0), ("X", i, 1), "rstd", "G"], writes=[("xnf", i % 2)])
                P.add("act", (lambda e: e.copy(xnTok[:, i, :], xf)), reads=[("xnf", i % 2)], writes=[("xnTok", i)])

            def stage_b(i):
                xf = xnf[i % 2]
                xt = xnTf[i % 2]
                lb = 3 + i % 2
                for c in range(KC):
                    bk = c // 4
                    P.add("pe", (lambda e, c=c, bk=bk: e.transpose(PS[bk][:, 128 * (c % 4):128 * (c % 4 + 1)], xf[:, 128 * c:128 * (c + 1)], ident_f)),
                          reads=[("xnf", i % 2), "ident_f"], writes=[("ps", bk)])
                P.add("act", lambda e: e.copy(xt[:, 0:4, :], PS[0][:, :].rearrange("p (k t) -> p k t", k=4)), reads=[("ps", 0)], writes=[("xnTf", i % 2, 0)])
                P.add("dve", lambda e: e.tensor_copy(xt[:, 4:8, :], PS[1][:, :].rearrange("p (k t) -> p k t", k=4)), reads=[("ps", 1)], writes=[("xnTf", i % 2, 1)])
                for k in range(KC):
                    P.add("pe", (lambda e, k=k: e.matmul(PS[2][0:E, 0:128], WR[:, k, :], xt[:, k, :], start=(k == 0), stop=(k == KC - 1))),
                          reads=[("xnTf", i % 2, k // 4), "WR"], writes=[("ps", 2)])
                P.add("act", (lambda e: e.copy(lg_sb[i % 2][0:E, :], PS[2][0:E, 0:128])), reads=[("ps", 2)], writes=[("lg", i % 2)])
                P.add("pe", (lambda e: e.transpose(PS[lb][:, 0:E], lg_sb[i % 2][0:E, :], ident_f[0:E, 0:E])),
                      reads=[("lg", i % 2), "ident_f"], writes=[("ps", lb)])

            stage_a(0)
            for i in range(NT):
                if i + 1 < NT:
                    stage_a(i + 1)
                if i > 0:
                    softmax_tile(i - 1)
                stage_b(i)
            softmax_tile(NT - 1)
            for i in range(NT):
                bk = 5 + (i // 4) % 2
                P.add("pe", (lambda e, i=i, bk=bk: e.transpose(PS[bk][0:E, 128 * (i % 4):128 * (i % 4 + 1)], aff_all[:, i, :], ident_f)),
                      reads=[("aff", i), "ident_f"], writes=[("ps", bk)])
                if i % 4 == 3:
                    j = i // 4
                    P.add("act", (lambda e, j=j, bk=bk: e.copy(affT[0:E, 512 * j:512 * (j + 1)], PS[bk][0:E, :])), reads=[("ps", bk)], writes=[("affT", j)])
            P.barrier()
            affT_keys = [("affT", j) for j in range(4)]
            P.add("dve", lambda e: e.memset(tq[0:E, 0:1], 0.0), writes=["tq_t"])
            for kbit in range(1, 31):
                step = 2.0 ** (-kbit)
                P.add("dve", lambda e: e.tensor_scalar(tq[0:E, 1:2], tq[0:E, 0:1], step, None, ALU.add), reads=["tq_t"], writes=["tq_c"])
                P.add("dve", lambda e: e.tensor_scalar(cjunk[0:E, :], affT[0:E, :], tq[0:E, 1:2], None, ALU.is_ge, ALU.add, accum_out=tq[0:E, 2:3]),
                      reads=affT_keys + ["tq_c"], writes=["cjunk", "tq_n"])
                P.add("dve", lambda e: e.tensor_scalar(tq[0:E, 3:4], tq[0:E, 2:3], CAP - 0.5, step, ALU.is_ge, ALU.mult), reads=["tq_n"], writes=["tq_g"])
                P.add("dve", lambda e: e.tensor_tensor(tq[0:E, 0:1], tq[0:E, 0:1], tq[0:E, 3:4], ALU.add), reads=["tq_t", "tq_g"], writes=["tq_t"])
            P.add("dve", lambda e: e.tensor_scalar(maskT[0:E, :], affT[0:E, :], tq[0:E, 0:1], None, ALU.is_ge), reads=affT_keys + ["tq_t"], writes=["maskT"])
            P.add("dve", lambda e: e.tensor_tensor_scan(cums[0:E, :], maskT[0:E, :], maskT[0:E, :], 0.0, ALU.add, ALU.max),
                  reads=["maskT"], writes=["cums"])
            P.add("dve", lambda e: e.tensor_tensor(selp[0:E, :], cums[0:E, :], maskT[0:E, :], ALU.mult), reads=["cums", "maskT"], writes=["selp"])
            for i in range(NT):
                P.add("pe", (lambda e, i=i: e.transpose(PS[7][:, E * i:E * (i + 1)], selp[0:E, 128 * i:128 * (i + 1)], ident_f[0:E, 0:E])),
                      reads=["selp", "ident_f"], writes=[("ps", 7)])
            P.add("act", lambda e: e.copy(sel_all, PS[7][:, 0:NT * E].rearrange("p (i e) -> p i e", i=NT)), reads=[("ps", 7)], writes=["sel"])
            P.barrier()

            ph.off = moe_base
            Ssel = ph.take(NT * CAP * 2, BF16, "p (i j) -> p i j", i=NT)
            Sg = ph.take(8 * CAP * 2, BF16, "p (i j) -> p i j", i=8)
            SgT = [ph.take(2 * S * 2, BF16, "p (j t) -> p j t", j=2) for _ in range(2)]
            xgT = ph.take(KC * CAP * 2, BF16, "p (k j) -> p k j", k=KC)
            hT = [ph.take(4 * CAP * 2, BF16, "p (c j) -> p c j", c=4) for _ in range(2)]
            sg_sb = [ph.take(CAP * 4, F32) for _ in range(2)]
            y_sb = ph.take(2 * D * 2, BF16, "p (j d) -> p j d", j=2)
            xg_keys = [("xgT", c) for c in range(KC)]

            def scatter_unit(es, u):
                i, hf = u // 2, u % 2
                bk = 6 + u % 2
                for jc in range(2):
                    P.add("pe", (lambda e, jc=jc: e.matmul(
                        PS[bk][:, :], SgT[es % 2][:, jc, 128 * i:128 * (i + 1)], y_sb[:, jc, 512 * hf:512 * (hf + 1)], start=(jc == 0), stop=(jc == 1))),
                        reads=[("SgT", es % 2, jc, i // 8, (i % 8) // 4), ("y", jc, hf)], writes=[("ps", bk)])
                P.add("dve", (lambda e: e.tensor_tensor(
                    X[:, i, 512 * hf:512 * (hf + 1)], X[:, i, 512 * hf:512 * (hf + 1)], PS[bk][:, :], ALU.add)),
                    reads=[("ps", bk), ("X", i, hf)], writes=[("X", i, hf)])

            def build_A(e_):
                th = []
                for i in range(NT):
                    th.append(lambda i=i: P.add("dve", (lambda e: e.tensor_scalar(Ssel[:, i, :], iota1, sel_all[:, i, e_:e_ + 1], None, ALU.is_equal)),
                                                reads=["iota1", "sel"], writes=[("Ssel", i)]))
                for ii in range(8):
                    th.append(lambda ii=ii: P.add("dve", (lambda e: e.tensor_scalar(Sg[:, ii, :], iota1, sel_all[:, ii, e_:e_ + 1], aff_all[:, ii, e_:e_ + 1],
                                                                                    ALU.is_equal, ALU.mult)),
                                                  reads=["iota1", "sel", ("aff", ii)], writes=[("Sg", ii)]))
                return th

            def build_B(e_):
                for ii in range(8):
                    i = 8 + ii
                    P.add("dve", (lambda e: e.tensor_scalar(Sg[:, ii, :], iota1, sel_all[:, i, e_:e_ + 1], aff_all[:, i, e_:e_ + 1],
                                                            ALU.is_equal, ALU.mult)),
                          reads=["iota1", "sel", ("aff", i)], writes=[("Sg", ii)])

            def sgt_half(e_, half):
                for jc in range(2):
                    for q in range(2):
                        bk = 6 + q
                        for i4 in range(4):
                            ii = 4 * q + i4
                            P.add("pe", (lambda e: e.matmul(PS[bk][:, 128 * i4:128 * (i4 + 1)], Sg[:, ii, 128 * jc:128 * (jc + 1)], ident_b,
                                                            start=True, stop=True)),
                                  reads=[("Sg", ii), "ident_b"], writes=[("ps", bk)])
                        en = alt()
                        c0 = 1024 * half + 512 * q
                        P.add(en, copy_op(en, SgT[e_ % 2][:, jc, c0:c0 + 512], PS[bk][:, :]), reads=[("ps", bk)],
                              writes=[("SgT", e_ % 2, jc, half, q)])

            for t_ in build_A(0):
                t_()
            for e_ in range(E):
                sgt_half(e_, 0)
                build_B(e_)
                for c in range(KC):
                    bk = 6 + c % 2
                    for i in range(NT):
                        P.add("pe", (lambda e, c=c, i=i, bk=bk: e.matmul(PS[bk][:, 0:CAP], xnTok[:, i, 128 * c:128 * (c + 1)], Ssel[:, i, :],
                                                                         start=(i == 0), stop=(i == NT - 1))),
                              reads=[("xnTok", i), ("Ssel", i)], writes=[("ps", bk)])
                    en = alt()
                    P.add(en, copy_op(en, xgT[:, c, :], PS[bk][:, 0:CAP]), reads=[("ps", bk)], writes=[("xgT", c)])
                sgt_half(e_, 1)
                nextA = build_A(e_ + 1) if e_ + 1 < E else []

                def gate_up(f):
                    fs, fc = f // 4, f % 4
                    buf = (e_ * 4 + fs) % 2
                    bk = 4 + f % 2
                    for k in range(KC):
                        P.add("pe", (lambda e, k=k: e.matmul(PS[bk][:, 0:CAP], WG[buf][:, k, 128 * fc:128 * (fc + 1)], xgT[:, k, :],
                                                             start=(k == 0), stop=(k == KC - 1))),
                              reads=[("WG", buf)] + xg_keys, writes=[("ps", bk)])
                    for k in range(KC):
                        P.add("pe", (lambda e, k=k: e.matmul(PS[bk][:, CAP:2 * CAP], WU[buf][:, k, 128 * fc:128 * (fc + 1)], xgT[:, k, :],
                                                             start=(k == 0), stop=(k == KC - 1))),
                              reads=[("WU", buf)] + xg_keys, writes=[("ps", bk)])

                gate_up(0)
                for f in range(16):
                    fs, fc = f // 4, f % 4
                    buf = (e_ * 4 + fs) % 2
                    bk = 4 + f % 2
                    hb = hT[fs % 2]
                    sgb = sg_sb[f % 2]
                    P.add("act", (lambda e: e.activation(sgb, PS[bk][:, 0:CAP], AF.Silu)), reads=[("ps", bk)], writes=[("sg", f % 2)])
                    P.add("dve", (lambda e: e.tensor_tensor(hb[:, fc, :], sgb, PS[bk][:, CAP:2 * CAP], ALU.mult)),
                          reads=[("sg", f % 2), ("ps", bk)], writes=[("hT", fs % 2, fc)])
                    if f + 1 < 16:
                        gate_up(f + 1)
                        if (f + 1) % 4 == 3:
                            nxt = e_ * 4 + (f + 1) // 4 + 2
                            if nxt < E * 4:
                                load_expert_gu(nxt // 4, nxt % 4)
                    for jc in range(2):
                        for hf in range(2):
                            by = 2 * jc + hf
                            P.add("pe", (lambda e, jc=jc, hf=hf, by=by: e.matmul(
                                PS[by][:, :], hb[:, fc, 128 * jc:128 * (jc + 1)], WD[buf][:, fc, 512 * hf:512 * (hf + 1)],
                                start=(f == 0), stop=(f == 15))),
                                reads=[("hT", fs % 2, fc), ("WD", buf)], writes=[("ps", by)])
                    if e_ > 0:
                        scatter_unit(e_ - 1, 2 * f)
                        scatter_unit(e_ - 1, 2 * f + 1)
                    for _ in range(2):
                        if nextA:
                            nextA.pop(0)()
                    if fc == 3:
                        nxt = e_ * 4 + fs + 2
                        if nxt < E * 4:
                            load_expert_d(nxt // 4, nxt % 4)
                for jc in range(2):
                    for hf in range(2):
                        by = 2 * jc + hf
                        en = alt()
                        P.add(en, copy_op(en, y_sb[:, jc, 512 * hf:512 * (hf + 1)], PS[by][:, :]), reads=[("ps", by)], writes=[("y", jc, hf)])
            for u in range(32):
                scatter_unit(E - 1, u)
            P.barrier()

        ph = Arena(ph_t, PH_BYTES)
        ob = [ph.take(D * 4, F32) for _ in range(4)]
        if debug:
            for i in range(NT):
                final_ops.append(P.add("sp", (lambda e, i=i: e.dma_start(out=out_d[128 * i:128 * (i + 1), :], in_=X[:, i, :])),
                                       reads=[("X", i, 0), ("X", i, 1)], dma_chan="dbg", extra_deps=final_ops[-1:]))
        else:
            load_gain(norm_final_d[0])
            rms_stats(lambda i: X[:, i, :], lambda i: [("X", i, 0), ("X", i, 1)], NT)
            for i in range(NT):
                P.add("dve", (lambda e, i=i: e.scalar_tensor_tensor(ob[i % 4], X[:, i, :], rstd[:, i:i + 1], G, ALU.mult, ALU.mult)),
                      reads=[("X", i, 0), ("X", i, 1), "rstd", "G"], writes=[("ob", i % 4)])
                final_ops.append(P.add("sp", (lambda e, i=i: e.dma_start(out=out_d[128 * i:128 * (i + 1), :], in_=ob[i % 4])),
                                       reads=[("ob", i % 4)], writes=[("obd", i % 4)], dma_chan=("o", i % 4)))
        P.emit(nc, final_waits=[("sp", o) for o in final_ops])
    return nc


def _etab():
    t = np.zeros((6, 128, ET_W), np.float64)
    p = np.arange(128)[:, None]
    v = np.arange(ET_W)[None, :]
    d = p - v + ET_OFF
    ad = np.abs(d)
    m = (ad <= 64).astype(np.float64) + ((d % 4 == 0) & (ad <= 256)) + ((d % 16 == 0) & (ad <= 1024))
    for h in range(6):
        slope = 2.0 ** (-8.0 * (h + 1) / 6)
        t[h] = m * np.exp(-slope * ad)
    return t.astype(np.float32).astype(ml_dtypes.bfloat16)


def _consts():
    return {
        "ident_bf": np.eye(128, dtype=np.float32).astype(ml_dtypes.bfloat16),
        "ident_f": np.eye(128, dtype=np.float32),
        "iota1": np.tile(np.arange(1, CAP + 1, dtype=np.float32)[None, :], (128, 1)),
        "etab": _etab(),
    }


def make_in_maps(x, mem, mem_norm, norm_mix, w_in, conv_w, w_mem_kv, w_out, norm_ffn,
                 w_router, w_gate, w_up, w_down, norm_final):
    f = lambda a: np.ascontiguousarray(np.asarray(a, dtype=np.float32))
    conv_w = f(conv_w)
    convw_t = np.ascontiguousarray(conv_w.reshape(L, 3, 3, 128).transpose(0, 3, 2, 1).reshape(L, 128, 9))
    shared = {
        "mem_norm": f(mem_norm).reshape(1, D), "norm_mix": f(norm_mix), "norm_ffn": f(norm_ffn),
        "norm_final": f(norm_final).reshape(1, D), "w_in": f(w_in), "convw_t": convw_t,
        "w_mem_kv": f(w_mem_kv), "w_out": f(w_out), "w_router": f(w_router),
        "w_gate": f(w_gate), "w_up": f(w_up), "w_down": f(w_down),
    }
    shared.update(_consts())
    x = f(x)
    mem = f(mem)
    return [dict(shared, x=x[b], mem=mem[b]) for b in range(N_CORES)]


def kernel(x, mem, mem_norm, norm_mix, w_in, conv_w, w_mem_kv, w_out, norm_ffn,
           w_router, w_gate, w_up, w_down, norm_final):
    in_maps = make_in_maps(x, mem, mem_norm, norm_mix, w_in, conv_w, w_mem_kv, w_out, norm_ffn,
                           w_router, w_gate, w_up, w_down, norm_final)
    nc = build_program()
    res = run_bass_kernel_spmd(nc, in_maps, core_ids=list(range(N_CORES)))
    return np.stack([np.asarray(r["out"], dtype=np.float32) for r in res.results], axis=0)
```
